# Optimizing a Trainium2 kernel written in Bass

```python
import math, functools
import jax, jax.numpy as jnp
from jax import lax
import numpy as np

D_MODEL = 1024
BATCH = 4
SEQ = 4096
DEPTH = 4

GRID_W = 64
CTX_LEN = 256
N_MIXERS = 4
EPS = 1e-6
CONV_W = 5
NEG_BIG = -1e30

N_SSD = (DEPTH + 3) // 4
N_HGRN = (DEPTH + 2) // 4
N_ATTN = (DEPTH + 1) // 4
N_MLSTM = DEPTH // 4
N_DENSE = (DEPTH + 1) // 2
N_MOE = DEPTH // 2

SSD_INNER = 2 * D_MODEL
SSD_HEAD_DIM = 64
SSD_HEADS = SSD_INNER // SSD_HEAD_DIM
SSD_STATE = 128
SSD_GROUPS = 8
SSD_R = SSD_HEADS // SSD_GROUPS
SSD_CHUNK = 128
SSD_CONV_DIM = SSD_INNER + 2 * SSD_GROUPS * SSD_STATE
SSD_IN_DIM = SSD_INNER + SSD_CONV_DIM + 2 * SSD_HEADS

HGRN_DK = 128
HGRN_HEADS = D_MODEL // HGRN_DK
HGRN_DV = D_MODEL // HGRN_HEADS
HGRN_CHUNK = 64

HEAD_DIM = 64
N_HEADS = D_MODEL // HEAD_DIM
N_KV = N_HEADS // 4
GQA_R = N_HEADS // N_KV
ATTN_SCALE = HEAD_DIM ** -0.5
ROPE_THETA = 10000.0
ROPE_FREQS = HEAD_DIM // 4
Q_BLOCK = 128

MLSTM_INNER = 2 * D_MODEL
MLSTM_HEADS = 4
MLSTM_DH = MLSTM_INNER // MLSTM_HEADS
MLSTM_CHUNK = 128

D_FF = 7 * D_MODEL // 2
N_EXPERTS = 8
TOP_K = 2

kernel_name = 'hybrid_diffusion_trunk'


def rms_norm(x, g):
    xf = x.astype(jnp.float32)
    y = xf * lax.rsqrt(jnp.mean(xf * xf, axis=-1, keepdims=True) + EPS)
    return (y * g.astype(jnp.float32)).astype(x.dtype)


def modulate(h, shift, scale):
    return h * (1.0 + scale) + shift


def dw_conv_centred(x, w, b):
    pad = w.shape[0] // 2
    y = lax.conv_general_dilated(x, w[:, None, :], window_strides=(1,), padding=[(pad, pad)],
                                 dimension_numbers=('NWC', 'WIO', 'NWC'), feature_group_count=x.shape[-1])
    return y + b


def axial_rope(seq_len):
    rows = seq_len // GRID_W
    row = jnp.repeat(jnp.arange(rows, dtype=jnp.float32), GRID_W)
    col = jnp.tile(jnp.arange(GRID_W, dtype=jnp.float32), rows)
    inv = ROPE_THETA ** (-jnp.arange(ROPE_FREQS, dtype=jnp.float32) / ROPE_FREQS)
    ang_r = row[:, None] * inv
    ang_c = col[:, None] * inv
    ang = jnp.concatenate([ang_r, ang_r, ang_c, ang_c], axis=-1)
    return jnp.cos(ang)[:, None, :], jnp.sin(ang)[:, None, :]


def apply_rope(x, cos, sin):
    xf = x.astype(jnp.float32)
    xr = xf.reshape(*x.shape[:-1], 2, 2, ROPE_FREQS)
    rot = jnp.stack([-xr[..., 1, :], xr[..., 0, :]], axis=-2).reshape(x.shape)
    return (xf * cos + rot * sin).astype(x.dtype)


def bidir_scan(scan_fn, ctx_f, ctx_b, lat_f, lat_b, par_f, par_b, init, want_ctx):
    rev = lambda seqs: tuple(jnp.flip(a, axis=1) for a in seqs)
    yc_f, s_f = scan_fn(ctx_f, par_f, init)
    yc_b, s_b = scan_fn(rev(ctx_b), par_b, init)
    yx_f, _ = scan_fn(lat_f, par_f, s_f)
    yx_b, _ = scan_fn(rev(lat_b), par_b, s_b)
    y_lat = yx_f + jnp.flip(yx_b, axis=1)
    y_ctx = (yc_f + jnp.flip(yc_b, axis=1)) if want_ctx else None
    return y_lat, y_ctx


def _chunks(a, length):
    bsz, t = a.shape[:2]
    return jnp.moveaxis(a.reshape(bsz, t // length, length, *a.shape[2:]), 1, 0)


def _unchunk(a):
    a = jnp.moveaxis(a, 0, 1)
    return a.reshape(a.shape[0], a.shape[1] * a.shape[2], *a.shape[3:])


def ssd_scan(seqs, par, s0):
    xs, dt, bm, cm = seqs
    (a_gr,) = par
    causal = jnp.tril(jnp.ones((SSD_CHUNK, SSD_CHUNK), dtype=bool))[None, :, :, None, None]

    def step(s, inp):
        xc, dtc, bc, cc = inp
        a = jnp.cumsum(dtc * a_gr, axis=1)
        rel = jnp.where(causal, a[:, :, None] - a[:, None], -jnp.inf)
        cb = jnp.einsum('blgn,bsgn->blsg', cc, bc)
        y = jnp.einsum('blsgr,bsgrp->blgrp', cb[..., None] * jnp.exp(rel) * dtc[:, None], xc)
        y = y + jnp.einsum('blgn,bgrpn->blgrp', cc, s) * jnp.exp(a)[..., None]
        a_end = a[:, -1]
        s_new = jnp.exp(a_end)[..., None, None] * s + jnp.einsum(
            'blgr,blgn,blgrp->bgrpn', jnp.exp(a_end[:, None] - a) * dtc, bc, xc)
        return s_new, y

    s_fin, y = lax.scan(step, s0, tuple(_chunks(t, SSD_CHUNK) for t in (xs, dt, bm, cm)))
    return _unchunk(y), s_fin


def ssd_mixer(hx, hc, w_in, conv_w, conv_b, dt_bias, a_log, d_skip, norm_g, w_out, want_ctx):
    a_neg = -jnp.exp(a_log.astype(jnp.float32)).reshape(2, SSD_GROUPS, SSD_R)

    def project(h):
        bsz, t = h.shape[:2]
        z, xbc, dt = jnp.split(h @ w_in, [SSD_INNER, SSD_INNER + SSD_CONV_DIM], axis=-1)
        xbc = jax.nn.silu(dw_conv_centred(xbc, conv_w, conv_b)).astype(jnp.float32)
        xs, bm, cm = jnp.split(xbc, [SSD_INNER, SSD_INNER + SSD_GROUPS * SSD_STATE], axis=-1)
        xs = xs.reshape(bsz, t, SSD_GROUPS, SSD_R, SSD_HEAD_DIM)
        bm = bm.reshape(bsz, t, SSD_GROUPS, SSD_STATE)
        cm = cm.reshape(bsz, t, SSD_GROUPS, SSD_STATE)
        dt = jax.nn.softplus(dt.astype(jnp.float32).reshape(bsz, t, 2, SSD_GROUPS, SSD_R)
                             + dt_bias.astype(jnp.float32).reshape(2, SSD_GROUPS, SSD_R))
        return z, xs, bm, cm, dt

    zx, xx, bx, cx, dtx = project(hx)
    zc, xc, bc, cc, dtc = project(hc)
    s0 = jnp.zeros((hx.shape[0], SSD_GROUPS, SSD_R, SSD_HEAD_DIM, SSD_STATE), jnp.float32)
    y_x, y_c = bidir_scan(ssd_scan,
                          (xc, dtc[:, :, 0], bc, cc), (xc, dtc[:, :, 1], bc, cc),
                          (xx, dtx[:, :, 0], bx, cx), (xx, dtx[:, :, 1], bx, cx),
                          (a_neg[0],), (a_neg[1],), s0, want_ctx)
    d = d_skip.astype(jnp.float32).reshape(SSD_GROUPS, SSD_R, 1)

    def finish(y, xs, z):
        bsz, t = y.shape[:2]
        y = (y + d * xs).reshape(bsz, t, SSD_INNER).astype(z.dtype) * jax.nn.silu(z)
        y = rms_norm(y.reshape(bsz, t, SSD_GROUPS, SSD_INNER // SSD_GROUPS), norm_g.reshape(SSD_GROUPS, -1))
        return y.reshape(bsz, t, SSD_INNER) @ w_out

    return finish(y_x, xx, zx), (finish(y_c, xc, zc) if want_ctx else None)


def gla_scan(seqs, par, s0):
    q, k, lf, v = seqs
    causal = jnp.tril(jnp.ones((HGRN_CHUNK, HGRN_CHUNK), dtype=bool))[None, :, :, None, None]

    def step(s, inp):
        qc, kc, lfc, vc = inp
        cum = jnp.cumsum(lfc, axis=1)
        rel = jnp.where(causal, cum[:, :, None] - cum[:, None], -jnp.inf)
        att = jnp.einsum('blhd,bshd,blshd->bhls', qc, kc, jnp.exp(rel))
        o = jnp.einsum('bhls,bshv->blhv', att, vc) + jnp.einsum('blhd,bhdv->blhv', qc * jnp.exp(cum), s)
        c_end = cum[:, -1]
        s_new = jnp.exp(c_end)[..., None] * s + jnp.einsum(
            'blhd,blhv->bhdv', kc * jnp.exp(c_end[:, None] - cum), vc)
        return s_new, o

    s_fin, o = lax.scan(step, s0, tuple(_chunks(t, HGRN_CHUNK) for t in (q, k, lf, v)))
    return _unchunk(o), s_fin


def hgrn2_mixer(hx, hc, w_in, lower_bound, norm_g, w_out, want_ctx):
    lb = lower_bound.astype(jnp.float32).reshape(2, HGRN_HEADS, HGRN_DK)

    def project(h):
        bsz, t = h.shape[:2]
        q, f_fw, f_bw, i, g = jnp.split(h @ w_in, 5, axis=-1)
        heads = lambda a, d: a.astype(jnp.float32).reshape(bsz, t, HGRN_HEADS, d)
        q = jax.nn.silu(heads(q, HGRN_DK)) * HGRN_DK ** -0.5

        def gates(f, lb_d):
            f = heads(f, HGRN_DK)
            log_f = jnp.logaddexp(jnp.log(lb_d), jnp.log1p(-lb_d) + jax.nn.log_sigmoid(f))
            return (1.0 - lb_d) * jax.nn.sigmoid(-f), log_f

        k_fw, lf_fw = gates(f_fw, lb[0])
        k_bw, lf_bw = gates(f_bw, lb[1])
        return q, k_fw, lf_fw, k_bw, lf_bw, heads(i, HGRN_DV), g

    qx, kxf, lxf, kxb, lxb, vx, gx = project(hx)
    qc, kcf, lcf, kcb, lcb, vc, gc = project(hc)
    s0 = jnp.zeros((hx.shape[0], HGRN_HEADS, HGRN_DK, HGRN_DV), jnp.float32)
    o_x, o_c = bidir_scan(gla_scan, (qc, kcf, lcf, vc), (qc, kcb, lcb, vc),
                          (qx, kxf, lxf, vx), (qx, kxb, lxb, vx), (), (), s0, want_ctx)

    def finish(o, g):
        bsz, t = o.shape[:2]
        o = rms_norm(o, norm_g.reshape(HGRN_HEADS, HGRN_DV)).reshape(bsz, t, D_MODEL).astype(g.dtype)
        return (o * jax.nn.silu(g)) @ w_out

    return finish(o_x, gx), (finish(o_c, gc) if want_ctx else None)


def gqa_attend(q, k, v):
    s = jnp.einsum('bqgrd,bsgd->bgrqs', q, k).astype(jnp.float32) * ATTN_SCALE
    p = jax.nn.softmax(s, axis=-1).astype(v.dtype)
    return jnp.einsum('bgrqs,bsgd->bqgrd', p, v)


def attn_mixer(hx, hc, w_qkv, q_g, k_g, w_o, cos, sin, want_ctx):
    def project(h):
        bsz, t = h.shape[:2]
        q, k, v = jnp.split(h @ w_qkv, [N_HEADS * HEAD_DIM, (N_HEADS + N_KV) * HEAD_DIM], axis=-1)
        q = rms_norm(q.reshape(bsz, t, N_HEADS, HEAD_DIM), q_g)
        k = rms_norm(k.reshape(bsz, t, N_KV, HEAD_DIM), k_g)
        return q, k, v.reshape(bsz, t, N_KV, HEAD_DIM)

    qx, kx, vx = project(hx)
    qc, kc, vc = project(hc)
    qx = apply_rope(qx, cos, sin)
    kx = apply_rope(kx, cos, sin)
    k_all = jnp.concatenate([kx, kc], axis=1)
    v_all = jnp.concatenate([vx, vc], axis=1)
    bsz, t = hx.shape[:2]
    qb = jnp.moveaxis(qx.reshape(bsz, t // Q_BLOCK, Q_BLOCK, N_KV, GQA_R, HEAD_DIM), 1, 0)
    ox = lax.map(lambda qblk: gqa_attend(qblk, k_all, v_all), qb)
    ox = jnp.moveaxis(ox, 0, 1).reshape(bsz, t, N_HEADS * HEAD_DIM) @ w_o
    oc = None
    if want_ctx:
        n_ctx = hc.shape[1]
        oc = gqa_attend(qc.reshape(bsz, n_ctx, N_KV, GQA_R, HEAD_DIM), kc, vc)
        oc = oc.reshape(bsz, n_ctx, N_HEADS * HEAD_DIM) @ w_o
    return ox, oc


def mlstm_scan(seqs, par, s0):
    q, k, v, li, lf = seqs
    causal = jnp.tril(jnp.ones((MLSTM_CHUNK, MLSTM_CHUNK), dtype=bool))[None, :, :, None]

    def step(carry, inp):
        c, n, m = carry
        qc, kc, vc, lic, lfc = inp
        cum = jnp.cumsum(lfc, axis=1)
        dmat = jnp.where(causal, cum[:, :, None] - cum[:, None] + lic[:, None], -jnp.inf)
        inter = cum + m[:, None]
        m_t = jnp.maximum(inter, jnp.max(dmat, axis=2))
        w = jnp.exp(dmat - m_t[:, :, None])
        w_c = jnp.exp(inter - m_t)
        qk = jnp.einsum('blhd,bshd->blsh', qc, kc) * w
        num = jnp.einsum('blsh,bshe->blhe', qk, vc) + w_c[..., None] * jnp.einsum('blhd,bhde->blhe', qc, c)
        den = jnp.sum(qk, axis=2) + w_c * jnp.einsum('blhd,bhd->blh', qc, n)
        h = num / jnp.maximum(jnp.abs(den), jnp.exp(-m_t))[..., None]
        w_end = cum[:, -1:] - cum + lic
        m_new = jnp.maximum(cum[:, -1] + m, jnp.max(w_end, axis=1))
        e_end = jnp.exp(w_end - m_new[:, None])
        a_old = jnp.exp(cum[:, -1] + m - m_new)
        c_new = a_old[..., None, None] * c + jnp.einsum('blh,blhd,blhe->bhde', e_end, kc, vc)
        n_new = a_old[..., None] * n + jnp.einsum('blh,blhd->bhd', e_end, kc)
        return (c_new, n_new, m_new), h

    s_fin, h = lax.scan(step, s0, tuple(_chunks(t, MLSTM_CHUNK) for t in (q, k, v, li, lf)))
    return _unchunk(h), s_fin


def mlstm_mixer(hx, hc, w_up, conv_w, conv_b, w_q, w_k, w_v, w_gate, b_gate, skip, norm_g, w_down, want_ctx):
    def project(h):
        bsz, t = h.shape[:2]
        xm, z = jnp.split(h @ w_up, 2, axis=-1)
        xc = jax.nn.silu(dw_conv_centred(xm, conv_w, conv_b))
        heads = lambda a: a.reshape(bsz, t, MLSTM_HEADS, MLSTM_DH)
        q = jnp.einsum('bthd,hde->bthe', heads(xc), w_q)
        k = jnp.einsum('bthd,hde->bthe', heads(xc), w_k)
        v = jnp.einsum('bthd,hde->bthe', heads(xm), w_v)
        qkv = jnp.concatenate([q, k, v], axis=2).reshape(bsz, t, 3 * MLSTM_INNER)
        gt = (qkv @ w_gate + b_gate).astype(jnp.float32).reshape(bsz, t, 4, MLSTM_HEADS)
        q = q.astype(jnp.float32)
        k = k.astype(jnp.float32) * MLSTM_DH ** -0.5
        v = v.astype(jnp.float32)
        fw = (q, k, v, gt[:, :, 0], jax.nn.log_sigmoid(gt[:, :, 1]))
        bw = (q, k, v, gt[:, :, 2], jax.nn.log_sigmoid(gt[:, :, 3]))
        return fw, bw, xc, z

    fx, bx, xcx, zx = project(hx)
    fc, bc, xcc, zc = project(hc)
    bsz = hx.shape[0]
    s0 = (jnp.zeros((bsz, MLSTM_HEADS, MLSTM_DH, MLSTM_DH), jnp.float32),
          jnp.zeros((bsz, MLSTM_HEADS, MLSTM_DH), jnp.float32),
          jnp.full((bsz, MLSTM_HEADS), NEG_BIG, jnp.float32))
    h_x, h_c = bidir_scan(mlstm_scan, fc, bc, fx, bx, (), (), s0, want_ctx)

    def finish(h, xc, z):
        bsz_, t = h.shape[:2]
        hn = rms_norm(h, norm_g.reshape(MLSTM_HEADS, MLSTM_DH)).reshape(bsz_, t, MLSTM_INNER).astype(xc.dtype)
        return ((hn + skip * xc) * jax.nn.silu(z)) @ w_down

    return finish(h_x, xcx, zx), (finish(h_c, xcc, zc) if want_ctx else None)


def swiglu(h, w1, w3, w2):
    return (jax.nn.silu(h @ w1) * (h @ w3)) @ w2


def moe_swiglu(h, w_router, w1, w3, w2):
    logits = (h @ w_router).astype(jnp.float32)
    top_val, top_idx = lax.top_k(logits, TOP_K)
    top_w = jax.nn.softmax(top_val, axis=-1)
    combine = jnp.einsum('btk,btke->bte', top_w, jax.nn.one_hot(top_idx, N_EXPERTS, dtype=jnp.float32)).astype(h.dtype)
    out = jnp.zeros_like(h)
    for e in range(N_EXPERTS):
        out = out + combine[..., e:e + 1] * swiglu(h, w1[e], w3[e], w2[e])
    return out


def setup_inputs(seed: int = 0) -> dict:
    key = jax.random.key(seed)
    ks = iter(list(jax.random.split(key, 64)))
    D = D_MODEL

    def nrm(shape, scale):
        return jax.random.normal(next(ks), shape, jnp.float32) * scale

    def gain(shape):
        return 1.0 + nrm(shape, 0.02)

    inp = {}
    inp['x'] = nrm((BATCH, SEQ, D), 1.0)
    inp['c'] = nrm((BATCH, D), 1.0)
    inp['ctx'] = nrm((BATCH, CTX_LEN, D), 1.0)
    inp['c_ctx'] = nrm((D,), 1.0)
    inp['ada_w'] = nrm((DEPTH, D, 6 * D), 0.5 * D ** -0.5)
    inp['ada_b'] = nrm((DEPTH, 6 * D), 0.02)
    inp['norm_g'] = gain((DEPTH, 2, D))
    inp['ssd_w_in'] = nrm((N_SSD, D, SSD_IN_DIM), D ** -0.5)
    inp['ssd_conv_w'] = nrm((N_SSD, CONV_W, SSD_CONV_DIM), CONV_W ** -0.5)
    inp['ssd_conv_b'] = nrm((N_SSD, SSD_CONV_DIM), 0.02)
    dt0 = jnp.exp(jax.random.uniform(next(ks), (N_SSD, 2, SSD_HEADS), jnp.float32,
                                     minval=math.log(1e-3), maxval=math.log(1e-1)))
    inp['ssd_dt_bias'] = dt0 + jnp.log(-jnp.expm1(-dt0))
    inp['ssd_a_log'] = jnp.log(jax.random.uniform(next(ks), (N_SSD, 2, SSD_HEADS), jnp.float32, minval=1.0, maxval=16.0))
    inp['ssd_d'] = gain((N_SSD, SSD_HEADS))
    inp['ssd_norm_g'] = gain((N_SSD, SSD_INNER))
    inp['ssd_w_out'] = nrm((N_SSD, SSD_INNER, D), SSD_INNER ** -0.5)
    inp['hgrn_w_in'] = nrm((N_HGRN, D, 5 * D), D ** -0.5)
    inp['hgrn_lb'] = nrm((2, DEPTH, D), 0.5)
    inp['hgrn_norm_g'] = gain((N_HGRN, D))
    inp['hgrn_w_out'] = nrm((N_HGRN, D, D), D ** -0.5)
    inp['attn_w_qkv'] = nrm((N_ATTN, D, (N_HEADS + 2 * N_KV) * HEAD_DIM), D ** -0.5)
    inp['attn_q_g'] = gain((N_ATTN, HEAD_DIM))
    inp['attn_k_g'] = gain((N_ATTN, HEAD_DIM))
    inp['attn_w_o'] = nrm((N_ATTN, N_HEADS * HEAD_DIM, D), D ** -0.5)
    inp['mlstm_w_up'] = nrm((N_MLSTM, D, 2 * MLSTM_INNER), D ** -0.5)
    inp['mlstm_conv_w'] = nrm((N_MLSTM, CONV_W, MLSTM_INNER), CONV_W ** -0.5)
    inp['mlstm_conv_b'] = nrm((N_MLSTM, MLSTM_INNER), 0.02)
    inp['mlstm_w_q'] = nrm((N_MLSTM, MLSTM_HEADS, MLSTM_DH, MLSTM_DH), MLSTM_DH ** -0.5)
    inp['mlstm_w_k'] = nrm((N_MLSTM, MLSTM_HEADS, MLSTM_DH, MLSTM_DH), MLSTM_DH ** -0.5)
    inp['mlstm_w_v'] = nrm((N_MLSTM, MLSTM_HEADS, MLSTM_DH, MLSTM_DH), MLSTM_DH ** -0.5)
    inp['mlstm_w_gate'] = nrm((N_MLSTM, 3 * MLSTM_INNER, 4 * MLSTM_HEADS), 0.1 * (3 * MLSTM_INNER) ** -0.5)
    f_bias = jnp.linspace(3.0, 6.0, MLSTM_HEADS, dtype=jnp.float32)
    inp['mlstm_b_gate'] = jnp.concatenate([nrm((N_MLSTM, MLSTM_HEADS), 0.1), f_bias + nrm((N_MLSTM, MLSTM_HEADS), 0.1),
                                           nrm((N_MLSTM, MLSTM_HEADS), 0.1), f_bias + nrm((N_MLSTM, MLSTM_HEADS), 0.1)], axis=-1)
    inp['mlstm_skip'] = gain((N_MLSTM, MLSTM_INNER))
    inp['mlstm_norm_g'] = gain((N_MLSTM, MLSTM_INNER))
    inp['mlstm_w_down'] = nrm((N_MLSTM, MLSTM_INNER, D), MLSTM_INNER ** -0.5)
    inp['ffn_w1'] = nrm((N_DENSE, D, D_FF), D ** -0.5)
    inp['ffn_w3'] = nrm((N_DENSE, D, D_FF), D ** -0.5)
    inp['ffn_w2'] = nrm((N_DENSE, D_FF, D), D_FF ** -0.5)
    inp['moe_router'] = nrm((N_MOE, D, N_EXPERTS), D ** -0.5)
    inp['moe_w1'] = nrm((N_MOE, N_EXPERTS, D, D_FF), D ** -0.5)
    inp['moe_w3'] = nrm((N_MOE, N_EXPERTS, D, D_FF), D ** -0.5)
    inp['moe_w2'] = nrm((N_MOE, N_EXPERTS, D_FF, D), D_FF ** -0.5)
    return inp


def reference(x, c, ctx, c_ctx, ada_w, ada_b, norm_g,
              ssd_w_in, ssd_conv_w, ssd_conv_b, ssd_dt_bias, ssd_a_log, ssd_d, ssd_norm_g, ssd_w_out,
              hgrn_w_in, hgrn_lb, hgrn_norm_g, hgrn_w_out,
              attn_w_qkv, attn_q_g, attn_k_g, attn_w_o,
              mlstm_w_up, mlstm_conv_w, mlstm_conv_b, mlstm_w_q, mlstm_w_k, mlstm_w_v, mlstm_w_gate, mlstm_b_gate,
              mlstm_skip, mlstm_norm_g, mlstm_w_down,
              ffn_w1, ffn_w3, ffn_w2,
              moe_router, moe_w1, moe_w3, moe_w2):
    bsz, seq_len = x.shape[0], x.shape[1]
    cos, sin = axial_rope(seq_len)
    lb_all = jnp.cumsum(jax.nn.softmax(hgrn_lb.astype(jnp.float32), axis=1), axis=1)
    lb_all = lb_all - lb_all[:, :1]
    for i in range(DEPTH):
        want_ctx = i < DEPTH - 1
        mod_x = (jax.nn.silu(c) @ ada_w[i] + ada_b[i]).reshape(bsz, 6, 1, D_MODEL)
        mod_c = (jax.nn.silu(c_ctx) @ ada_w[i] + ada_b[i]).reshape(6, 1, D_MODEL)
        hx = modulate(rms_norm(x, norm_g[i, 0]), mod_x[:, 0], mod_x[:, 1])
        hc = modulate(rms_norm(ctx, norm_g[i, 0]), mod_c[0], mod_c[1])
        kind, j = i % N_MIXERS, i // N_MIXERS
        if kind == 0:
            yx, yc = ssd_mixer(hx, hc, ssd_w_in[j], ssd_conv_w[j], ssd_conv_b[j], ssd_dt_bias[j], ssd_a_log[j],
                               ssd_d[j], ssd_norm_g[j], ssd_w_out[j], want_ctx)
        elif kind == 1:
            yx, yc = hgrn2_mixer(hx, hc, hgrn_w_in[j], lb_all[:, i], hgrn_norm_g[j], hgrn_w_out[j], want_ctx)
        elif kind == 2:
            yx, yc = attn_mixer(hx, hc, attn_w_qkv[j], attn_q_g[j], attn_k_g[j], attn_w_o[j], cos, sin, want_ctx)
        else:
            yx, yc = mlstm_mixer(hx, hc, mlstm_w_up[j], mlstm_conv_w[j], mlstm_conv_b[j], mlstm_w_q[j], mlstm_w_k[j],
                                 mlstm_w_v[j], mlstm_w_gate[j], mlstm_b_gate[j], mlstm_skip[j], mlstm_norm_g[j],
                                 mlstm_w_down[j], want_ctx)
        x = x + mod_x[:, 2] * yx
        hx = modulate(rms_norm(x, norm_g[i, 1]), mod_x[:, 3], mod_x[:, 4])
        if want_ctx:
            ctx = ctx + mod_c[2] * yc
            hc = modulate(rms_norm(ctx, norm_g[i, 1]), mod_c[3], mod_c[4])
        if i % 2 == 0:
            ffn = functools.partial(swiglu, w1=ffn_w1[i // 2], w3=ffn_w3[i // 2], w2=ffn_w2[i // 2])
        else:
            ffn = functools.partial(moe_swiglu, w_router=moe_router[i // 2], w1=moe_w1[i // 2],
                                    w3=moe_w3[i // 2], w2=moe_w2[i // 2])
        x = x + mod_x[:, 5] * ffn(hx)
        if want_ctx:
            ctx = ctx + mod_c[5] * ffn(hc)
    return x
```

```python
import numpy as np
import concourse.bass as bass
import concourse.mybir as mybir

F32 = mybir.dt.float32
BF16 = mybir.dt.bfloat16
I32 = mybir.dt.int32
AF = mybir.ActivationFunctionType
OP = mybir.AluOpType
AX = mybir.AxisListType


class V:
    __slots__ = ("t", "ap")

    def __init__(self, t, ap):
        self.t = t
        self.ap = ap

    def __getitem__(self, idx):
        return V(self.t, self.ap[idx])

    def re(self, pat, **kw):
        return V(self.t, self.ap.rearrange(pat, **kw))

    def bc(self, shape):
        return V(self.t, self.ap.to_broadcast(shape))

    def bitcast(self, dt):
        return V(self.t, self.ap.bitcast(dt))

    def unsq(self, d):
        return V(self.t, self.ap.unsqueeze(d))


class Tile:
    def __init__(self, h, name, const=False):
        self.h = h
        self.name = name
        self.last_w = None
        self.readers = {}
        self.const = const

    def __getitem__(self, idx):
        return V(self, self.h[idx])

    @property
    def v(self):
        return V(self, self.h.ap())


class P:
    EPOCH = 16000
    NDMA = 24

    def __init__(self, nc):
        self.nc = nc
        self.eng = {"pe": nc.tensor, "act": nc.scalar, "dve": nc.vector, "pool": nc.gpsimd, "sp": nc.sync}
        self.cnt = {e: 0 for e in self.eng}
        self.esems = {e: [] for e in self.eng}
        self.waited = {e: {} for e in self.eng}
        self.dma_sems = []
        self.dma_cnt = []
        self.dma_last = []
        self.dma_rr = 0
        self.nsem = 0
        self.ninst = 0
        self.nwait = 0
        self.uid = 0
        self.psum_tiles = []

    def name(self, n):
        self.uid += 1
        return f"{n}_{self.uid}"

    def sb(self, name, shape, dt=F32):
        return Tile(self.nc.alloc_sbuf_tensor(self.name(name), list(shape), dt), name)

    def ps(self, name, shape, dt=F32):
        t = Tile(self.nc.alloc_psum_tensor(self.name(name), list(shape), dt), name)
        t.psum = True
        return t

    def dram(self, name, shape, dt=F32, kind="Internal"):
        h = self.nc.dram_tensor(name, list(shape), dt, kind=kind)
        return Tile(h, name, const=(kind == "ExternalInput"))

    def sem(self, name):
        self.nsem += 1
        return self.nc.alloc_semaphore(self.name(name))

    def _next_token(self, e):
        n = self.cnt[e]
        ep, val = divmod(n, self.EPOCH)
        while len(self.esems[e]) <= ep:
            self.esems[e].append(self.sem(f"s_{e}"))
        self.cnt[e] = n + 1
        return (self.esems[e][ep], val + 1, e, (e, ep))

    def _dma_token(self, e):
        if len(self.dma_sems) < self.NDMA:
            self.dma_sems.append(self.sem("s_dma"))
            self.dma_cnt.append(0)
            self.dma_last.append(None)
        k = self.dma_rr
        self.dma_rr = (self.dma_rr + 1) % self.NDMA
        if k >= len(self.dma_sems):
            k = len(self.dma_sems) - 1
        if self.dma_last[k] is not None:
            self._wait(e, self.dma_last[k])
        self.dma_cnt[k] += 1
        tok = (self.dma_sems[k], 16 * self.dma_cnt[k], "dma", ("dma", k))
        self.dma_last[k] = tok
        return tok

    def _wait(self, e, tok):
        sem, val, te, key = tok
        if te == e and e == "pe":
            return
        w = self.waited[e]
        if w.get(key, 0) >= val:
            return
        w[key] = val
        self.eng[e].wait_ge(sem, val)
        self.nwait += 1

    def emit(self, e, reads, writes, fn, dma=False):
        if e != "pe" and any(getattr(v.t, "psum", False) for v in reads):
            writes = list(writes) + [v for v in reads if getattr(v.t, "psum", False)]
            reads = [v for v in reads if not getattr(v.t, "psum", False)]
        for v in reads:
            t = v.t
            if t.last_w is not None:
                self._wait(e, t.last_w)
        for v in writes:
            t = v.t
            if t.last_w is not None:
                self._wait(e, t.last_w)
            for tok in t.readers.values():
                self._wait(e, tok)
        tok = self._dma_token(e) if dma else self._next_token(e)
        inst = fn()
        inst.then_inc(tok[0], 16 if dma else 1)
        self.ninst += 1
        for v in reads:
            t = v.t
            if not t.const:
                t.readers[tok[3]] = tok
        for v in writes:
            t = v.t
            t.last_w = tok
            t.readers = {}
        return tok

    def mm(self, out, lhsT, rhs, start=True, stop=True, **kw):
        return self.emit("pe", [lhsT, rhs], [out],
                         lambda: self.nc.tensor.matmul(out.ap, lhsT.ap, rhs.ap, start=start, stop=stop, **kw))

    def tr(self, out, in_, ident):
        return self.emit("pe", [in_, ident], [out],
                         lambda: self.nc.tensor.transpose(out.ap, in_.ap, ident.ap))

    def act(self, out, in_, func, bias=None, scale=None, accum=None):
        reads = [in_]
        kw = {}
        if bias is not None:
            if isinstance(bias, V):
                reads.append(bias); kw["bias"] = bias.ap
            else:
                kw["bias"] = bias
        if scale is not None:
            if isinstance(scale, V):
                reads.append(scale); kw["scale"] = scale.ap
            else:
                kw["scale"] = scale
        writes = [out]
        if accum is not None:
            writes.append(accum); kw["accum_out"] = accum.ap
        return self.emit("act", reads, writes, lambda: self.nc.scalar.activation(out.ap, in_.ap, func, **kw))

    def tt(self, out, a, b, op, e="dve"):
        return self.emit(e, [a, b], [out], lambda: self.eng[e].tensor_tensor(out.ap, a.ap, b.ap, op))

    def ts(self, out, a, s1, s2=None, op0=OP.mult, op1=None, e="dve", accum=None):
        reads = [a]
        a1 = s1.ap if isinstance(s1, V) else s1
        a2 = s2.ap if isinstance(s2, V) else s2
        if isinstance(s1, V): reads.append(s1)
        if isinstance(s2, V): reads.append(s2)
        kw = {}
        writes = [out]
        if e == "pool" and op1 is None and s2 is None:
            if op0 == OP.mult:
                a2, op1 = 0.0, OP.add
            elif op0 == OP.add:
                a2, op1 = 1.0, OP.mult
        if op1 is not None: kw["op1"] = op1
        if accum is not None:
            kw["accum_out"] = accum.ap; writes.append(accum)
        return self.emit(e, reads, writes, lambda: self.eng[e].tensor_scalar(out.ap, a.ap, a1, a2, op0, **kw))

    def stt(self, out, a, s, b, op0, op1, accum=None):
        reads = [a, b]
        a1 = s.ap if isinstance(s, V) else s
        if isinstance(s, V): reads.append(s)
        kw = {}
        writes = [out]
        if accum is not None:
            kw["accum_out"] = accum.ap; writes.append(accum)
        return self.emit("dve", reads, writes,
                         lambda: self.nc.vector.scalar_tensor_tensor(out.ap, a.ap, a1, b.ap, op0, op1, **kw))

    def copy(self, out, in_, e="dve"):
        if e == "act":
            return self.emit("act", [in_], [out], lambda: self.nc.scalar.copy(out.ap, in_.ap))
        if e == "pool":
            return self.emit(e, [in_], [out],
                             lambda: self.eng[e].tensor_scalar(out.ap, in_.ap, 1.0, 0.0, OP.mult, op1=OP.add))
        return self.emit(e, [in_], [out], lambda: self.eng[e].tensor_copy(out.ap, in_.ap))

    def memset(self, out, val, e="dve"):
        return self.emit(e, [], [out], lambda: self.eng[e].memset(out.ap, val))

    def reduce(self, out, in_, op=OP.add, axis=AX.X, **kw):
        return self.emit("dve", [in_], [out], lambda: self.nc.vector.tensor_reduce(out.ap, in_.ap, axis, op, **kw))

    def recip(self, out, in_):
        return self.emit("dve", [in_], [out], lambda: self.nc.vector.reciprocal(out.ap, in_.ap))

    def scan(self, out, d0, d1, initial, op0, op1):
        reads = [d0, d1]
        ini = initial.ap if isinstance(initial, V) else initial
        if isinstance(initial, V): reads.append(initial)
        return self.emit("dve", reads, [out],
                         lambda: self.nc.vector.tensor_tensor_scan(out.ap, d0.ap, d1.ap, ini, op0, op1))

    def iota(self, out, pattern, base=0, cm=0):
        return self.emit("pool", [], [out], lambda: self.nc.gpsimd.iota(
            out.ap, pattern, base=base, channel_multiplier=cm, allow_small_or_imprecise_dtypes=True))

    def aselect(self, out, in_, pattern, cmp, fill, base=0, cm=0):
        return self.emit("pool", [in_], [out], lambda: self.nc.gpsimd.affine_select(
            out.ap, in_.ap, pattern, cmp, fill, base=base, channel_multiplier=cm))

    def dma(self, out, in_, q="sp", **kw):
        return self.emit(q, [in_], [out], lambda: self.eng[q].dma_start(out.ap, in_.ap, **kw), dma=True)

    def finish(self):
        toks = []
        for e in self.eng:
            if self.cnt[e] > 0:
                n = self.cnt[e] - 1
                ep, val = divmod(n, self.EPOCH)
                toks.append((self.esems[e][ep], val + 1, e, (e, ep)))
        toks += [t for t in self.dma_last if t is not None]
        for tok in toks:
            sem, val, te, key = tok
            if self.waited["sp"].get(key, 0) < val:
                self.nc.sync.wait_ge(sem, val)


import contextlib

D = 1024
KC = 8
NCTX = 256
EPS = 1e-6


class Phase:
    def __init__(self, p):
        import sys
        self.p = p
        self.st = contextlib.ExitStack()
        if getattr(p, "scopes", False):
            f = sys._getframe(1)
            p.uid += 1
            self.st.enter_context(p.nc.named_scope(f"{f.f_code.co_name}_{f.f_lineno}_{p.uid}"))

    def sb(self, name, shape, dt=F32):
        h = self.st.enter_context(self.p.nc.sbuf_tensor(self.p.name(name), list(shape), dt))
        return Tile(h, name)

    def rot(self, name, shape, dt=F32, n=2):
        return Rot([self.sb(f"{name}{i}", shape, dt) for i in range(n)])

    def close(self):
        self.p.barrier()
        self.st.close()


class Rot:
    def __init__(self, tiles):
        self.tiles = tiles
        self.i = 0

    def next(self):
        t = self.tiles[self.i % len(self.tiles)]
        self.i += 1
        return t


def _barrier(self):
    toks = []
    for e in self.eng:
        if self.cnt[e] > 0:
            n = self.cnt[e] - 1
            ep, val = divmod(n, self.EPOCH)
            toks.append((self.esems[e][ep], val + 1, e, (e, ep)))
    toks += [t for t in self.dma_last if t is not None]
    for e in self.eng:
        for tok in toks:
            if tok[2] != e:
                self._wait(e, tok)


P.barrier = _barrier


def _sub(self, key):
    if not hasattr(self, "_subs"):
        self._subs = {}
    if key not in self._subs:
        self._subs[key] = Tile(self.h, f"{self.name}[{key}]", const=self.const)
    return self._subs[key]


Tile.sub = _sub


class Ctx:
    pass


def setup_consts(K):
    p = K.p
    K.ident_f = p.sb("ident_f", [128, 128], F32)
    K.ident_b = p.sb("ident_b", [128, 128], BF16)
    K.ones_f = p.sb("ones_f", [128, 128], F32)
    K.ones_b = p.sb("ones_b", [128, 128], BF16)
    K.mask_le = p.sb("mask_le", [128, 128], F32)
    K.mask_ge = p.sb("mask_ge", [128, 128], F32)
    p.memset(K.ones_f.v, 1.0)
    p.memset(K.ones_b.v, 1.0)
    p.aselect(K.ident_f.v, K.ones_f.v, [[1, 128]], OP.is_equal, 0.0, base=0, cm=-1)
    p.copy(K.ident_b.v, K.ident_f.v)
    p.aselect(K.mask_le.v, K.ones_f.v, [[1, 128]], OP.is_ge, 0.0, base=0, cm=-1)
    p.aselect(K.mask_ge.v, K.ones_f.v, [[-1, 128]], OP.is_ge, 0.0, base=0, cm=1)
    K.mask_le_b = p.sb("mask_le_b", [128, 128], BF16)
    K.mask_ge_b = p.sb("mask_ge_b", [128, 128], BF16)
    p.copy(K.mask_le_b.v, K.mask_le.v)
    p.copy(K.mask_ge_b.v, K.mask_ge.v)
    for t in (K.ident_f, K.ident_b, K.ones_f, K.ones_b, K.mask_le, K.mask_ge, K.mask_le_b, K.mask_ge_b):
        t.const = True
    K.ps = [p.ps(f"ps{i}", [128, 512], F32) for i in range(8)]
    K.eps_t = p.sb("eps_t", [128, 1], F32)
    p.memset(K.eps_t.v, EPS)
    K.eps_t.const = True
    K.stage = p.sb("stage", [128, 128], F32)
    K.scT = p.sb("scT", [128, KC, 2], F32)
    K.modTs = [p.sb(f"modT{i}", [128, 48, 2], F32) for i in range(4)]
    K.gms = [p.sb(f"gm{i}", [128, 2, KC, 2], F32) for i in range(4)]
    K.have_scT = False


def load_vec_fm(K, owner, dram_v, n, name):
    p = K.p
    out = owner.sb(name, [128, n], F32)
    done = 0
    while done < n:
        m = min(128, n - done)
        st = K.stage
        p.dma(st[0:m, :], dram_v[done:done + m, :])
        ps = K.ps[7]
        p.tr(ps[:, 0:m], st[0:m, :], K.ident_f[0:m, 0:m])
        p.copy(out[:, done:done + m], ps[:, 0:m])
        done += m
    return out


_NOCTX = [False]


def sub_blocks(NT):
    if _NOCTX[0]:
        subs = []
        t = 0
    else:
        subs = [(0, NCTX, 1)]
        t = NCTX
    while t < NT:
        n = min(512, NT - t)
        subs.append((t, n, 0))
        t += n
    return subs


def phase_load_input(K):
    p = K.p
    ph = Phase(p)
    xt_r = ph.rot("li_x", [128, 4, D], F32)
    xs_r = ph.rot("li_xs", [128, KC, 512], F32)
    xTv = K.xT.v.re("(c q) t -> q c t", q=128)
    for (t0, n, _) in sub_blocks(K.NT):
        nj = n // 128
        xt = xt_r.next()
        p.dma(xt[:, 0:nj, :], K.xin[t0:t0 + n, :].re("(j q) f -> q j f", q=128))
        xs = xs_r.next()
        for c in range(KC):
            ps = K.ps[c % 4]
            for j in range(nj):
                p.tr(ps[:, j * 128:(j + 1) * 128], xt[:, j, c * 128:(c + 1) * 128], K.ident_f.v)
            p.copy(xs[:, c, 0:n], ps[:, 0:n], e=("act" if c % 2 else "dve"))
        p.dma(V(K.xT.sub(t0), xTv.ap[:, :, t0:t0 + n]), xs[:, :, 0:n])
    ph.close()


def phase_mod(K, li):
    p = K.p
    ph = Phase(p)
    if not K.have_scT:
        K.have_scT = True
        cr = ph.sb("c_row", [2, D], F32)
        p.dma(cr.v, K.c_row.v)
        ps = K.ps[7]
        for c in range(KC):
            p.tr(ps[:, 2 * c:2 * c + 2], cr[0:2, c * 128:(c + 1) * 128], K.ident_f[0:2, 0:2])
        p.act(K.scT.v.re("q c r -> q (c r)"), ps[:, 0:16], AF.Silu)
    ab = load_vec_fm(K, ph, K.ada_b[li].re("(n q) -> n q", q=128), 48, f"ada_b{li}")
    modT = K.modTs[li]
    w_r = ph.rot("ada_w", [128, KC, 1024], F32)
    for j in range(6):
        w = w_r.next()
        for kc in range(KC):
            p.dma(w[:, kc, :], K.ada_w[li, kc * 128:(kc + 1) * 128, j * 1024:(j + 1) * 1024])
        ps = K.ps[j % 2]
        for c in range(KC):
            for kc in range(KC):
                p.mm(ps[:, 2 * c:2 * c + 2], w[:, kc, c * 128:(c + 1) * 128], K.scT[:, kc, :],
                     start=(kc == 0), stop=(kc == KC - 1))
        p.tt(modT[:, j * 8:(j + 1) * 8, :], ps[:, 0:16].re("q (c r) -> q c r", r=2),
             ab[:, j * 8:(j + 1) * 8].unsq(2).bc([128, 8, 2]), OP.add)
    ng = load_vec_fm(K, ph, K.norm_g[li].re("a (n q) -> (a n) q", q=128), 16, f"ng{li}")
    gm = K.gms[li]
    for a, j in ((0, 1), (1, 4)):
        p.ts(gm[:, a, :, :], modT[:, j * 8:(j + 1) * 8, :], 1.0, None, op0=OP.add)
        p.tt(gm[:, a, :, :], gm[:, a, :, :], ng[:, a * 8:(a + 1) * 8].unsq(2).bc([128, 8, 2]), OP.mult)
    ph.close()
    modT.const = True
    gm.const = True
    K.modT = modT
    K.gm = gm


def norm_block(K, ph, bufs, t0, n, which, isctx, want_f32=False):
    p = K.p
    xt = bufs["xt"].next()
    p.dma(xt[:, :, 0:n], V(K.xT.sub(t0), K.xT.v.re("(c q) t -> q c t", q=128).ap[:, :, t0:t0 + n]))
    sq = bufs["sq"].next()
    p.act(sq[:, :, 0:n], xt[:, :, 0:n], AF.Square)
    ps = K.ps[6]
    for c in range(KC):
        p.mm(ps[:, 0:n], K.ones_b.v, sq[:, c, 0:n], start=(c == 0), stop=(c == KC - 1))
    rs = bufs["rs"].next()
    p.act(rs[:, 0:n], ps[:, 0:n], AF.Sqrt, scale=1.0 / D, bias=K.eps_t[:, 0:1])
    p.recip(rs[:, 0:n], rs[:, 0:n])
    hT = bufs["hT"].next()
    shj = 0 if which == 0 else 3
    hf = bufs["hf"].next() if want_f32 else None
    for c in range(KC):
        tmp = bufs["tmp"].next()
        p.stt(tmp[:, 0:n], xt[:, c, 0:n], K.gm[:, which, c, isctx:isctx + 1], rs[:, 0:n], OP.mult, OP.mult)
        p.act(hT[:, c, 0:n], tmp[:, 0:n], AF.Identity, bias=K.modT[:, shj * 8 + c, isctx:isctx + 1])
        if want_f32:
            p.ts(hf[:, c, 0:n], tmp[:, 0:n], K.modT[:, shj * 8 + c, isctx:isctx + 1], None, op0=OP.add, e="pool")
    return xt, hT, hf


def norm_bufs(ph, want_f32=False, nbuf=2):
    b = {
        "xt": ph.rot("nb_xt", [128, KC, 512], F32, nbuf),
        "sq": ph.rot("nb_sq", [128, KC, 512], BF16, 1),
        "rs": ph.rot("nb_rs", [128, 512], F32, 2),
        "hT": ph.rot("nb_hT", [128, KC, 512], BF16, nbuf),
        "tmp": ph.rot("nb_tmp", [128, 512], F32, 3),
    }
    if want_f32:
        b["hf"] = ph.rot("nb_hf", [128, KC, 512], F32, 1)
    return b


def load_w_bf16(K, dst, src_rows_fn, kcs, ncols, q="pool"):
    p = K.p
    for kc in range(kcs):
        p.dma(dst[:, kc, 0:ncols], src_rows_fn(kc), q=q, max_dma_last_dim=2048 * 4)


def phase_proj(K, which, wfn, ncols, sink, extra=None, want_f32=False, setup=None):
    p = K.p
    ph = Phase(p)
    W = ph.sb("pw", [128, KC, ncols], BF16)
    load_w_bf16(K, W, wfn, KC, ncols)
    bufs = norm_bufs(ph, want_f32, nbuf=2)
    st = {"ph": ph, "W": W}
    if setup is not None:
        setup(st)
    pi = 0
    for (t0, n, isctx) in sub_blocks(K.NT):
        xt, hT, hf = norm_block(K, ph, bufs, t0, n, which, isctx, want_f32)
        for j in range((ncols + 127) // 128):
            m = min(128, ncols - j * 128)
            if sink is None or m < 128:
                continue
            ps = K.ps[pi % 4]
            pi += 1
            for kc in range(KC):
                p.mm(ps[0:m, 0:n], W[:, kc, j * 128:j * 128 + m], hT[:, kc, 0:n], start=(kc == 0), stop=(kc == KC - 1))
            sink(st, j, ps, t0, n, isctx)
        if extra is not None:
            extra(st, hT, hf, t0, n, isctx)
    ph.close()


def phase_outproj(K, srcT, kin_c, wfn, gate_j, row_scale=None):
    p = K.p
    ph = Phase(p)
    W = ph.sb("ow", [128, kin_c, D], BF16)
    load_w_bf16(K, W, wfn, kin_c, D)
    if row_scale is not None:
        for kc in range(kin_c):
            p.ts(W[:, kc, :], W[:, kc, :], row_scale[:, kc:kc + 1], None, op0=OP.mult, e="pool")
    src_r = ph.rot("op_src", [128, kin_c, 512], BF16, 2)
    xt_r = ph.rot("op_xt", [128, KC, 512], F32, 2)
    xTv = K.xT.v.re("(c q) t -> q c t", q=128)
    sv = srcT.v.re("(c q) t -> q c t", q=128)
    pi = 0
    for (t0, n, isctx) in sub_blocks(K.NT):
        if isctx and not K.want_ctx:
            continue
        src = src_r.next()
        p.dma(src[:, :, 0:n], V(srcT.sub(t0), sv.ap[:, :, t0:t0 + n]))
        xt = xt_r.next()
        p.dma(xt[:, :, 0:n], V(K.xT.sub(t0), xTv.ap[:, :, t0:t0 + n]))
        for o in range(KC):
            ps = K.ps[pi % 4]
            pi += 1
            for kc in range(kin_c):
                p.mm(ps[:, 0:n], W[:, kc, o * 128:(o + 1) * 128], src[:, kc, 0:n], start=(kc == 0), stop=(kc == kin_c - 1))
            p.stt(xt[:, o, 0:n], ps[:, 0:n], K.modT[:, gate_j * 8 + o, isctx:isctx + 1], xt[:, o, 0:n], OP.mult, OP.add)
        p.dma(V(K.xT.sub(t0), xTv.ap[:, :, t0:t0 + n]), xt[:, :, 0:n])
    ph.close()


FF = 3584
FFC = 28


def ffn_blocks(K, BLK):
    subs = [s for s in sub_blocks(K.NT) if (K.want_ctx or not s[2])]
    blocks = []
    cur = []
    tot = 0
    for s in subs:
        if tot + s[1] > BLK:
            blocks.append(cur)
            cur, tot = [], 0
        cur.append(s)
        tot += s[1]
    if cur:
        blocks.append(cur)
    return blocks


def phase_ffn(K, experts, w1fn, w3fn, w2fn, router=None):
    p = K.p
    ph = Phase(p)
    moe = router is not None
    BLK = K.FFN_BLK
    NS = getattr(K, "FFN_NS", 4)
    FS = FF // NS
    FK = FS // 128
    hTb = ph.sb("f_hT", [128, KC, BLK], BF16)
    acc = ph.sb("f_acc", [128, KC, BLK], F32)
    xTv = K.xT.v.re("(c q) t -> q c t", q=128)
    if moe:
        cbT = ph.sb("f_cbT", [8, BLK], F32)
        cbE = ph.sb("f_cbE", [128, BLK], F32)
        wr = ph.sb("f_wr", [128, KC, 8], F32)
        p.dma(wr.v, router.re("(c q) e -> q c e", q=128))
        sel = ph.sb("f_sel", [8, 8, 128], F32)
        p.memset(sel.v, 1.0)
        p.aselect(sel.v, sel.v, [[1, 8], [0, 128]], OP.is_equal, 0.0, base=0, cm=-1)
        lg = ph.sb("f_lg", [128, 8], F32)
        m8 = ph.sb("f_m8", [128, 8], F32)
        w12 = ph.sb("f_w12", [128, 4], F32)
        cbm = ph.sb("f_cbm", [128, 8], F32)
        cb2 = ph.sb("f_cb2", [128, 8], F32)
    pi = 0
    for blk in ffn_blocks(K, BLK):
        b0 = blk[0][0]
        ph1 = Phase(p)
        bufs = norm_bufs(ph1, want_f32=moe, nbuf=(1 if moe else 2))
        for (t0, n, isctx) in blk:
            o0 = t0 - b0
            xt, hT, hf = norm_block(K, ph1, bufs, t0, n, 1, isctx, want_f32=moe)
            p.copy(hTb[:, :, o0:o0 + n], hT[:, :, 0:n], e="act")
            if moe:
                for jt in range(n // 128):
                    psl = K.ps[7]
                    for kc in range(KC):
                        p.mm(psl[:, 0:8], hf[:, kc, jt * 128:(jt + 1) * 128], wr[:, kc, :], start=(kc == 0), stop=(kc == KC - 1))
                    p.copy(lg.v, psl[:, 0:8])
                    p.emit("dve", [lg.v], [m8.v], lambda: p.nc.vector.max(m8.v.ap, lg.v.ap))
                    p.tt(w12[:, 0:1], m8[:, 0:1], m8[:, 1:2], OP.subtract)
                    p.act(w12[:, 1:2], w12[:, 0:1], AF.Sigmoid)
                    p.ts(w12[:, 2:3], w12[:, 1:2], -1.0, 1.0, op0=OP.mult, op1=OP.add)
                    p.ts(cbm.v, lg.v, m8[:, 0:1], w12[:, 1:2], op0=OP.is_equal, op1=OP.mult)
                    p.ts(cb2.v, lg.v, m8[:, 1:2], w12[:, 2:3], op0=OP.is_equal, op1=OP.mult)
                    p.tt(cbm.v, cbm.v, cb2.v, OP.add)
                    pst = K.ps[6]
                    p.tr(pst[0:8, 0:128], cbm.v, K.ident_f.v)
                    p.copy(cbT[:, o0 + jt * 128:o0 + (jt + 1) * 128], pst[0:8, 0:128])
        ph1.close()
        ph2 = Phase(p)
        g = ph2.sb("f_g", [128, FK, BLK], BF16)
        w1_r = ph2.rot("f_w1", [128, KC, FS], BF16, 2)
        w3_r = ph2.rot("f_w3", [128, KC, FS], BF16, 2)
        w2_r = ph2.rot("f_w2", [128, FK, D], BF16, 2)
        su_r = ph2.rot("f_su", [128, 512], F32, 2)
        tm_r = ph2.rot("f_tm", [128, 512], F32, 2)
        xt_r = ph2.rot("f_xt", [128, 512], F32, 2)
        first = True
        for e in experts:
            if moe:
                for (t0, n, isctx) in blk:
                    o0 = t0 - b0
                    pc = K.ps[6]
                    p.mm(pc[:, 0:n], sel[:, e, :], cbT[:, o0:o0 + n])
                    p.copy(cbE[:, o0:o0 + n], pc[:, 0:n], e="act")
            for s_ in range(NS):
                w1 = w1_r.next(); w3 = w3_r.next(); w2 = w2_r.next()
                p.dma(w1.v, w1fn(e, s_ * FS, FS), q="pool", max_dma_last_dim=2048 * 4)
                p.dma(w3.v, w3fn(e, s_ * FS, FS), q="pool", max_dma_last_dim=2048 * 4)
                p.dma(w2.v, w2fn(e, s_ * FK, FK), q="pool", max_dma_last_dim=2048 * 4)
                for j in range(FK):
                    for (t0, n, isctx) in blk:
                        o0 = t0 - b0
                        pu = K.ps[pi % 2]
                        pv = K.ps[2 + pi % 2]
                        pi += 1
                        for kc in range(KC):
                            p.mm(pu[:, 0:n], w1[:, kc, j * 128:(j + 1) * 128], hTb[:, kc, o0:o0 + n], start=(kc == 0), stop=(kc == KC - 1))
                        for kc in range(KC):
                            p.mm(pv[:, 0:n], w3[:, kc, j * 128:(j + 1) * 128], hTb[:, kc, o0:o0 + n], start=(kc == 0), stop=(kc == KC - 1))
                        su = su_r.next()
                        p.act(su[:, 0:n], pu[:, 0:n], AF.Silu)
                        p.tt(g[:, j, o0:o0 + n], su[:, 0:n], pv[:, 0:n], OP.mult)
                for o in range(KC):
                    for (t0, n, isctx) in blk:
                        o0 = t0 - b0
                        po = K.ps[4 + pi % 2]
                        pi += 1
                        for kc in range(FK):
                            p.mm(po[:, 0:n], w2[:, kc, o * 128:(o + 1) * 128], g[:, kc, o0:o0 + n], start=(kc == 0), stop=(kc == FK - 1))
                        av = acc[:, o, o0:o0 + n]
                        if not moe:
                            if first:
                                p.copy(av, po[:, 0:n])
                            else:
                                p.tt(av, av, po[:, 0:n], OP.add)
                        elif first:
                            p.tt(av, po[:, 0:n], cbE[:, o0:o0 + n], OP.mult)
                        else:
                            tm = tm_r.next()
                            p.tt(tm[:, 0:n], po[:, 0:n], cbE[:, o0:o0 + n], OP.mult)
                            p.tt(av, av, tm[:, 0:n], OP.add)
                first = False
        for (t0, n, isctx) in blk:
            o0 = t0 - b0
            for o in range(KC):
                xt = xt_r.next()
                xv = V(K.xT.sub((t0, o)), K.xT.v.ap[o * 128:(o + 1) * 128, t0:t0 + n])
                p.dma(xt[:, 0:n], xv)
                p.stt(xt[:, 0:n], acc[:, o, o0:o0 + n], K.modT[:, 5 * 8 + o, isctx:isctx + 1], xt[:, 0:n], OP.mult, OP.add)
                p.dma(xv, xt[:, 0:n])
        ph2.close()
    ph.close()


def phase_store_output(K):
    p = K.p
    ph = Phase(p)
    xs_r = ph.rot("so_xs", [128, KC, 512], F32, 2)
    xo_r = ph.rot("so_xo", [128, 4, D], F32, 2)
    xTv = K.xT.v.re("(c q) t -> q c t", q=128)
    pi = 0
    for (t0, n, isctx) in sub_blocks(K.NT):
        if isctx and not K.debug_full_out:
            continue
        off = 0 if (K.debug_full_out or _NOCTX[0]) else NCTX
        xs = xs_r.next()
        p.dma(xs[:, :, 0:n], V(K.xT.sub(t0), xTv.ap[:, :, t0:t0 + n]))
        xo = xo_r.next()
        for j in range(n // 128):
            for half in range(2):
                ps = K.ps[pi % 4]
                pi += 1
                for c4 in range(4):
                    c = half * 4 + c4
                    p.tr(ps[:, c4 * 128:(c4 + 1) * 128], xs[:, c, j * 128:(j + 1) * 128], K.ident_f.v)
                p.copy(xo[:, j, half * 512:(half + 1) * 512], ps.v, e=("act" if half else "dve"))
        p.dma(K.xout[t0 - off:t0 - off + n, :].re("(j q) f -> q j f", q=128), xo[:, 0:n // 128, :])
    ph.close()


def phase_select_half(K):
    p = K.p
    ph = Phase(p)
    H = K.TL // 2
    xH = p.dram("xH", [D, H], F32)
    sv = ph.sb("sel_sv", [128, 2], F32)
    p.dma(sv.v, K.selv.v)
    a_r = ph.rot("sel_a", [128, KC, 512], F32, 2)
    b_r = ph.rot("sel_b", [128, KC, 512], F32, 2)
    xTv = K.xT.v.re("(c q) t -> q c t", q=128)
    xHv = xH.v.re("(c q) t -> q c t", q=128)
    for t0 in range(0, H, 512):
        a = a_r.next(); b = b_r.next()
        p.dma(a.v, xTv[:, :, NCTX + t0:NCTX + t0 + 512])
        p.dma(b.v, xTv[:, :, NCTX + H + t0:NCTX + H + t0 + 512])
        p.ts(a.v, a.v, sv[:, 0:1], None, op0=OP.mult)
        p.stt(a.v, b.v, sv[:, 1:2], a.v, OP.mult, OP.add)
        p.dma(V(xH.sub(t0), xHv.ap[:, :, t0:t0 + 512]), a.v)
    ph.close()
    K.xT = xH
    K.NT = H
    _NOCTX[0] = True
    K.FFN_BLK = H
    K.FFN_NS = 7


SSD_INNER = 2048
SSD_H = 32
SSD_G = 8


def ssd_layer(K, li, j):
    p = K.p
    NT = K.NT
    NTI = NT // 128
    w_in = K.W["ssd_w_in"]
    szT = p.dram("ssd_szT", [2048, NT], BF16)
    xbcT = p.dram("ssd_xbcT", [4096, NT], BF16)
    Xtm = p.dram("ssd_Xtm", [NT, 2048], BF16)
    Btm = p.dram("ssd_Btm", [NT, 1024], BF16)
    BT = p.dram("ssd_BT", [1024, NT], BF16)
    CT = p.dram("ssd_CT", [1024, NT], BF16)
    dxsT = p.dram("ssd_dxsT", [2048, NT], BF16)
    YfT = p.dram("ssd_YfT", [2048, NT], F32)
    ynT = p.dram("ssd_ynT", [2048, NT], BF16)

    lph = Phase(p)
    dt_all = lph.sb("dt_all", [128, NTI, 64], F32)
    lndt_all = lph.sb("lndt_all", [128, NTI, 64], F32)
    dtA_all = lph.sb("dtA_all", [128, NTI, 64], F32)
    dtb_bc = lph.sb("dtb_bc", [128, 64], F32)
    A_bc = lph.sb("A_bc", [128, 64], F32)
    p.dma(dtb_bc.v, K.W["ssd_dt_bias"][j].re("d h -> (d h)").unsq(0).bc([128, 64]))
    p.dma(A_bc.v, K.W["ssd_a_log"][j].re("d h -> (d h)").unsq(0).bc([128, 64]))
    p.act(A_bc.v, A_bc.v, AF.Exp)
    p.ts(A_bc.v, A_bc.v, -1.0, None, op0=OP.mult)
    convw = load_vec_fm(K, lph, K.W["ssd_conv_w"][j].re("k (n q) -> (k n) q", q=128), 5 * 32, "ssd_cw")
    convb = load_vec_fm(K, lph, K.W["ssd_conv_b"][j].re("(n q) -> n q", q=128), 32, "ssd_cb")
    ngT = load_vec_fm(K, lph, K.W["ssd_norm_g"][j].re("(n q) -> n q", q=128), 16, "ssd_ng")
    dT = lph.sb("ssd_dT", [128, 16], F32)
    dv = K.W["ssd_d"][j].re("(c two) -> two c", two=2)
    p.dma(dT[0:64, :], dv[0:1, :].bc([64, 16]), allow_slow_non_contiguous=True)
    p.dma(dT[64:128, :], dv[1:2, :].bc([64, 16]), allow_slow_non_contiguous=True)

    def setupA(st):
        st["sz"] = st["ph"].rot("sz", [128, 16, 512], BF16, 1)
        st["Wdt"] = st["ph"].sb("Wdt", [128, KC, 64], BF16)
        load_w_bf16(K, st["Wdt"], lambda kc: w_in[j, kc * 128:(kc + 1) * 128, 6144:6208], KC, 64)
        st["t64"] = st["ph"].rot("t64", [128, 64], F32, 2)

    def sinkA(st, jj, ps, t0, n, isctx):
        if jj == 0:
            st["cur"] = st["sz"].next()
        p.act(st["cur"][:, jj, 0:n], ps[:, 0:n], AF.Silu)
        if jj == 15:
            p.dma(V(szT.sub(t0), szT.v.re("(c q) t -> q c t", q=128).ap[:, :, t0:t0 + n]), st["cur"][:, :, 0:n])

    def extraA(st, hT, hf, t0, n, isctx):
        for jt in range(n // 128):
            ti = t0 // 128 + jt
            ps = K.ps[5]
            for kc in range(KC):
                p.mm(ps[:, 0:64], hT[:, kc, jt * 128:(jt + 1) * 128], st["Wdt"][:, kc, :], start=(kc == 0), stop=(kc == KC - 1))
            t = st["t64"].next()
            p.tt(t.v, ps[:, 0:64], dtb_bc.v, OP.add)
            p.act(t.v, t.v, AF.Exp)
            p.act(dt_all[:, ti, :], t.v, AF.Ln, bias=1.0)
            p.act(lndt_all[:, ti, :], dt_all[:, ti, :], AF.Ln)
            p.tt(dtA_all[:, ti, :], dt_all[:, ti, :], A_bc.v, OP.mult)

    phase_proj(K, 0, lambda kc: w_in[j, kc * 128:(kc + 1) * 128, 0:2048], 2048, sinkA, extraA, setup=setupA)

    def setupB(st):
        st["xb"] = st["ph"].rot("xb", [128, 16, 512], BF16, 2)

    def sinkB(st, jj, ps, t0, n, isctx):
        if jj % 16 == 0:
            st["cur"] = st["xb"].next()
        p.copy(st["cur"][:, jj % 16, 0:n], ps[:, 0:n], e=("act" if jj % 2 else "dve"))
        if jj % 16 == 15:
            half = jj // 16
            p.dma(V(xbcT.sub((t0, half)), xbcT.v.re("(c q) t -> q c t", q=128).ap[:, half * 16:(half + 1) * 16, t0:t0 + n]),
                  st["cur"][:, :, 0:n])

    phase_proj(K, 0, lambda kc: w_in[j, kc * 128:(kc + 1) * 128, 2048:6144], 4096, sinkB, setup=setupB)
    if getattr(K, "dbg_stop", 0) == 1:
        lph.close()
        return

    ph = Phase(p)
    CB = 1024
    xin_r = ph.rot("cv_in", [128, 4, CB + 4], BF16, 2)
    acc_r = ph.rot("cv_acc", [128, CB], F32, 2)
    out_r = ph.rot("cv_out", [128, 4, CB], BF16, 2)
    dx_r = ph.rot("cv_dx", [128, 4, CB], BF16, 2)
    tm_r = ph.rot("cv_tm", [128, CB // 128, 512], BF16, 2)
    segs = [(0, NCTX), (NCTX, NT - NCTX)]
    pi = 0
    for s0, slen in segs:
        for b0 in range(0, slen, CB):
            n = min(CB, slen - b0)
            t0 = s0 + b0
            lo = 2 if b0 > 0 else 0
            hi = 2 if b0 + n < slen else 0
            for cg in range(8):
                xin = xin_r.next()
                if lo == 0:
                    p.memset(xin[:, :, 0:2], 0.0, e="pool")
                if hi == 0:
                    p.memset(xin[:, :, n + 2:n + 4], 0.0, e="pool")
                p.dma(xin[:, :, 2 - lo:n + 2 + hi],
                      xbcT.v.re("(c q) t -> q c t", q=128)[:, cg * 4:(cg + 1) * 4, t0 - lo:t0 + n + hi])
                out = out_r.next()
                for c4 in range(4):
                    cc = cg * 4 + c4
                    acc = acc_r.next()
                    p.ts(acc[:, 0:n], xin[:, c4, 0:n], convw[:, cc:cc + 1], convb[:, cc:cc + 1], op0=OP.mult, op1=OP.add)
                    for k in range(1, 5):
                        p.stt(acc[:, 0:n], xin[:, c4, k:k + n], convw[:, k * 32 + cc:k * 32 + cc + 1], acc[:, 0:n], OP.mult, OP.add)
                    p.act(out[:, c4, 0:n], acc[:, 0:n], AF.Silu)
                if cg < 4:
                    dx = dx_r.next()
                    for c4 in range(4):
                        cc = cg * 4 + c4
                        p.act(dx[:, c4, 0:n], out[:, c4, 0:n], AF.Identity, scale=dT[:, cc:cc + 1])
                    p.dma(V(dxsT.sub((t0, cg)), dxsT.v.re("(c q) t -> q c t", q=128).ap[:, cg * 4:(cg + 1) * 4, t0:t0 + n]), dx[:, :, 0:n])
                if cg < 6:
                    tm = tm_r.next()
                    for jt in range(n // 128):
                        ps = K.ps[pi % 4]
                        pi += 1
                        psb = ps.v.bitcast(BF16)
                        for c4 in range(4):
                            p.tr(psb[:, c4 * 128:(c4 + 1) * 128], out[:, c4, jt * 128:(jt + 1) * 128], K.ident_b.v)
                        p.copy(tm[:, jt, :], psb[:, 0:512], e=("act" if jt % 2 else "dve"))
                    if cg < 4:
                        dst = V(Xtm.sub((t0, cg)), Xtm.v.re("(jt q) c -> q jt c", q=128).ap[:, t0 // 128:(t0 + n) // 128, cg * 512:(cg + 1) * 512])
                    else:
                        dst = V(Btm.sub((t0, cg)), Btm.v.re("(jt q) c -> q jt c", q=128).ap[:, t0 // 128:(t0 + n) // 128, (cg - 4) * 512:(cg - 3) * 512])
                    p.dma(dst, tm[:, 0:n // 128, :])
                if 4 <= cg < 6:
                    p.dma(V(BT.sub((t0, cg)), BT.v.re("(c q) t -> q c t", q=128).ap[:, (cg - 4) * 4:(cg - 3) * 4, t0:t0 + n]), out[:, :, 0:n])
                if cg >= 6:
                    p.dma(V(CT.sub((t0, cg)), CT.v.re("(c q) t -> q c t", q=128).ap[:, (cg - 6) * 4:(cg - 5) * 4, t0:t0 + n]), out[:, :, 0:n])
    ph.close()
    if getattr(K, "dbg_stop", 0) == 2:
        lph.close()
        return

    ssd_scan(K, dict(Xtm=Xtm, Btm=Btm, BT=BT, CT=CT, dxsT=dxsT, szT=szT, YfT=YfT, ynT=ynT,
                     dtA_all=dtA_all, lndt_all=lndt_all))
    if getattr(K, "dbg_stop", 0) == 3:
        lph.close()
        return
    w_out = K.W["ssd_w_out"]
    phase_outproj(K, ynT, 16, lambda kc: w_out[j, kc * 128:(kc + 1) * 128, :], 2, row_scale=ngT)
    lph.close()


def ssd_scan(K, T):
    p = K.p
    NT = K.NT
    NTI = NT // 128
    nctx_t = NCTX // 128
    ph = Phase(p)
    S32 = ph.sb("S32", [128, 2048], F32)
    Sbf = ph.sb("Sbf", [128, 2048], BF16)
    X_r = ph.rot("sc_X", [128, 2048], BF16, 2)
    X2_r = ph.rot("sc_X2", [128, 2048], BF16, 2)
    Bt_r = ph.rot("sc_Bt", [128, 1024], BF16, 2)
    BT_r = ph.rot("sc_BT", [128, 8, 128], BF16, 2)
    CT_r = ph.rot("sc_CT", [128, 8, 128], BF16, 2)
    sm_r = ph.rot("sc_sm", [128, 4, 32], F32, 2)
    E_r = ph.rot("sc_E", [128, 4, 128], F32, 3)
    ea_r = ph.rot("sc_ea", [128, 4, 128], F32, 3)
    WT_r = ph.rot("sc_WT", [128, 4, 128], BF16, 3)
    Cp_r = ph.rot("sc_Cp", [128, 4, 128], BF16, 3)
    Y_r = ph.rot("sc_Y", [128, 16, 128], F32, 2)
    Yf_r = ph.rot("sc_Yf", [128, 16, 128], F32, 2)
    dx_r = ph.rot("sc_dx", [128, 16, 128], BF16, 2)
    sz_r = ph.rot("sc_sz", [128, 16, 128], BF16, 2)
    sq_r = ph.rot("sc_sq", [128, 16, 128], BF16, 1)
    rs_r = ph.rot("sc_rs", [128, 8, 128], F32, 1)
    yn_r = ph.rot("sc_yn", [128, 16, 128], BF16, 2)
    tmp_r = ph.rot("sc_tmp", [128, 256], F32, 2)
    fm = lambda t: t.v.re("(c q) t -> q c t", q=128)

    def run(d, chunks, fresh):
        mask = K.mask_le if d == 0 else K.mask_ge
        if fresh:
            p.memset(S32.v, 0.0)
            p.memset(Sbf.v, 0.0, e="pool")
        for ci in chunks:
            c0 = ci * 128
            X = X_r.next(); Bt = Bt_r.next(); BTc = BT_r.next(); CTc = CT_r.next()
            p.dma(X.v, T["Xtm"][c0:c0 + 128, :])
            p.dma(Bt.v, T["Btm"][c0:c0 + 128, :])
            p.dma(BTc.v, fm(T["BT"])[:, :, c0:c0 + 128])
            p.dma(CTc.v, fm(T["CT"])[:, :, c0:c0 + 128])
            dtA = T["dtA_all"][:, ci, d * 32:(d + 1) * 32]
            lndt = T["lndt_all"][:, ci, d * 32:(d + 1) * 32]
            pm = K.ps[7]
            p.mm(pm[:, 0:32], mask.v, dtA)
            p.mm(pm[:, 32:64], K.ones_f.v, dtA)
            sm = sm_r.next()
            nb = sm[:, 1, :]; wend = sm[:, 2, :]; et = sm[:, 3, :]
            p.tt(nb, lndt, pm[:, 0:32], OP.subtract)
            p.tt(wend, pm[:, 32:64], nb, OP.add)
            p.act(wend, wend, AF.Exp)
            p.act(et, pm[:, 32:64], AF.Exp)
            X2 = X2_r.next()
            p.tt(X2.v.re("q (h e) -> q h e", e=64), X.v.re("q (h e) -> q h e", e=64), wend.unsq(2).bc([128, 32, 64]), OP.mult)
            Ysb = Y_r.next()

            def stageA(g):
                pa = K.ps[g % 2]
                pav = pa.v.re("q (r l) -> q r l", r=4)
                for r in range(4):
                    h = 4 * g + r
                    p.mm(pav[:, r, :], dtA[:, h:h + 1].bc([128, 128]), mask.v)
                pg = K.ps[2 + g % 2]
                p.mm(pg[:, 0:128], BTc[:, g, :], CTc[:, g, :])

            BUF = {}

            def stageB(g):
                pa = K.ps[g % 2]
                pav = pa.v.re("q (r l) -> q r l", r=4)
                pg = K.ps[2 + g % 2]
                E = E_r.next(); ea = ea_r.next()
                for r in range(4):
                    h = 4 * g + r
                    p.act(E[:, r, :], pav[:, r, :], AF.Exp, bias=nb[:, h:h + 1])
                p.act(ea.v, pav, AF.Exp)
                if d == 0:
                    p.aselect(E.v, E.v, [[0, 4], [1, 128]], OP.is_ge, 0.0, base=0, cm=-1)
                else:
                    p.aselect(E.v, E.v, [[0, 4], [-1, 128]], OP.is_ge, 0.0, base=0, cm=1)
                WT = WT_r.next(); Cp = Cp_r.next()
                p.tt(WT.v, E.v, pg[:, 0:128].unsq(1).bc([128, 4, 128]), OP.mult)
                p.tt(Cp.v, ea.v, CTc[:, g, :].unsq(1).bc([128, 4, 128]), OP.mult)
                BUF[g] = (WT, Cp)

            def stageCD(g):
                WT, Cp = BUF.pop(g)
                py = K.ps[4 + g % 2]
                pyv = py[:, 0:256].re("q (c l) -> q c l", c=2)
                for r in range(4):
                    h = 4 * g + r
                    o = pyv[(r % 2) * 64:(r % 2) * 64 + 64, r // 2, :]
                    p.mm(o, X[:, h * 64:(h + 1) * 64], WT[:, r, :], start=True, stop=False)
                    p.mm(o, V(Sbf.sub(g), Sbf.h[:, h * 64:(h + 1) * 64]), Cp[:, r, :], start=False, stop=True)
                pst = K.ps[6 + g % 2] if g % 2 == 0 else K.ps[6]
                pst = K.ps[6]
                p.mm(pst[:, 0:256], Bt[:, g * 128:(g + 1) * 128], X2[:, g * 256:(g + 1) * 256])
                p.copy(Ysb[:, 2 * g:2 * g + 2, :], pyv, e="act")
                sg = V(S32.sub(g), S32.h[:, g * 256:(g + 1) * 256])
                p.tt(sg.re("q (r e) -> q r e", e=64), sg.re("q (r e) -> q r e", e=64),
                     et[:, 4 * g:4 * g + 4].unsq(2).bc([128, 4, 64]), OP.mult)
                p.tt(sg, sg, pst[:, 0:256], OP.add)
                p.copy(V(Sbf.sub(g), Sbf.h[:, g * 256:(g + 1) * 256]), sg, e="pool")

            stageA(0)
            stageB(0)
            stageA(1)
            for g in range(8):
                if g + 1 < 8:
                    stageB(g + 1)
                if g + 2 < 8:
                    stageA(g + 2)
                stageCD(g)
            if d == 0:
                p.dma(V(T["YfT"].sub(ci), fm(T["YfT"]).ap[:, :, c0:c0 + 128]), Ysb.v)
            else:
                Yf = Yf_r.next(); dx = dx_r.next(); sz = sz_r.next()
                p.dma(Yf.v, V(T["YfT"].sub(ci), fm(T["YfT"]).ap[:, :, c0:c0 + 128]))
                p.dma(dx.v, fm(T["dxsT"])[:, :, c0:c0 + 128])
                p.dma(sz.v, fm(T["szT"])[:, :, c0:c0 + 128])
                p.tt(Ysb.v, Ysb.v, Yf.v, OP.add)
                p.tt(Ysb.v, Ysb.v, dx.v, OP.add, e="pool")
                p.tt(Ysb.v, Ysb.v, sz.v, OP.mult)
                sq = sq_r.next()
                p.act(sq.v, Ysb.v, AF.Square)
                pr0 = K.ps[0]; pr1 = K.ps[1]
                for g in range(8):
                    pr = (pr0 if g < 4 else pr1)[:, (g % 4) * 128:(g % 4 + 1) * 128]
                    p.mm(pr, K.ones_b.v, sq[:, 2 * g, :], start=True, stop=False)
                    p.mm(pr, K.ones_b.v, sq[:, 2 * g + 1, :], start=False, stop=True)
                rs = rs_r.next()
                p.act(rs[:, 0:4, :], pr0.v.re("q (g l) -> q g l", g=4), AF.Sqrt, scale=1.0 / 256, bias=K.eps_t[:, 0:1])
                p.act(rs[:, 4:8, :], pr1.v.re("q (g l) -> q g l", g=4), AF.Sqrt, scale=1.0 / 256, bias=K.eps_t[:, 0:1])
                p.recip(rs.v, rs.v)
                yn = yn_r.next()
                p.tt(yn.v.re("q (g c) l -> q g c l", c=2), Ysb.v.re("q (g c) l -> q g c l", c=2),
                     rs.v.unsq(2).bc([128, 8, 2, 128]), OP.mult)
                p.dma(V(T["ynT"].sub(ci), fm(T["ynT"]).ap[:, :, c0:c0 + 128]), yn.v)

    ctx_chunks = list(range(nctx_t))
    lat_chunks = list(range(nctx_t, NTI))
    run(0, ctx_chunks + lat_chunks, True)
    p.barrier()
    run(1, ctx_chunks[::-1] + lat_chunks[::-1], True)
    ph.close()


def hgrn_layer(K, li, j):
    p = K.p
    NT = K.NT
    w_in = K.W["hgrn_w_in"]
    qT = p.dram("hg_qT", [1024, NT], BF16)
    fT = p.dram("hg_fT", [2048, NT], F32)
    sgT = p.dram("hg_sgT", [1024, NT], BF16)
    Vtm = p.dram("hg_Vtm", [NT, 1024], BF16)
    OfT = p.dram("hg_OfT", [1024, NT], F32)
    ynT = p.dram("hg_ynT", [1024, NT], BF16)
    fm = lambda t: t.v.re("(c q) t -> q c t", q=128)

    lph = Phase(p)
    ngT = load_vec_fm(K, lph, K.W["hgrn_norm_g"][j].re("(n q) -> n q", q=128), 8, "hg_ng")
    lbr = load_vec_fm(K, lph, K.W["hgrn_lb"].v.re("d l (n q) -> (d l n) q", q=128), 64, "hg_lbr")
    lb = lph.sb("hg_lb", [128, 2, 8], F32)
    oml = lph.sb("hg_oml", [128, 2, 8], F32)
    ssum = lph.sb("hg_ssum", [128, 2, 8], F32)
    p.act(lbr.v, lbr.v, AF.Exp)
    l4 = lbr.v.re("q (d l n) -> q d l n", d=2, l=4)
    p.tt(ssum.v, l4[:, :, 0, :], l4[:, :, 1, :], OP.add)
    p.tt(ssum.v, ssum.v, l4[:, :, 2, :], OP.add)
    p.tt(ssum.v, ssum.v, l4[:, :, 3, :], OP.add)
    p.recip(ssum.v, ssum.v)
    p.memset(lb.v, 0.0)
    for l in range(1, li + 1):
        p.tt(lb.v, lb.v, l4[:, :, l, :], OP.add)
    p.tt(lb.v, lb.v, ssum.v, OP.mult)
    p.ts(oml.v, lb.v, -1.0, 1.0, op0=OP.mult, op1=OP.add)

    def setupA(st):
        st["qs"] = st["ph"].rot("qs", [128, 8, 512], BF16, 1)
        st["fs"] = st["ph"].rot("fs", [128, 8, 512], F32, 2)

    def sinkA(st, jj, ps, t0, n, isctx):
        if jj < 8:
            if jj == 0:
                st["cq"] = st["qs"].next()
            p.act(st["cq"][:, jj, 0:n], ps[:, 0:n], AF.Silu)
            if jj == 7:
                p.ts(st["cq"][:, :, 0:n], st["cq"][:, :, 0:n], 128.0 ** -0.5, None, op0=OP.mult, e="pool")
                p.dma(V(qT.sub(t0), fm(qT).ap[:, :, t0:t0 + n]), st["cq"][:, :, 0:n])
        else:
            k = (jj - 8) % 8
            if k == 0:
                st["cf"] = st["fs"].next()
            p.copy(st["cf"][:, k, 0:n], ps[:, 0:n], e=("act" if jj % 2 else "dve"))
            if k == 7:
                half = (jj - 8) // 8
                p.dma(V(fT.sub((t0, half)), fm(fT).ap[:, half * 8:(half + 1) * 8, t0:t0 + n]), st["cf"][:, :, 0:n])

    phase_proj(K, 0, lambda kc: w_in[j, kc * 128:(kc + 1) * 128, 0:3072], 3072, sinkA, setup=setupA)

    def setupB(st):
        st["gs"] = st["ph"].rot("gs", [128, 8, 512], BF16, 1)
        st["vs"] = st["ph"].rot("vs", [128, 4, 1024], BF16, 2)

    def sinkB(st, jj, ps, t0, n, isctx):
        if jj < 8:
            return
        k = jj - 8
        if k == 0:
            st["cg"] = st["gs"].next()
        p.act(st["cg"][:, k, 0:n], ps[:, 0:n], AF.Silu)
        if k == 7:
            p.dma(V(sgT.sub(t0), fm(sgT).ap[:, :, t0:t0 + n]), st["cg"][:, :, 0:n])

    def extraB(st, hT, hf, t0, n, isctx):
        vs = st["vs"].next()
        W = st["W"]
        for jt in range(n // 128):
            for half in range(2):
                ps = K.ps[4 + half]
                for kc in range(KC):
                    p.mm(ps.v, hT[:, kc, jt * 128:(jt + 1) * 128], W[:, kc, half * 512:(half + 1) * 512],
                         start=(kc == 0), stop=(kc == KC - 1))
                p.copy(vs[:, jt, half * 512:(half + 1) * 512], ps.v, e=("act" if half else "dve"))
        p.dma(V(Vtm.sub(t0), Vtm.v.re("(jt q) c -> q jt c", q=128).ap[:, t0 // 128:(t0 + n) // 128, :]), vs[:, 0:n // 128, :])

    class SkipSink:
        pass

    def sinkB_wrap(st, jj, ps, t0, n, isctx):
        sinkB(st, jj, ps, t0, n, isctx)

    phase_proj(K, 0, lambda kc: w_in[j, kc * 128:(kc + 1) * 128, 3072:5120], 2048, sinkB_wrap, extraB, setup=setupB,
               )

    ph = Phase(p)
    S = ph.sb("hg_S", [128, 8, 128], F32)
    qf_r = ph.rot("hg_q", [128, 512], BF16, 2)
    ff_r = ph.rot("hg_f", [128, 512], F32, 2)
    t1_r = ph.rot("hg_t1", [128, 512], F32, 2)
    t2_r = ph.rot("hg_t2", [128, 512], F32, 2)
    t3_r = ph.rot("hg_t3", [128, 512], F32, 2)
    kk_r = ph.rot("hg_kk", [128, 512], F32, 2)
    qt_a = ph.sb("hg_qt", [128, 8, 512], BF16)
    kt_a = ph.sb("hg_kt", [128, 8, 512], BF16)
    kh_r = ph.rot("hg_kh", [128, 512], BF16, 2)
    khtm = ph.sb("hg_khtm", [128, 4, 8, 128], BF16)
    sc_a = ph.sb("hg_sc", [128, 8, 3, 8], F32)
    sm_r = ph.rot("hg_sm", [128, 4, 8], F32, 2)
    V_r = ph.rot("hg_V", [128, 4, 1024], BF16, 2)
    at_r = ph.rot("hg_at", [128, 8, 64], BF16, 2)
    Sp_r = ph.rot("hg_Sp", [128, 8, 128], BF16, 2)
    O_r = ph.rot("hg_O", [128, 8, 128], F32, 2)
    Of_r = ph.rot("hg_Of", [128, 8, 128], F32, 2)
    sg_r = ph.rot("hg_sg", [128, 8, 128], BF16, 2)
    sq_r = ph.rot("hg_sq", [128, 8, 128], BF16, 1)
    rs_r = ph.rot("hg_rs", [128, 8, 128], F32, 1)
    yn_r = ph.rot("hg_yn", [128, 8, 128], BF16, 2)

    blocks = sub_blocks(NT)

    def run(d):
        sig = 1.0 if d == 0 else -1.0
        p.memset(S.v, 0.0)
        order = blocks if d == 0 else [blocks[0]] + blocks[1:][::-1]
        pi = 0
        for (t0, n, isctx) in order:
            nch = n // 64
            ntl = n // 128
            Vt = V_r.next()
            p.dma(Vt[:, 0:ntl, :], Vtm.v.re("(jt q) c -> q jt c", q=128)[:, t0 // 128:(t0 + n) // 128, :])
            for h in range(8):
                qf = qf_r.next(); ff = ff_r.next()
                p.dma(qf[:, 0:n], qT[h * 128:(h + 1) * 128, t0:t0 + n])
                p.dma(ff[:, 0:n], fT[(d * 8 + h) * 128:(d * 8 + h + 1) * 128, t0:t0 + n])
                t1 = t1_r.next(); t2 = t2_r.next(); t3 = t3_r.next(); kk = kk_r.next()
                p.act(t1[:, 0:n], ff[:, 0:n], AF.Sigmoid)
                p.ts(t1[:, 0:n], t1[:, 0:n], oml[:, d, h:h + 1], lb[:, d, h:h + 1], op0=OP.mult, op1=OP.add)
                p.act(t2[:, 0:n], t1[:, 0:n], AF.Ln)
                p.ts(kk[:, 0:n], t1[:, 0:n], -1.0, 1.0, op0=OP.mult, op1=OP.add, e="pool")
                p.scan(t3[:, 0:n], K.ones_f[:, 0:1].bc([128, n]), t2[:, 0:n], 0.0, OP.mult, OP.add)
                sm = sm_r.next()
                ref, lo, hi, tm = sm[:, 0, 0:nch], sm[:, 1, 0:nch], sm[:, 2, 0:nch], sm[:, 3, 0:nch]
                P3 = t3[:, 0:n].re("q (c l) -> q c l", l=64)
                L3 = t2[:, 0:n].re("q (c l) -> q c l", l=64)
                p.tt(lo, P3[:, :, 0], L3[:, :, 0], OP.subtract)
                p.copy(hi, P3[:, :, 63])
                if d == 0:
                    Z = t3
                    p.copy(ref, P3[:, :, 32])
                else:
                    Z = t1
                    p.tt(Z[:, 0:n], t3[:, 0:n], t2[:, 0:n], OP.subtract)
                    p.copy(ref, Z[:, 0:n].re("q (c l) -> q c l", l=64)[:, :, 32])
                p.tt(Z[:, 0:n].re("q (c l) -> q c l", l=64), Z[:, 0:n].re("q (c l) -> q c l", l=64),
                     ref.unsq(2).bc([128, nch, 64]), OP.subtract)
                p.act(t2[:, 0:n], Z[:, 0:n], AF.Exp, scale=sig)
                p.tt(qt_a[:, h, 0:n], qf[:, 0:n], t2[:, 0:n], OP.mult)
                p.act(t2[:, 0:n], Z[:, 0:n], AF.Exp, scale=-sig)
                p.tt(kt_a[:, h, 0:n], kk[:, 0:n], t2[:, 0:n], OP.mult)
                sc = sc_a[:, h]
                a_, b_ = (0, 1) if d == 0 else (1, 0)
                p.tt(tm, ref, lo, OP.subtract)
                p.act(sc[:, a_, 0:nch], tm, AF.Exp)
                p.tt(tm, hi, ref, OP.subtract)
                p.act(sc[:, b_, 0:nch], tm, AF.Exp)
                p.tt(tm, hi, lo, OP.subtract)
                p.act(sc[:, 2, 0:nch], tm, AF.Exp)
                kh = kh_r.next()
                p.tt(kh[:, 0:n].re("q (c l) -> q c l", l=64), kt_a[:, h, 0:n].re("q (c l) -> q c l", l=64),
                     sc[:, 1, 0:nch].unsq(2).bc([128, nch, 64]), OP.mult)
                for jt in range(ntl):
                    ps = K.ps[7]
                    psb = ps.v.bitcast(BF16)
                    p.tr(psb[:, 0:128], kh[:, jt * 128:(jt + 1) * 128], K.ident_b.v)
                    p.copy(khtm[:, jt, h, :], psb[:, 0:128], e=("act" if jt % 2 else "dve"))
            tiles = list(range(ntl)) if d == 0 else list(range(ntl))[::-1]
            for jt in tiles:
                c0 = t0 + jt * 128
                Osb = O_r.next()
                for half in ((0, 1) if d == 0 else (1, 0)):
                    ci = jt * 2 + half
                    pb = half * 64
                    cs = slice(ci * 64, ci * 64 + 64)
                    pat = K.ps[pi % 2]
                    po = K.ps[2 + pi % 2]
                    pi += 1
                    at = at_r.next(); Sp = Sp_r.next()
                    mk = (K.mask_le_b if d == 0 else K.mask_ge_b)[pb:pb + 64, pb:pb + 64]
                    for h in range(8):
                        p.mm(pat[pb:pb + 64, h * 64:(h + 1) * 64], kt_a[:, h, cs], qt_a[:, h, cs])
                    p.tt(at[pb:pb + 64, :, :], pat[pb:pb + 64, :].re("q (h l) -> q h l", l=64),
                         mk.unsq(1).bc([64, 8, 64]), OP.mult)
                    p.tt(Sp.v, S.v, sc_a[:, :, 0, ci:ci + 1].bc([128, 8, 128]), OP.mult, e="pool")
                    for h in range(8):
                        o = po[:, h * 64:(h + 1) * 64]
                        p.mm(o, Vt[pb:pb + 64, jt, h * 128:(h + 1) * 128], at[pb:pb + 64, h, :], start=True, stop=False)
                        p.mm(o, Sp[:, h, :], qt_a[:, h, cs], start=False, stop=True)
                    p.copy(Osb[:, :, half * 64:(half + 1) * 64], po.v.re("q (h l) -> q h l", l=64), e="act")
                    pS = [K.ps[4], K.ps[5]]
                    for h in range(8):
                        p.mm(pS[h // 4][:, (h % 4) * 128:(h % 4 + 1) * 128], khtm[pb:pb + 64, jt, h, :],
                             Vt[pb:pb + 64, jt, h * 128:(h + 1) * 128])
                    p.tt(S.v, S.v, sc_a[:, :, 2, ci:ci + 1].bc([128, 8, 128]), OP.mult)
                    p.tt(S[:, 0:4, :], S[:, 0:4, :], pS[0].v.re("q (h v) -> q h v", v=128), OP.add)
                    p.tt(S[:, 4:8, :], S[:, 4:8, :], pS[1].v.re("q (h v) -> q h v", v=128), OP.add)
                if d == 0:
                    p.dma(V(OfT.sub(c0), fm(OfT).ap[:, :, c0:c0 + 128]), Osb.v)
                else:
                    if isctx and not K.want_ctx:
                        continue
                    Of = Of_r.next(); sg = sg_r.next()
                    p.dma(Of.v, V(OfT.sub(c0), fm(OfT).ap[:, :, c0:c0 + 128]))
                    p.dma(sg.v, fm(sgT)[:, :, c0:c0 + 128])
                    p.tt(Osb.v, Osb.v, Of.v, OP.add, e="pool")
                    sq = sq_r.next()
                    p.act(sq.v, Osb.v, AF.Square)
                    pr = [K.ps[6], K.ps[7]]
                    for hh in range(2):
                        p.mm(pr[hh].v, K.ones_b.v, sq[:, hh * 4:(hh + 1) * 4, :])
                    rs = rs_r.next()
                    for hh in range(2):
                        p.act(rs[:, hh * 4:(hh + 1) * 4, :], pr[hh].v.re("q (h l) -> q h l", l=128), AF.Sqrt,
                              scale=1.0 / 128, bias=K.eps_t[:, 0:1])
                    p.recip(rs.v, rs.v)
                    p.tt(Osb.v, Osb.v, rs.v, OP.mult)
                    yn = yn_r.next()
                    p.tt(yn.v, Osb.v, sg.v, OP.mult)
                    p.dma(V(ynT.sub(c0), fm(ynT).ap[:, :, c0:c0 + 128]), yn.v)

    run(0)
    p.barrier()
    run(1)
    ph.close()
    w_out = K.W["hgrn_w_out"]
    phase_outproj(K, ynT, 8, lambda kc: w_out[j, kc * 128:(kc + 1) * 128, :], 2, row_scale=ngT)
    lph.close()


import math


def attn_layer(K, li, j):
    p = K.p
    NT = K.NT
    TL = K.TL
    w_qkv = K.W["attn_w_qkv"]
    qkT = p.dram("at_qkT", [1280, NT], BF16)
    Vtm = p.dram("at_Vtm", [NT, 256], BF16)
    oT = p.dram("at_oT", [1024, NT], BF16)

    lph = Phase(p)
    gq = lph.sb("at_gq", [128, 2], F32)
    for half in range(2):
        p.dma(gq[half * 64:(half + 1) * 64, 0:1], K.W["attn_q_g"][j].unsq(1), allow_slow_non_contiguous=True)
        p.dma(gq[half * 64:(half + 1) * 64, 1:2], K.W["attn_k_g"][j].unsq(1), allow_slow_non_contiguous=True)

    def setupA(st):
        ph = st["ph"]
        bd = ph.sb("at_bd", [128, 128], BF16)
        p.memset(bd.v, 0.0)
        p.memset(bd[0:64, 0:64], 1.0)
        p.memset(bd[64:128, 64:128], 1.0)
        st["bd"] = bd
        mfree = ph.sb("at_mfree", [128, 128], F32)
        p.iota(mfree.v.re("q (a b c) -> q a b c", a=4, b=2), [[0, 4], [1, 2], [0, 16]], base=0, cm=0)
        up = ph.sb("at_up", [128, 128], F32)
        dn = ph.sb("at_dn", [128, 128], F32)
        p.aselect(up.v, K.ones_f.v, [[1, 128]], OP.is_equal, 0.0, base=-16, cm=-1)
        p.aselect(dn.v, K.ones_f.v, [[-1, 128]], OP.is_equal, 0.0, base=-16, cm=1)
        Rm = ph.sb("at_Rm", [128, 128], F32)
        p.tt(up.v, up.v, mfree.v, OP.mult)
        p.ts(mfree.v, mfree.v, -1.0, 1.0, op0=OP.mult, op1=OP.add)
        p.tt(dn.v, dn.v, mfree.v, OP.mult)
        p.tt(Rm.v, up.v, dn.v, OP.subtract)
        st["Rm"] = Rm
        pid = ph.sb("at_pid", [128, 1], I32)
        p.iota(pid.v, [[0, 1]], base=0, cm=1)
        p.emit("dve", [pid.v], [pid.v], lambda: p.nc.vector.tensor_single_scalar(pid.v.ap, pid.v.ap, 15, op=OP.bitwise_and))
        inv = ph.sb("at_inv", [128, 1], F32)
        p.copy(inv.v, pid.v)
        p.act(inv.v, inv.v, AF.Exp, scale=-math.log(10000.0) / 16.0)
        cosT = ph.sb("at_cos", [128, TL], F32)
        sinT = ph.sb("at_sin", [128, TL], F32)
        GW = 64
        rows = TL // GW
        for q0 in range(0, 128, 32):
            pat = [[1, rows], [0, GW]] if (q0 % 64) < 32 else [[0, rows], [1, GW]]
            p.iota(sinT[q0:q0 + 32, :].re("q (r c) -> q r c", c=GW), pat, base=0, cm=0)
        p.ts(sinT.v, sinT.v, inv[:, 0:1], None, op0=OP.mult)
        kk = ph.sb("at_kk", [128, TL], I32)
        p.ts(cosT.v, sinT.v, 1.0 / (2 * math.pi), 0.5, op0=OP.mult, op1=OP.add)
        p.copy(kk.v, cosT.v)
        p.copy(cosT.v, kk.v)
        p.stt(sinT.v, cosT.v, -2.0 * math.pi, sinT.v, OP.mult, OP.add)
        wt = ph.sb("at_wt", [128, TL], F32)

        def wrap(dst, src, shift):
            if shift != 0.0:
                p.ts(dst.v, src.v, shift, None, op0=OP.add)
            elif dst is not src:
                p.copy(dst.v, src.v)
            p.ts(wt.v, dst.v, math.pi, -2.0 * math.pi, op0=OP.is_gt, op1=OP.mult)
            p.tt(dst.v, dst.v, wt.v, OP.add)
            p.ts(wt.v, dst.v, -math.pi, 2.0 * math.pi, op0=OP.is_lt, op1=OP.mult)
            p.tt(dst.v, dst.v, wt.v, OP.add)

        wrap(sinT, sinT, 0.0)
        wrap(cosT, sinT, math.pi / 2)
        p.ts(sinT.v, sinT.v, math.pi, -math.pi, op0=OP.min, op1=OP.max)
        p.ts(cosT.v, cosT.v, math.pi, -math.pi, op0=OP.min, op1=OP.max)
        p.act(sinT.v, sinT.v, AF.Sin)
        p.act(cosT.v, cosT.v, AF.Sin)
        st["cos"], st["sin"] = cosT, sinT
        st["sq"] = ph.rot("at_sq", [128, 512], BF16, 2)
        st["rs"] = ph.rot("at_rs", [128, 512], F32, 2)
        st["qn"] = ph.rot("at_qn", [128, 512], F32, 2)
        st["t1"] = ph.rot("at_t1", [128, 512], F32, 2)
        st["t2"] = ph.rot("at_t2", [128, 512], F32, 2)
        st["ob"] = ph.rot("at_ob", [128, 512], BF16, 3)
        st["vs"] = ph.rot("at_vs", [128, 4, 256], BF16, 2)

    def sinkA(st, jj, ps, t0, n, isctx):
        if jj >= 10:
            return
        isk = 1 if jj >= 8 else 0
        sq = st["sq"].next()
        p.act(sq[:, 0:n], ps[:, 0:n], AF.Square)
        pss = K.ps[4 + jj % 2]
        p.mm(pss[:, 0:n], st["bd"].v, sq[:, 0:n])
        rs = st["rs"].next()
        p.act(rs[:, 0:n], pss[:, 0:n], AF.Sqrt, scale=1.0 / 64, bias=K.eps_t[:, 0:1])
        p.recip(rs[:, 0:n], rs[:, 0:n])
        qn = st["qn"].next()
        p.stt(qn[:, 0:n], ps[:, 0:n], gq[:, isk:isk + 1], rs[:, 0:n], OP.mult, OP.mult)
        ob = st["ob"].next()
        if isctx:
            p.copy(ob[:, 0:n], qn[:, 0:n], e="act")
        else:
            l0 = t0 - NCTX
            pr = K.ps[6 + jj % 2]
            p.mm(pr[:, 0:n], st["Rm"].v, qn[:, 0:n])
            t1 = st["t1"].next(); t2 = st["t2"].next()
            p.tt(t1[:, 0:n], qn[:, 0:n], st["cos"][:, l0:l0 + n], OP.mult, e="pool")
            p.tt(t2[:, 0:n], pr[:, 0:n], st["sin"][:, l0:l0 + n], OP.mult)
            p.tt(ob[:, 0:n], t1[:, 0:n], t2[:, 0:n], OP.add)
        p.dma(V(qkT.sub((t0, jj)), qkT.v.ap[jj * 128:(jj + 1) * 128, t0:t0 + n]), ob[:, 0:n])

    def extraA(st, hT, hf, t0, n, isctx):
        vs = st["vs"].next()
        W = st["W"]
        for jt in range(n // 128):
            ps = K.ps[5]
            for kc in range(KC):
                p.mm(ps[:, 0:256], hT[:, kc, jt * 128:(jt + 1) * 128], W[:, kc, 1280:1536], start=(kc == 0), stop=(kc == KC - 1))
            p.copy(vs[:, jt, :], ps[:, 0:256], e="act")
        p.dma(V(Vtm.sub(t0), Vtm.v.re("(jt q) c -> q jt c", q=128).ap[:, t0 // 128:(t0 + n) // 128, :]), vs[:, 0:n // 128, :])

    phase_proj(K, 0, lambda kc: w_qkv[j, kc * 128:(kc + 1) * 128, :], 1536, sinkA, extraA, setup=setupA)

    ph = Phase(p)
    NKT = NT // 128
    KT_r = ph.rot("at_KT", [64, NT], BF16, 2)
    Vg_r = ph.rot("at_Vg", [128, NKT, 128], BF16, 2)
    Q_r = ph.rot("at_Q", [64, 4, 128], BF16, 4)
    P_r = ph.rot("at_P", [128, 512], BF16, 4)
    rl_r = ph.rot("at_rl", [128, 512], F32, 2)
    rs_r = ph.rot("at_rs2", [64, 512], F32, 2)
    o_r = ph.rot("at_o", [64, 512], BF16, 2)
    shiftM = ph.sb("at_shift", [128, 64], F32)
    p.aselect(shiftM.v, K.ones_f[:, 0:64], [[-1, 64]], OP.is_equal, 0.0, base=-64, cm=1)
    for t in rl_r.tiles:
        p.memset(t.v, 0.0)
    nctx_t = NCTX // 128
    for g in range(4):
        KTg = KT_r.next(); Vg = Vg_r.next()
        p.dma(KTg.v, qkT[1024 + g * 64:1024 + (g + 1) * 64, :])
        p.memset(Vg[:, :, 64:128], 1.0, e="pool")
        p.dma(Vg[:, :, 0:64], Vtm.v.re("(kt q) (g d) -> q kt g d", q=128, g=4)[:, :, g, :])
        steps = []
        for jq in range(NKT):
            isctx = jq < nctx_t
            if isctx and not K.want_ctx:
                continue
            kts = list(range(nctx_t)) if isctx else list(range(NKT))
            for ki, kt in enumerate(kts):
                steps.append((jq, ki, kt, len(kts)))
        Qs = {}

        def getQ(jq):
            if jq not in Qs:
                Q = Q_r.next()
                p.dma(Q.v, qkT.v.re("(h d) t -> d h t", d=64)[:, g * 4:(g + 1) * 4, jq * 128:(jq + 1) * 128])
                Qs[jq] = Q
            return Qs[jq]

        def emitS(si):
            jq, ki, kt, nk = steps[si]
            Q = getQ(jq)
            p.mm(K.ps[si % 4].v, KTg[:, kt * 128:(kt + 1) * 128], Q.v.re("d r q -> d (r q)"))

        emitS(0)
        for si, (jq, ki, kt, nk) in enumerate(steps):
            if si + 1 < len(steps):
                emitS(si + 1)
            po = K.ps[4 + jq % 2]
            Pt = P_r.next()
            p.act(Pt.v, K.ps[si % 4].v, AF.Exp, scale=0.125)
            p.mm(po.v, Vg[:, kt, :], Pt.v, start=(ki == 0), stop=(ki == nk - 1))
            if ki == nk - 1:
                q0 = jq * 128
                rl = rl_r.next()
                p.recip(rl[64:128, :], po[64:128, :])
                pr = K.ps[6 + jq % 2]
                p.mm(pr[0:64, :], shiftM.v, rl.v)
                rs = rs_r.next()
                p.copy(rs.v, pr[0:64, :], e="act")
                o = o_r.next()
                p.tt(o.v, po[0:64, :], rs.v, OP.mult)
                p.dma(V(oT.sub((g, jq)), oT.v.re("(h d) t -> d h t", d=64).ap[:, g * 4:(g + 1) * 4, q0:q0 + 128]),
                      o.v.re("d (r q) -> d r q", r=4))
                Qs.pop(jq, None)
    ph.close()
    w_o = K.W["attn_w_o"]
    phase_outproj(K, oT, 8, lambda kc: w_o[j, kc * 128:(kc + 1) * 128, :], 2)
    lph.close()


def mlstm_layer(K, li, j):
    p = K.p
    NT = K.NT
    NTI = NT // 128
    nctx_t = NCTX // 128
    w_up = K.W["mlstm_w_up"]
    xmT = p.dram("ml_xmT", [2048, NT], BF16)
    szT = p.dram("ml_szT", [2048, NT], BF16)
    xcT = p.dram("ml_xcT", [2048, NT], BF16)
    sxcT = p.dram("ml_sxcT", [2048, NT], BF16)
    qT = p.dram("ml_qT", [2048, NT], BF16)
    kT = p.dram("ml_kT", [2048, NT], BF16)
    ktm = p.dram("ml_ktm", [NT, 2048], BF16)
    vtm = p.dram("ml_vtm", [NT, 2048], BF16)
    HfT = p.dram("ml_HfT", [2048, NT], F32)
    ynT = p.dram("ml_ynT", [2048, NT], BF16)
    fm = lambda t: t.v.re("(c q) t -> q c t", q=128)
    tmv = lambda t: t.v.re("(jt q) c -> q jt c", q=128)

    lph = Phase(p)
    convw = load_vec_fm(K, lph, K.W["mlstm_conv_w"][j].re("k (n q) -> (k n) q", q=128), 5 * 16, "ml_cw")
    convb = load_vec_fm(K, lph, K.W["mlstm_conv_b"][j].re("(n q) -> n q", q=128), 16, "ml_cb")
    ngT = load_vec_fm(K, lph, K.W["mlstm_norm_g"][j].re("(n q) -> n q", q=128), 16, "ml_ng")
    skT = load_vec_fm(K, lph, K.W["mlstm_skip"][j].re("(n q) -> n q", q=128), 16, "ml_sk")
    li_all = lph.sb("ml_li", [128, NTI, 8], F32)
    lf_all = lph.sb("ml_lf", [128, NTI, 8], F32)
    bg = lph.sb("ml_bg", [16, 1], F32)
    p.dma(bg.v, K.W["mlstm_b_gate"][j].unsq(1), allow_slow_non_contiguous=True)

    def setup1(st):
        st["xs"] = st["ph"].rot("ml_xs", [128, 16, 512], BF16, 2)

    def sink1(st, jj, ps, t0, n, isctx):
        k = jj % 16
        if k == 0:
            st["cur"] = st["xs"].next()
        if jj < 16:
            p.copy(st["cur"][:, k, 0:n], ps[:, 0:n], e=("act" if jj % 2 else "dve"))
        else:
            p.act(st["cur"][:, k, 0:n], ps[:, 0:n], AF.Silu)
        if k == 15:
            dst = xmT if jj < 16 else szT
            p.dma(V(dst.sub(t0), fm(dst).ap[:, :, t0:t0 + n]), st["cur"][:, :, 0:n])

    phase_proj(K, 0, lambda kc: w_up[j, kc * 128:(kc + 1) * 128, :], 4096, sink1, setup=setup1)
    if getattr(K, "dbg_stop", 0) == 1:
        lph.close()
        return

    ph = Phase(p)
    CB = 1024
    xin_r = ph.rot("cv_in", [128, 4, CB + 4], BF16, 2)
    acc_r = ph.rot("cv_acc", [128, CB], F32, 2)
    out_r = ph.rot("cv_out", [128, 4, CB], BF16, 2)
    sx_r = ph.rot("cv_sx", [128, 4, CB], BF16, 2)
    for s0, slen in [(0, NCTX), (NCTX, NT - NCTX)]:
        for b0 in range(0, slen, CB):
            n = min(CB, slen - b0)
            t0 = s0 + b0
            lo = 2 if b0 > 0 else 0
            hi = 2 if b0 + n < slen else 0
            for cg in range(4):
                xin = xin_r.next()
                if lo == 0:
                    p.memset(xin[:, :, 0:2], 0.0, e="pool")
                if hi == 0:
                    p.memset(xin[:, :, n + 2:n + 4], 0.0, e="pool")
                p.dma(xin[:, :, 2 - lo:n + 2 + hi], fm(xmT)[:, cg * 4:(cg + 1) * 4, t0 - lo:t0 + n + hi])
                out = out_r.next(); sx = sx_r.next()
                for c4 in range(4):
                    cc = cg * 4 + c4
                    acc = acc_r.next()
                    p.ts(acc[:, 0:n], xin[:, c4, 0:n], convw[:, cc:cc + 1], convb[:, cc:cc + 1], op0=OP.mult, op1=OP.add)
                    for k in range(1, 5):
                        p.stt(acc[:, 0:n], xin[:, c4, k:k + n], convw[:, k * 16 + cc:k * 16 + cc + 1], acc[:, 0:n], OP.mult, OP.add)
                    p.act(out[:, c4, 0:n], acc[:, 0:n], AF.Silu)
                    p.act(sx[:, c4, 0:n], out[:, c4, 0:n], AF.Identity, scale=skT[:, cc:cc + 1])
                p.dma(V(xcT.sub((t0, cg)), fm(xcT).ap[:, cg * 4:(cg + 1) * 4, t0:t0 + n]), out[:, :, 0:n])
                p.dma(V(sxcT.sub((t0, cg)), fm(sxcT).ap[:, cg * 4:(cg + 1) * 4, t0:t0 + n]), sx[:, :, 0:n])
    ph.close()
    if getattr(K, "dbg_stop", 0) == 2:
        lph.close()
        return

    ph = Phase(p)
    Wq = ph.sb("ml_Wq", [128, 16, 512], BF16)
    Wk = ph.sb("ml_Wk", [128, 16, 512], BF16)
    Wv = ph.sb("ml_Wv", [128, 16, 512], BF16)
    for Wt, nm in ((Wq, "mlstm_w_q"), (Wk, "mlstm_w_k"), (Wv, "mlstm_w_v")):
        src = K.W[nm][j].re("h (dc q) e -> q (h dc) e", q=128)
        for i4 in range(4):
            p.dma(Wt[:, i4 * 4:(i4 + 1) * 4, :], src[:, i4 * 4:(i4 + 1) * 4, :], q="pool", max_dma_last_dim=2048 * 4)
    Wg = ph.sb("ml_Wg", [128, 48, 16], BF16)
    p.dma(Wg.v, K.W["mlstm_w_gate"][j].re("(c q) g -> q c g", q=128), q="pool")
    xc_r = ph.rot("ml_xc", [128, 16, 512], BF16, 1)
    xm_r = ph.rot("ml_xm", [128, 16, 512], BF16, 1)
    qkv_r = ph.rot("ml_qkv", [128, 48, 512], BF16, 1)
    ks_r = ph.rot("ml_ks", [128, 16, 512], BF16, 1)
    tm_r = ph.rot("ml_tm", [128, 4, 2048], BF16, 1)
    gT_r = ph.rot("ml_gT", [16, 512], F32, 2)
    g16_r = ph.rot("ml_g16", [128, 16], F32, 2)
    pi = 0
    import os
    P3L = int(os.environ.get("DBG_P3", "9"))
    for (t0, n, isctx) in sub_blocks(NT):
        if P3L < 9 and t0 > 0:
            break
        if P3L < 1:
            break
        ntl = n // 128
        xc = xc_r.next(); xm = xm_r.next()
        p.dma(xc[:, :, 0:n], fm(xcT)[:, :, t0:t0 + n])
        p.dma(xm[:, :, 0:n], fm(xmT)[:, :, t0:t0 + n])
        qkv = qkv_r.next()
        ks = ks_r.next()
        for which, (Wt, src) in enumerate(((Wq, xc), (Wk, xc), (Wv, xm))):
            for h in range(4):
                for oc in range(4):
                    ps = K.ps[pi % 4]
                    pi += 1
                    for dc in range(4):
                        p.mm(ps[:, 0:n], Wt[:, h * 4 + dc, oc * 128:(oc + 1) * 128], src[:, h * 4 + dc, 0:n],
                             start=(dc == 0), stop=(dc == 3))
                    idx = which * 16 + h * 4 + oc
                    p.copy(qkv[:, idx, 0:n], ps[:, 0:n], e=("act" if pi % 2 else "dve"))
                    if which == 1:
                        p.ts(ks[:, h * 4 + oc, 0:n], ps[:, 0:n], 512.0 ** -0.5, None, op0=OP.mult, e="dve")
        if P3L < 2:
            continue
        pg = K.ps[4]
        for c in range(48):
            p.mm(pg[0:16, 0:n], Wg[:, c, :], qkv[:, c, 0:n], start=(c == 0), stop=(c == 47))
        gT = gT_r.next()
        p.act(gT[:, 0:n], pg[0:16, 0:n], AF.Identity, bias=bg[:, 0:1])
        if P3L < 3:
            continue
        for jt in range(ntl):
            ti = t0 // 128 + jt
            pt = K.ps[5]
            p.tr(pt[:, 0:16], gT[:, jt * 128:(jt + 1) * 128], K.ident_f[0:16, 0:16])
            g16 = g16_r.next()
            p.copy(g16.v, pt[:, 0:16])
            g4 = g16.v.re("q (k h) -> q k h", k=4)
            la = li_all[:, ti, :].re("q (d h) -> q d h", d=2)
            lfv = lf_all[:, ti, :].re("q (d h) -> q d h", d=2)
            p.copy(la[:, 0, :], g4[:, 0, :])
            p.copy(la[:, 1, :], g4[:, 2, :])
            p.act(lfv[:, 0, :], g4[:, 1, :], AF.Exp, scale=-1.0)
            p.act(lfv[:, 1, :], g4[:, 3, :], AF.Exp, scale=-1.0)
            p.act(lf_all[:, ti, :], lf_all[:, ti, :], AF.Ln, bias=1.0)
            p.ts(lf_all[:, ti, :], lf_all[:, ti, :], -1.0, None, op0=OP.mult)
        if P3L < 4:
            continue
        p.dma(V(qT.sub(t0), fm(qT).ap[:, :, t0:t0 + n]), qkv[:, 0:16, 0:n])
        p.dma(V(kT.sub(t0), fm(kT).ap[:, :, t0:t0 + n]), ks[:, :, 0:n])
        if P3L < 5:
            continue
        for which, (src, s0, dst) in enumerate(((ks, 0, ktm), (qkv, 32, vtm))):
            tm = tm_r.next()
            for jt in range(ntl):
                for c4 in range(4):
                    ps = K.ps[6 + pi % 2]
                    pi += 1
                    psb = ps.v.bitcast(BF16)
                    for cc in range(4):
                        p.tr(psb[:, cc * 128:(cc + 1) * 128], src[:, s0 + c4 * 4 + cc, jt * 128:(jt + 1) * 128], K.ident_b.v)
                    p.copy(tm[:, jt, c4 * 512:(c4 + 1) * 512], psb[:, 0:512], e=("act" if pi % 2 else "dve"))
            p.dma(V(dst.sub(t0), tmv(dst).ap[:, t0 // 128:(t0 + n) // 128, :]), tm[:, 0:ntl, :])
    ph.close()
    if getattr(K, "dbg_stop", 0) == 3:
        lph.close()
        return

    ph = Phase(p)
    S32 = ph.sb("ml_S32", [128, 16, 512], F32)
    Sbf = ph.sb("ml_Sbf", [128, 16, 512], BF16)
    n32 = ph.sb("ml_n32", [128, 16], F32)
    nbf = ph.sb("ml_nbf", [128, 16], BF16)
    q_r = ph.rot("ms_q", [128, 16, 128], BF16, 2)
    k_r = ph.rot("ms_k", [128, 16, 128], BF16, 2)
    kt_r = ph.rot("ms_kt", [128, 2048], BF16, 2)
    vt_r = ph.rot("ms_vt", [128, 2048], BF16, 2)
    X2_r = ph.rot("ms_X2", [128, 2048], BF16, 2)
    sm_r = ph.rot("ms_sm", [128, 4, 4], F32, 3)
    wb_r = ph.rot("ms_wb", [128, 4], BF16, 3)
    E_r = ph.rot("ms_E", [128, 4, 128], F32, 2)
    ea_r = ph.rot("ms_ea", [128, 4, 128], F32, 2)
    WT_r = ph.rot("ms_WT", [128, 4, 128], BF16, 2)
    Cp_r = ph.rot("ms_Cp", [128, 16, 128], BF16, 2)
    rd_r = ph.rot("ms_rd", [128, 4, 128], F32, 2)
    H_r = ph.rot("ms_H", [128, 16, 128], F32, 2)
    Hf_r = ph.rot("ms_Hf", [128, 16, 128], F32, 2)
    sx_r = ph.rot("ms_sx", [128, 16, 128], BF16, 2)
    sz_r = ph.rot("ms_sz", [128, 16, 128], BF16, 2)
    sq_r = ph.rot("ms_sq", [128, 16, 128], BF16, 1)
    rs_r = ph.rot("ms_rs", [128, 4, 128], F32, 1)
    yn_r = ph.rot("ms_yn", [128, 16, 128], BF16, 2)

    def run(d, chunks):
        mask = K.mask_le if d == 0 else K.mask_ge
        p.memset(S32.v, 0.0)
        p.memset(Sbf.v, 0.0, e="pool")
        p.memset(n32.v, 0.0)
        p.memset(nbf.v, 0.0, e="pool")

        def stageAB(ci):
            c0 = ci * 128
            qc = q_r.next(); kc_ = k_r.next(); kt = kt_r.next(); vt = vt_r.next()
            p.dma(qc.v, fm(qT)[:, :, c0:c0 + 128])
            p.dma(kc_.v, fm(kT)[:, :, c0:c0 + 128])
            p.dma(kt.v, ktm[c0:c0 + 128, :])
            p.dma(vt.v, vtm[c0:c0 + 128, :])
            lf = lf_all[:, ci, d * 4:(d + 1) * 4]
            lii = li_all[:, ci, d * 4:(d + 1) * 4]
            pm = K.ps[7]
            p.mm(pm[:, 0:4], mask.v, lf)
            p.mm(pm[:, 4:8], K.ones_f.v, lf)
            sm = sm_r.next()
            nb = sm[:, 1, :]; wend = sm[:, 2, :]; et = sm[:, 3, :]
            p.tt(nb, lii, pm[:, 0:4], OP.subtract)
            p.tt(wend, pm[:, 4:8], nb, OP.add)
            p.act(wend, wend, AF.Exp)
            p.act(et, pm[:, 4:8], AF.Exp)
            wb = wb_r.next()
            p.copy(wb.v, wend)
            X2 = X2_r.next()
            for h in range(4):
                p.ts(X2[:, h * 512:(h + 1) * 512], vt[:, h * 512:(h + 1) * 512], wend[:, h:h + 1], None, op0=OP.mult,
                     e=("pool" if h % 2 else "dve"))
            pa = K.ps[0]
            pav = pa.v.re("q (r l) -> q r l", r=4)
            pg = K.ps[1]
            pgv = pg.v.re("q (r l) -> q r l", r=4)
            for h in range(4):
                p.mm(pav[:, h, :], lf[:, h:h + 1].bc([128, 128]), mask.v)
            for h in range(4):
                for dc in range(4):
                    p.mm(pgv[:, h, :], kc_[:, h * 4 + dc, :], qc[:, h * 4 + dc, :], start=(dc == 0), stop=(dc == 3))
            E = E_r.next(); ea = ea_r.next()
            for h in range(4):
                p.act(E[:, h, :], pav[:, h, :], AF.Exp, bias=nb[:, h:h + 1])
            p.act(ea.v, pav, AF.Exp)
            if d == 0:
                p.aselect(E.v, E.v, [[0, 4], [1, 128]], OP.is_ge, 0.0, base=0, cm=-1)
            else:
                p.aselect(E.v, E.v, [[0, 4], [-1, 128]], OP.is_ge, 0.0, base=0, cm=1)
            WT = WT_r.next(); Cp = Cp_r.next()
            p.tt(WT.v, E.v, pgv, OP.mult)
            p.tt(Cp.v.re("q (h c) l -> q h c l", h=4), qc.v.re("q (h c) l -> q h c l", h=4),
                 ea.v.unsq(2).bc([128, 4, 4, 128]), OP.mult)
            return dict(kt=kt, vt=vt, X2=X2, wb=wb, et=et, WT=WT, Cp=Cp)

        def stageC(ci, H_):
            c0 = ci * 128
            isctx = ci < nctx_t
            kt, vt, X2, wb, et, WT, Cp = H_["kt"], H_["vt"], H_["X2"], H_["wb"], H_["et"], H_["WT"], H_["Cp"]
            pd = K.ps[2]
            pdv = pd.v.re("q (r l) -> q r l", r=4)
            for h in range(4):
                p.mm(pdv[:, h, :], K.ones_b.v, WT[:, h, :], start=True, stop=False)
                for dc in range(4):
                    p.mm(pdv[:, h, :], nbf[:, h * 4 + dc:h * 4 + dc + 1].bc([128, 128]), Cp[:, h * 4 + dc, :],
                         start=False, stop=(dc == 3))
            rd = rd_r.next()
            p.ts(rd.v, pdv, -1.0, 1.0, op0=OP.mult, op1=OP.max)
            p.tt(rd.v, rd.v, pdv, OP.max)
            p.recip(rd.v, rd.v)
            Hsb = H_r.next()
            for h in range(4):
                py = K.ps[3 + h % 2]
                pyv = py.v.re("q (c l) -> q c l", c=4)
                for pc in range(4):
                    o = pyv[:, pc, :]
                    p.mm(o, vt[:, h * 512 + pc * 128:h * 512 + (pc + 1) * 128], WT[:, h, :], start=True, stop=False)
                    for dc in range(4):
                        p.mm(o, V(Sbf.sub(h), Sbf.h[:, h * 4 + dc, pc * 128:(pc + 1) * 128]), Cp[:, h * 4 + dc, :],
                             start=False, stop=(dc == 3))
                p.tt(Hsb[:, h * 4:(h + 1) * 4, :], pyv, rd[:, h, :].unsq(1).bc([128, 4, 128]), OP.mult)
                for dc in range(4):
                    pst = K.ps[5 + dc % 2]
                    p.mm(pst.v, kt[:, h * 512 + dc * 128:h * 512 + (dc + 1) * 128], X2[:, h * 512:(h + 1) * 512])
                    sg = V(S32.sub(h), S32.h[:, h * 4 + dc, :])
                    p.stt(sg, sg, et[:, h:h + 1], pst.v, OP.mult, OP.add)
                    p.copy(V(Sbf.sub(h), Sbf.h[:, h * 4 + dc, :]), sg, e=("act" if dc % 2 else "pool"))
            pn = K.ps[7]
            for h in range(4):
                for dc in range(4):
                    p.mm(pn[:, 16 + h * 4 + dc:16 + h * 4 + dc + 1], kt[:, h * 512 + dc * 128:h * 512 + (dc + 1) * 128],
                         wb[:, h:h + 1])
            p.tt(n32.v.re("q (h c) -> q h c", h=4), n32.v.re("q (h c) -> q h c", h=4), et.unsq(2).bc([128, 4, 4]), OP.mult)
            p.tt(n32.v, n32.v, pn[:, 16:32], OP.add)
            p.copy(nbf.v, n32.v)
            if d == 0:
                p.dma(V(HfT.sub(ci), fm(HfT).ap[:, :, c0:c0 + 128]), Hsb.v)
            else:
                if isctx and not K.want_ctx:
                    return
                Hf = Hf_r.next(); sx = sx_r.next(); sz = sz_r.next()
                p.dma(Hf.v, V(HfT.sub(ci), fm(HfT).ap[:, :, c0:c0 + 128]))
                p.dma(sx.v, fm(sxcT)[:, :, c0:c0 + 128])
                p.dma(sz.v, fm(szT)[:, :, c0:c0 + 128])
                p.tt(Hsb.v, Hsb.v, Hf.v, OP.add, e="pool")
                sq = sq_r.next()
                p.act(sq.v, Hsb.v, AF.Square)
                pr = K.ps[2]
                for h in range(4):
                    for pc in range(4):
                        p.mm(pr[:, h * 128:(h + 1) * 128], K.ones_b.v, sq[:, h * 4 + pc, :], start=(pc == 0), stop=(pc == 3))
                rs = rs_r.next()
                p.act(rs.v, pr.v.re("q (h l) -> q h l", h=4), AF.Sqrt, scale=1.0 / 512, bias=K.eps_t[:, 0:1])
                p.recip(rs.v, rs.v)
                p.tt(Hsb.v.re("q (h c) l -> q h c l", h=4), Hsb.v.re("q (h c) l -> q h c l", h=4),
                     rs.v.unsq(2).bc([128, 4, 4, 128]), OP.mult)
                for c in range(16):
                    p.stt(Hsb[:, c, :], Hsb[:, c, :], ngT[:, c:c + 1], sx[:, c, :], OP.mult, OP.add)
                yn = yn_r.next()
                p.tt(yn.v, Hsb.v, sz.v, OP.mult)
                p.dma(V(ynT.sub(ci), fm(ynT).ap[:, :, c0:c0 + 128]), yn.v)

        cur = stageAB(chunks[0])
        for i, ci in enumerate(chunks):
            nxt = stageAB(chunks[i + 1]) if i + 1 < len(chunks) else None
            stageC(ci, cur)
            cur = nxt

    ctx_chunks = list(range(nctx_t))
    lat_chunks = list(range(nctx_t, NTI))
    run(0, ctx_chunks + lat_chunks)
    p.barrier()
    if getattr(K, "dbg_stop", 0) != 4:
        run(1, ctx_chunks[::-1] + lat_chunks[::-1])
    ph.close()
    w_down = K.W["mlstm_w_down"]
    phase_outproj(K, ynT, 16, lambda kc: w_down[j, kc * 128:(kc + 1) * 128, :], 2)
    lph.close()


from concourse.bass_utils import run_bass_kernel_spmd

W_SHAPES = {
    "ada_w": (4, 1024, 6144), "ada_b": (4, 6144), "norm_g": (4, 2, 1024),
    "ssd_w_in": (1, 1024, 6208), "ssd_conv_w": (1, 5, 4096), "ssd_conv_b": (1, 4096), "ssd_dt_bias": (1, 2, 32),
    "ssd_a_log": (1, 2, 32), "ssd_d": (1, 32), "ssd_norm_g": (1, 2048), "ssd_w_out": (1, 2048, 1024),
    "hgrn_w_in": (1, 1024, 5120), "hgrn_lb": (2, 4, 1024), "hgrn_norm_g": (1, 1024), "hgrn_w_out": (1, 1024, 1024),
    "attn_w_qkv": (1, 1024, 1536), "attn_q_g": (1, 64), "attn_k_g": (1, 64), "attn_w_o": (1, 1024, 1024),
    "mlstm_w_up": (1, 1024, 4096), "mlstm_conv_w": (1, 5, 2048), "mlstm_conv_b": (1, 2048),
    "mlstm_w_q": (1, 4, 512, 512), "mlstm_w_k": (1, 4, 512, 512), "mlstm_w_v": (1, 4, 512, 512),
    "mlstm_w_gate": (1, 6144, 16), "mlstm_b_gate": (1, 16), "mlstm_skip": (1, 2048), "mlstm_norm_g": (1, 2048),
    "mlstm_w_down": (1, 2048, 1024),
    "ffn_w1": (2, 1024, 3584), "ffn_w3": (2, 1024, 3584), "ffn_w2": (2, 3584, 1024),
    "moe_router": (2, 1024, 8), "moe_w1": (2, 8, 1024, 3584), "moe_w3": (2, 8, 1024, 3584), "moe_w2": (2, 8, 3584, 1024),
}

LAYER_W = {
    0: ["ssd_w_in", "ssd_conv_w", "ssd_conv_b", "ssd_dt_bias", "ssd_a_log", "ssd_d", "ssd_norm_g", "ssd_w_out",
        "ffn_w1", "ffn_w3", "ffn_w2"],
    1: ["hgrn_w_in", "hgrn_lb", "hgrn_norm_g", "hgrn_w_out", "moe_router", "moe_w1", "moe_w3", "moe_w2"],
    2: ["attn_w_qkv", "attn_q_g", "attn_k_g", "attn_w_o", "ffn_w1", "ffn_w3", "ffn_w2"],
    3: ["mlstm_w_up", "mlstm_conv_w", "mlstm_conv_b", "mlstm_w_q", "mlstm_w_k", "mlstm_w_v", "mlstm_w_gate",
        "mlstm_b_gate", "mlstm_skip", "mlstm_norm_g", "mlstm_w_down", "moe_router", "moe_w1", "moe_w3", "moe_w2"],
}


def needed_weights(layers):
    names = ["ada_w", "ada_b", "norm_g"]
    for li in layers:
        for n in LAYER_W[li]:
            if n not in names:
                names.append(n)
    return names


def build(TL, layers, debug_full_out=False, stop_after=None, dbg_stop=0, split_last=False):
    nc = bass.Bass("TRN2", target_bir_lowering=False)
    p = P(nc)
    import os
    p.scopes = bool(os.environ.get('KSCOPES'))
    K = Ctx()
    K.p = p
    K.NT = NCTX + TL
    K.TL = TL
    K.FFN_BLK = 1280
    K.dbg_stop = dbg_stop
    K.MOE_BLK = 1024
    K.xin = p.dram("xin", [K.NT, D], F32, kind="ExternalInput")
    K.c_row = p.dram("c_row", [2, D], F32, kind="ExternalInput")
    K.W = {}
    for n in needed_weights(layers):
        K.W[n] = p.dram(n, list(W_SHAPES[n]), F32, kind="ExternalInput")
    K.ada_w, K.ada_b, K.norm_g = K.W["ada_w"], K.W["ada_b"], K.W["norm_g"]
    K.debug_full_out = debug_full_out
    _NOCTX[0] = False
    K.split_last = split_last
    if split_last:
        K.selv = p.dram("selv", [128, 2], F32, kind="ExternalInput")
    K.xout = p.dram("xout", [K.NT if debug_full_out else (TL // 2 if split_last else TL), D], F32, kind="ExternalOutput")
    K.xT = p.dram("xT", [D, K.NT], F32)
    setup_consts(K)
    phase_load_input(K)
    for li in layers:
        K.want_ctx = li < 3
        phase_mod(K, li)
        K.modT, K.gm = K.modTs[li], K.gms[li]
        if stop_after == "mod":
            break
        if li == 0:
            ssd_layer(K, li, 0)
        elif li == 1:
            hgrn_layer(K, li, 0)
        elif li == 2:
            attn_layer(K, li, 0)
        else:
            mlstm_layer(K, li, 0)
        if stop_after == "mixer":
            break
        if split_last and li == layers[-1]:
            phase_select_half(K)
        if li % 2 == 0:
            w1, w3, w2 = K.W["ffn_w1"], K.W["ffn_w3"], K.W["ffn_w2"]
            jj = li // 2
            phase_ffn(K, [None],
                      lambda e, c0, n_: w1[jj].re("(c q) f -> q c f", q=128)[:, :, c0:c0 + n_],
                      lambda e, c0, n_: w3[jj].re("(c q) f -> q c f", q=128)[:, :, c0:c0 + n_],
                      lambda e, k0, nk: w2[jj].re("(c q) o -> q c o", q=128)[:, k0:k0 + nk, :])
        else:
            w1, w3, w2 = K.W["moe_w1"], K.W["moe_w3"], K.W["moe_w2"]
            jj = li // 2
            phase_ffn(K, list(range(8)),
                      lambda e, c0, n_: w1[jj, e].re("(c q) f -> q c f", q=128)[:, :, c0:c0 + n_],
                      lambda e, c0, n_: w3[jj, e].re("(c q) f -> q c f", q=128)[:, :, c0:c0 + n_],
                      lambda e, k0, nk: w2[jj, e].re("(c q) o -> q c o", q=128)[:, k0:k0 + nk, :],
                      router=K.W["moe_router"][jj])
    phase_store_output(K)
    p.finish()
    _NOCTX[0] = False
    return nc, K


def run_cores(nc, layers, xins, crows, weights):
    names = needed_weights(layers)
    in_maps = []
    for xi, cr in zip(xins, crows):
        m = {"xin": xi, "c_row": cr}
        for n in names:
            m[n] = weights[n]
        in_maps.append(m)
    res = run_bass_kernel_spmd(nc, in_maps, core_ids=list(range(len(in_maps))))
    return [r["xout"] for r in res.results]


def kernel(**inputs):
    x = np.asarray(inputs["x"], dtype=np.float32)
    c = np.asarray(inputs["c"], dtype=np.float32)
    ctx = np.asarray(inputs["ctx"], dtype=np.float32)
    c_ctx = np.asarray(inputs["c_ctx"], dtype=np.float32)
    B, T, _ = x.shape
    layers = [0, 1, 2, 3]
    nc, K = build(T, layers, split_last=True)
    names = needed_weights(layers)
    weights = {n: np.ascontiguousarray(np.asarray(inputs[n], dtype=np.float32)) for n in names}
    in_maps = []
    for i in range(2 * B):
        b, half = i % B, i // B
        m = {"xin": np.ascontiguousarray(np.concatenate([ctx[b], x[b]], axis=0)),
             "c_row": np.ascontiguousarray(np.stack([c[b], c_ctx], axis=0)),
             "selv": np.ascontiguousarray(np.tile(np.array([[1.0 - half, float(half)]], np.float32), (128, 1)))}
        for n in names:
            m[n] = weights[n]
        in_maps.append(m)
    res = run_bass_kernel_spmd(nc, in_maps, core_ids=list(range(2 * B)))
    out = np.empty((B, T, D), np.float32)
    H = T // 2
    for i in range(2 * B):
        b, half = i % B, i // B
        out[b, half * H:(half + 1) * H] = res.results[i]["xout"]
    return out
```

```python
import numpy as np
import concourse.bass as bass
import concourse.mybir as mybir

F32 = mybir.dt.float32
BF16 = mybir.dt.bfloat16
I32 = mybir.dt.int32
AF = mybir.ActivationFunctionType
OP = mybir.AluOpType
AX = mybir.AxisListType


class V:
    __slots__ = ("t", "ap")

    def __init__(self, t, ap):
        self.t = t
        self.ap = ap

    def __getitem__(self, idx):
        return V(self.t, self.ap[idx])

    def re(self, pat, **kw):
        return V(self.t, self.ap.rearrange(pat, **kw))

    def bc(self, shape):
        return V(self.t, self.ap.to_broadcast(shape))

    def bitcast(self, dt):
        return V(self.t, self.ap.bitcast(dt))

    def unsq(self, d):
        return V(self.t, self.ap.unsqueeze(d))


class Tile:
    def __init__(self, h, name, const=False):
        self.h = h
        self.name = name
        self.last_w = None
        self.readers = {}
        self.const = const

    def __getitem__(self, idx):
        return V(self, self.h[idx])

    @property
    def v(self):
        return V(self, self.h.ap())


class P:
    EPOCH = 16000
    NDMA = 24

    def __init__(self, nc):
        self.nc = nc
        self.eng = {"pe": nc.tensor, "act": nc.scalar, "dve": nc.vector, "pool": nc.gpsimd, "sp": nc.sync}
        self.cnt = {e: 0 for e in self.eng}
        self.esems = {e: [] for e in self.eng}
        self.waited = {e: {} for e in self.eng}
        self.dma_sems = []
        self.dma_cnt = []
        self.dma_last = []
        self.dma_rr = 0
        self.nsem = 0
        self.ninst = 0
        self.nwait = 0
        self.uid = 0
        self.psum_tiles = []

    def name(self, n):
        self.uid += 1
        return f"{n}_{self.uid}"

    def sb(self, name, shape, dt=F32):
        return Tile(self.nc.alloc_sbuf_tensor(self.name(name), list(shape), dt), name)

    def ps(self, name, shape, dt=F32):
        t = Tile(self.nc.alloc_psum_tensor(self.name(name), list(shape), dt), name)
        t.psum = True
        return t

    def dram(self, name, shape, dt=F32, kind="Internal"):
        h = self.nc.dram_tensor(name, list(shape), dt, kind=kind)
        return Tile(h, name, const=(kind == "ExternalInput"))

    def sem(self, name):
        self.nsem += 1
        return self.nc.alloc_semaphore(self.name(name))

    def _next_token(self, e):
        n = self.cnt[e]
        ep, val = divmod(n, self.EPOCH)
        while len(self.esems[e]) <= ep:
            self.esems[e].append(self.sem(f"s_{e}"))
        self.cnt[e] = n + 1
        return (self.esems[e][ep], val + 1, e, (e, ep))

    def _dma_token(self, e):
        if len(self.dma_sems) < self.NDMA:
            self.dma_sems.append(self.sem("s_dma"))
            self.dma_cnt.append(0)
            self.dma_last.append(None)
        k = self.dma_rr
        self.dma_rr = (self.dma_rr + 1) % self.NDMA
        if k >= len(self.dma_sems):
            k = len(self.dma_sems) - 1
        if self.dma_last[k] is not None:
            self._wait(e, self.dma_last[k])
        self.dma_cnt[k] += 1
        tok = (self.dma_sems[k], 16 * self.dma_cnt[k], "dma", ("dma", k))
        self.dma_last[k] = tok
        return tok

    def _wait(self, e, tok):
        sem, val, te, key = tok
        if te == e and e == "pe":
            return
        w = self.waited[e]
        if w.get(key, 0) >= val:
            return
        w[key] = val
        self.eng[e].wait_ge(sem, val)
        self.nwait += 1

    def emit(self, e, reads, writes, fn, dma=False):
        if e != "pe" and any(getattr(v.t, "psum", False) for v in reads):
            writes = list(writes) + [v for v in reads if getattr(v.t, "psum", False)]
            reads = [v for v in reads if not getattr(v.t, "psum", False)]
        for v in reads:
            t = v.t
            if t.last_w is not None:
                self._wait(e, t.last_w)
        for v in writes:
            t = v.t
            if t.last_w is not None:
                self._wait(e, t.last_w)
            for tok in t.readers.values():
                self._wait(e, tok)
        tok = self._dma_token(e) if dma else self._next_token(e)
        inst = fn()
        inst.then_inc(tok[0], 16 if dma else 1)
        self.ninst += 1
        for v in reads:
            t = v.t
            if not t.const:
                t.readers[tok[3]] = tok
        for v in writes:
            t = v.t
            t.last_w = tok
            t.readers = {}
        return tok

    def mm(self, out, lhsT, rhs, start=True, stop=True, **kw):
        return self.emit("pe", [lhsT, rhs], [out],
                         lambda: self.nc.tensor.matmul(out.ap, lhsT.ap, rhs.ap, start=start, stop=stop, **kw))

    def tr(self, out, in_, ident):
        return self.emit("pe", [in_, ident], [out],
                         lambda: self.nc.tensor.transpose(out.ap, in_.ap, ident.ap))

    def act(self, out, in_, func, bias=None, scale=None, accum=None):
        reads = [in_]
        kw = {}
        if bias is not None:
            if isinstance(bias, V):
                reads.append(bias); kw["bias"] = bias.ap
            else:
                kw["bias"] = bias
        if scale is not None:
            if isinstance(scale, V):
                reads.append(scale); kw["scale"] = scale.ap
            else:
                kw["scale"] = scale
        writes = [out]
        if accum is not None:
            writes.append(accum); kw["accum_out"] = accum.ap
        return self.emit("act", reads, writes, lambda: self.nc.scalar.activation(out.ap, in_.ap, func, **kw))

    def tt(self, out, a, b, op, e="dve"):
        return self.emit(e, [a, b], [out], lambda: self.eng[e].tensor_tensor(out.ap, a.ap, b.ap, op))

    def ts(self, out, a, s1, s2=None, op0=OP.mult, op1=None, e="dve", accum=None):
        reads = [a]
        a1 = s1.ap if isinstance(s1, V) else s1
        a2 = s2.ap if isinstance(s2, V) else s2
        if isinstance(s1, V): reads.append(s1)
        if isinstance(s2, V): reads.append(s2)
        kw = {}
        writes = [out]
        if e == "pool" and op1 is None and s2 is None:
            if op0 == OP.mult:
                a2, op1 = 0.0, OP.add
            elif op0 == OP.add:
                a2, op1 = 1.0, OP.mult
        if op1 is not None: kw["op1"] = op1
        if accum is not None:
            kw["accum_out"] = accum.ap; writes.append(accum)
        return self.emit(e, reads, writes, lambda: self.eng[e].tensor_scalar(out.ap, a.ap, a1, a2, op0, **kw))

    def stt(self, out, a, s, b, op0, op1, accum=None):
        reads = [a, b]
        a1 = s.ap if isinstance(s, V) else s
        if isinstance(s, V): reads.append(s)
        kw = {}
        writes = [out]
        if accum is not None:
            kw["accum_out"] = accum.ap; writes.append(accum)
        return self.emit("dve", reads, writes,
                         lambda: self.nc.vector.scalar_tensor_tensor(out.ap, a.ap, a1, b.ap, op0, op1, **kw))

    def copy(self, out, in_, e="dve"):
        if e == "act":
            return self.emit("act", [in_], [out], lambda: self.nc.scalar.copy(out.ap, in_.ap))
        if e == "pool":
            return self.emit(e, [in_], [out],
                             lambda: self.eng[e].tensor_scalar(out.ap, in_.ap, 1.0, 0.0, OP.mult, op1=OP.add))
        return self.emit(e, [in_], [out], lambda: self.eng[e].tensor_copy(out.ap, in_.ap))

    def memset(self, out, val, e="dve"):
        return self.emit(e, [], [out], lambda: self.eng[e].memset(out.ap, val))

    def reduce(self, out, in_, op=OP.add, axis=AX.X, **kw):
        return self.emit("dve", [in_], [out], lambda: self.nc.vector.tensor_reduce(out.ap, in_.ap, axis, op, **kw))

    def recip(self, out, in_):
        return self.emit("dve", [in_], [out], lambda: self.nc.vector.reciprocal(out.ap, in_.ap))

    def scan(self, out, d0, d1, initial, op0, op1):
        reads = [d0, d1]
        ini = initial.ap if isinstance(initial, V) else initial
        if isinstance(initial, V): reads.append(initial)
        return self.emit("dve", reads, [out],
                         lambda: self.nc.vector.tensor_tensor_scan(out.ap, d0.ap, d1.ap, ini, op0, op1))

    def iota(self, out, pattern, base=0, cm=0):
        return self.emit("pool", [], [out], lambda: self.nc.gpsimd.iota(
            out.ap, pattern, base=base, channel_multiplier=cm, allow_small_or_imprecise_dtypes=True))

    def aselect(self, out, in_, pattern, cmp, fill, base=0, cm=0):
        return self.emit("pool", [in_], [out], lambda: self.nc.gpsimd.affine_select(
            out.ap, in_.ap, pattern, cmp, fill, base=base, channel_multiplier=cm))

    def dma(self, out, in_, q="sp", **kw):
        return self.emit(q, [in_], [out], lambda: self.eng[q].dma_start(out.ap, in_.ap, **kw), dma=True)

    def finish(self):
        toks = []
        for e in self.eng:
            if self.cnt[e] > 0:
                n = self.cnt[e] - 1
                ep, val = divmod(n, self.EPOCH)
                toks.append((self.esems[e][ep], val + 1, e, (e, ep)))
        toks += [t for t in self.dma_last if t is not None]
        for tok in toks:
            sem, val, te, key = tok
            if self.waited["sp"].get(key, 0) < val:
                self.nc.sync.wait_ge(sem, val)


import contextlib

D = 1024
KC = 8
NCTX = 256
EPS = 1e-6


class Phase:
    def __init__(self, p):
        import sys
        self.p = p
        self.st = contextlib.ExitStack()
        if getattr(p, "scopes", False):
            f = sys._getframe(1)
            p.uid += 1
            self.st.enter_context(p.nc.named_scope(f"{f.f_code.co_name}_{f.f_lineno}_{p.uid}"))

    def sb(self, name, shape, dt=F32):
        h = self.st.enter_context(self.p.nc.sbuf_tensor(self.p.name(name), list(shape), dt))
        return Tile(h, name)

    def rot(self, name, shape, dt=F32, n=2):
        return Rot([self.sb(f"{name}{i}", shape, dt) for i in range(n)])

    def close(self):
        self.p.barrier()
        self.st.close()


class Rot:
    def __init__(self, tiles):
        self.tiles = tiles
        self.i = 0

    def next(self):
        t = self.tiles[self.i % len(self.tiles)]
        self.i += 1
        return t


def _barrier(self):
    toks = []
    for e in self.eng:
        if self.cnt[e] > 0:
            n = self.cnt[e] - 1
            ep, val = divmod(n, self.EPOCH)
            toks.append((self.esems[e][ep], val + 1, e, (e, ep)))
    toks += [t for t in self.dma_last if t is not None]
    for e in self.eng:
        for tok in toks:
            if tok[2] != e:
                self._wait(e, tok)


P.barrier = _barrier


def _sub(self, key):
    if not hasattr(self, "_subs"):
        self._subs = {}
    if key not in self._subs:
        self._subs[key] = Tile(self.h, f"{self.name}[{key}]", const=self.const)
    return self._subs[key]


Tile.sub = _sub


class Ctx:
    pass


def setup_consts(K):
    p = K.p
    K.ident_f = p.sb("ident_f", [128, 128], F32)
    K.ident_b = p.sb("ident_b", [128, 128], BF16)
    K.ones_f = p.sb("ones_f", [128, 128], F32)
    K.ones_b = p.sb("ones_b", [128, 128], BF16)
    K.mask_le = p.sb("mask_le", [128, 128], F32)
    K.mask_ge = p.sb("mask_ge", [128, 128], F32)
    p.memset(K.ones_f.v, 1.0)
    p.memset(K.ones_b.v, 1.0)
    p.aselect(K.ident_f.v, K.ones_f.v, [[1, 128]], OP.is_equal, 0.0, base=0, cm=-1)
    p.copy(K.ident_b.v, K.ident_f.v)
    p.aselect(K.mask_le.v, K.ones_f.v, [[1, 128]], OP.is_ge, 0.0, base=0, cm=-1)
    p.aselect(K.mask_ge.v, K.ones_f.v, [[-1, 128]], OP.is_ge, 0.0, base=0, cm=1)
    K.mask_le_b = p.sb("mask_le_b", [128, 128], BF16)
    K.mask_ge_b = p.sb("mask_ge_b", [128, 128], BF16)
    p.copy(K.mask_le_b.v, K.mask_le.v)
    p.copy(K.mask_ge_b.v, K.mask_ge.v)
    for t in (K.ident_f, K.ident_b, K.ones_f, K.ones_b, K.mask_le, K.mask_ge, K.mask_le_b, K.mask_ge_b):
        t.const = True
    K.ps = [p.ps(f"ps{i}", [128, 512], F32) for i in range(8)]
    K.eps_t = p.sb("eps_t", [128, 1], F32)
    p.memset(K.eps_t.v, EPS)
    K.eps_t.const = True
    K.stage = p.sb("stage", [128, 128], F32)
    K.scT = p.sb("scT", [128, KC, 2], F32)
    K.modTs = [p.sb(f"modT{i}", [128, 48, 2], F32) for i in range(4)]
    K.gms = [p.sb(f"gm{i}", [128, 2, KC, 2], F32) for i in range(4)]
    K.have_scT = False


def load_vec_fm(K, owner, dram_v, n, name):
    p = K.p
    out = owner.sb(name, [128, n], F32)
    done = 0
    while done < n:
        m = min(128, n - done)
        st = K.stage
        p.dma(st[0:m, :], dram_v[done:done + m, :])
        ps = K.ps[7]
        p.tr(ps[:, 0:m], st[0:m, :], K.ident_f[0:m, 0:m])
        p.copy(out[:, done:done + m], ps[:, 0:m])
        done += m
    return out


_NOCTX = [False]


def sub_blocks(NT):
    if _NOCTX[0]:
        subs = []
        t = 0
    else:
        subs = [(0, NCTX, 1)]
        t = NCTX
    while t < NT:
        n = min(512, NT - t)
        subs.append((t, n, 0))
        t += n
    return subs


def phase_load_input(K):
    p = K.p
    ph = Phase(p)
    xt_r = ph.rot("li_x", [128, 4, D], F32)
    xs_r = ph.rot("li_xs", [128, KC, 512], F32)
    xTv = K.xT.v.re("(c q) t -> q c t", q=128)
    for (t0, n, _) in sub_blocks(K.NT):
        nj = n // 128
        xt = xt_r.next()
        p.dma(xt[:, 0:nj, :], K.xin[t0:t0 + n, :].re("(j q) f -> q j f", q=128))
        xs = xs_r.next()
        for c in range(KC):
            ps = K.ps[c % 4]
            for j in range(nj):
                p.tr(ps[:, j * 128:(j + 1) * 128], xt[:, j, c * 128:(c + 1) * 128], K.ident_f.v)
            p.copy(xs[:, c, 0:n], ps[:, 0:n], e=("act" if c % 2 else "dve"))
        p.dma(V(K.xT.sub(t0), xTv.ap[:, :, t0:t0 + n]), xs[:, :, 0:n])
    ph.close()


def phase_mod(K, li):
    p = K.p
    ph = Phase(p)
    if not K.have_scT:
        K.have_scT = True
        cr = ph.sb("c_row", [2, D], F32)
        p.dma(cr.v, K.c_row.v)
        ps = K.ps[7]
        for c in range(KC):
            p.tr(ps[:, 2 * c:2 * c + 2], cr[0:2, c * 128:(c + 1) * 128], K.ident_f[0:2, 0:2])
        p.act(K.scT.v.re("q c r -> q (c r)"), ps[:, 0:16], AF.Silu)
    ab = load_vec_fm(K, ph, K.ada_b[li].re("(n q) -> n q", q=128), 48, f"ada_b{li}")
    modT = K.modTs[li]
    w_r = ph.rot("ada_w", [128, KC, 1024], F32)
    for j in range(6):
        w = w_r.next()
        for kc in range(KC):
            p.dma(w[:, kc, :], K.ada_w[li, kc * 128:(kc + 1) * 128, j * 1024:(j + 1) * 1024])
        ps = K.ps[j % 2]
        for c in range(KC):
            for kc in range(KC):
                p.mm(ps[:, 2 * c:2 * c + 2], w[:, kc, c * 128:(c + 1) * 128], K.scT[:, kc, :],
                     start=(kc == 0), stop=(kc == KC - 1))
        p.tt(modT[:, j * 8:(j + 1) * 8, :], ps[:, 0:16].re("q (c r) -> q c r", r=2),
             ab[:, j * 8:(j + 1) * 8].unsq(2).bc([128, 8, 2]), OP.add)
    ng = load_vec_fm(K, ph, K.norm_g[li].re("a (n q) -> (a n) q", q=128), 16, f"ng{li}")
    gm = K.gms[li]
    for a, j in ((0, 1), (1, 4)):
        p.ts(gm[:, a, :, :], modT[:, j * 8:(j + 1) * 8, :], 1.0, None, op0=OP.add)
        p.tt(gm[:, a, :, :], gm[:, a, :, :], ng[:, a * 8:(a + 1) * 8].unsq(2).bc([128, 8, 2]), OP.mult)
    ph.close()
    modT.const = True
    gm.const = True
    K.modT = modT
    K.gm = gm


def norm_block(K, ph, bufs, t0, n, which, isctx, want_f32=False):
    p = K.p
    xt = bufs["xt"].next()
    p.dma(xt[:, :, 0:n], V(K.xT.sub(t0), K.xT.v.re("(c q) t -> q c t", q=128).ap[:, :, t0:t0 + n]))
    sq = bufs["sq"].next()
    p.act(sq[:, :, 0:n], xt[:, :, 0:n], AF.Square)
    ps = K.ps[6]
    for c in range(KC):
        p.mm(ps[:, 0:n], K.ones_b.v, sq[:, c, 0:n], start=(c == 0), stop=(c == KC - 1))
    rs = bufs["rs"].next()
    p.act(rs[:, 0:n], ps[:, 0:n], AF.Sqrt, scale=1.0 / D, bias=K.eps_t[:, 0:1])
    p.recip(rs[:, 0:n], rs[:, 0:n])
    hT = bufs["hT"].next()
    shj = 0 if which == 0 else 3
    hf = bufs["hf"].next() if want_f32 else None
    for c in range(KC):
        tmp = bufs["tmp"].next()
        p.stt(tmp[:, 0:n], xt[:, c, 0:n], K.gm[:, which, c, isctx:isctx + 1], rs[:, 0:n], OP.mult, OP.mult)
        p.act(hT[:, c, 0:n], tmp[:, 0:n], AF.Identity, bias=K.modT[:, shj * 8 + c, isctx:isctx + 1])
        if want_f32:
            p.ts(hf[:, c, 0:n], tmp[:, 0:n], K.modT[:, shj * 8 + c, isctx:isctx + 1], None, op0=OP.add, e="pool")
    return xt, hT, hf


def norm_bufs(ph, want_f32=False, nbuf=2):
    b = {
        "xt": ph.rot("nb_xt", [128, KC, 512], F32, nbuf),
        "sq": ph.rot("nb_sq", [128, KC, 512], BF16, 1),
        "rs": ph.rot("nb_rs", [128, 512], F32, 2),
        "hT": ph.rot("nb_hT", [128, KC, 512], BF16, nbuf),
        "tmp": ph.rot("nb_tmp", [128, 512], F32, 3),
    }
    if want_f32:
        b["hf"] = ph.rot("nb_hf", [128, KC, 512], F32, 1)
    return b


def load_w_bf16(K, dst, src_rows_fn, kcs, ncols, q="pool"):
    p = K.p
    for kc in range(kcs):
        p.dma(dst[:, kc, 0:ncols], src_rows_fn(kc), q=q, max_dma_last_dim=2048 * 4)


def phase_proj(K, which, wfn, ncols, sink, extra=None, want_f32=False, setup=None):
    p = K.p
    ph = Phase(p)
    W = ph.sb("pw", [128, KC, ncols], BF16)
    load_w_bf16(K, W, wfn, KC, ncols)
    bufs = norm_bufs(ph, want_f32, nbuf=2)
    st = {"ph": ph, "W": W}
    if setup is not None:
        setup(st)
    pi = 0
    for (t0, n, isctx) in sub_blocks(K.NT):
        xt, hT, hf = norm_block(K, ph, bufs, t0, n, which, isctx, want_f32)
        for j in range((ncols + 127) // 128):
            m = min(128, ncols - j * 128)
            if sink is None or m < 128:
                continue
            ps = K.ps[pi % 4]
            pi += 1
            for kc in range(KC):
                p.mm(ps[0:m, 0:n], W[:, kc, j * 128:j * 128 + m], hT[:, kc, 0:n], start=(kc == 0), stop=(kc == KC - 1))
            sink(st, j, ps, t0, n, isctx)
        if extra is not None:
            extra(st, hT, hf, t0, n, isctx)
    ph.close()


def phase_outproj(K, srcT, kin_c, wfn, gate_j, row_scale=None):
    p = K.p
    ph = Phase(p)
    W = ph.sb("ow", [128, kin_c, D], BF16)
    load_w_bf16(K, W, wfn, kin_c, D)
    if row_scale is not None:
        for kc in range(kin_c):
            p.ts(W[:, kc, :], W[:, kc, :], row_scale[:, kc:kc + 1], None, op0=OP.mult, e="pool")
    src_r = ph.rot("op_src", [128, kin_c, 512], BF16, 2)
    xt_r = ph.rot("op_xt", [128, KC, 512], F32, 2)
    xTv = K.xT.v.re("(c q) t -> q c t", q=128)
    sv = srcT.v.re("(c q) t -> q c t", q=128)
    pi = 0
    for (t0, n, isctx) in sub_blocks(K.NT):
        if isctx and not K.want_ctx:
            continue
        src = src_r.next()
        p.dma(src[:, :, 0:n], V(srcT.sub(t0), sv.ap[:, :, t0:t0 + n]))
        xt = xt_r.next()
        p.dma(xt[:, :, 0:n], V(K.xT.sub(t0), xTv.ap[:, :, t0:t0 + n]))
        for o in range(KC):
            ps = K.ps[pi % 4]
            pi += 1
            for kc in range(kin_c):
                p.mm(ps[:, 0:n], W[:, kc, o * 128:(o + 1) * 128], src[:, kc, 0:n], start=(kc == 0), stop=(kc == kin_c - 1))
            p.stt(xt[:, o, 0:n], ps[:, 0:n], K.modT[:, gate_j * 8 + o, isctx:isctx + 1], xt[:, o, 0:n], OP.mult, OP.add)
        p.dma(V(K.xT.sub(t0), xTv.ap[:, :, t0:t0 + n]), xt[:, :, 0:n])
    ph.close()


FF = 3584
FFC = 28


def ffn_blocks(K, BLK):
    subs = [s for s in sub_blocks(K.NT) if (K.want_ctx or not s[2])]
    blocks = []
    cur = []
    tot = 0
    for s in subs:
        if tot + s[1] > BLK:
            blocks.append(cur)
            cur, tot = [], 0
        cur.append(s)
        tot += s[1]
    if cur:
        blocks.append(cur)
    return blocks


def phase_ffn(K, experts, w1fn, w3fn, w2fn, router=None):
    p = K.p
    ph = Phase(p)
    moe = router is not None
    BLK = K.FFN_BLK
    NS = getattr(K, "FFN_NS", 4)
    FS = FF // NS
    FK = FS // 128
    hTb = ph.sb("f_hT", [128, KC, BLK], BF16)
    acc = ph.sb("f_acc", [128, KC, BLK], F32)
    xTv = K.xT.v.re("(c q) t -> q c t", q=128)
    if moe:
        cbT = ph.sb("f_cbT", [8, BLK], F32)
        cbE = ph.sb("f_cbE", [128, BLK], F32)
        wr = ph.sb("f_wr", [128, KC, 8], F32)
        p.dma(wr.v, router.re("(c q) e -> q c e", q=128))
        sel = ph.sb("f_sel", [8, 8, 128], F32)
        p.memset(sel.v, 1.0)
        p.aselect(sel.v, sel.v, [[1, 8], [0, 128]], OP.is_equal, 0.0, base=0, cm=-1)
        lg = ph.sb("f_lg", [128, 8], F32)
        m8 = ph.sb("f_m8", [128, 8], F32)
        w12 = ph.sb("f_w12", [128, 4], F32)
        cbm = ph.sb("f_cbm", [128, 8], F32)
        cb2 = ph.sb("f_cb2", [128, 8], F32)
    pi = 0
    for blk in ffn_blocks(K, BLK):
        b0 = blk[0][0]
        ph1 = Phase(p)
        bufs = norm_bufs(ph1, want_f32=moe, nbuf=(1 if moe else 2))
        for (t0, n, isctx) in blk:
            o0 = t0 - b0
            xt, hT, hf = norm_block(K, ph1, bufs, t0, n, 1, isctx, want_f32=moe)
            p.copy(hTb[:, :, o0:o0 + n], hT[:, :, 0:n], e="act")
            if moe:
                for jt in range(n // 128):
                    psl = K.ps[7]
                    for kc in range(KC):
                        p.mm(psl[:, 0:8], hf[:, kc, jt * 128:(jt + 1) * 128], wr[:, kc, :], start=(kc == 0), stop=(kc == KC - 1))
                    p.copy(lg.v, psl[:, 0:8])
                    p.emit("dve", [lg.v], [m8.v], lambda: p.nc.vector.max(m8.v.ap, lg.v.ap))
                    p.tt(w12[:, 0:1], m8[:, 0:1], m8[:, 1:2], OP.subtract)
                    p.act(w12[:, 1:2], w12[:, 0:1], AF.Sigmoid)
                    p.ts(w12[:, 2:3], w12[:, 1:2], -1.0, 1.0, op0=OP.mult, op1=OP.add)
                    p.ts(cbm.v, lg.v, m8[:, 0:1], w12[:, 1:2], op0=OP.is_equal, op1=OP.mult)
                    p.ts(cb2.v, lg.v, m8[:, 1:2], w12[:, 2:3], op0=OP.is_equal, op1=OP.mult)
                    p.tt(cbm.v, cbm.v, cb2.v, OP.add)
                    pst = K.ps[6]
                    p.tr(pst[0:8, 0:128], cbm.v, K.ident_f.v)
                    p.copy(cbT[:, o0 + jt * 128:o0 + (jt + 1) * 128], pst[0:8, 0:128])
        ph1.close()
        ph2 = Phase(p)
        g = ph2.sb("f_g", [128, FK, BLK], BF16)
        w1_r = ph2.rot("f_w1", [128, KC, FS], BF16, 2)
        w3_r = ph2.rot("f_w3", [128, KC, FS], BF16, 2)
        w2_r = ph2.rot("f_w2", [128, FK, D], BF16, 2)
        su_r = ph2.rot("f_su", [128, 512], F32, 2)
        tm_r = ph2.rot("f_tm", [128, 512], F32, 2)
        xt_r = ph2.rot("f_xt", [128, 512], F32, 2)
        first = True
        for e in experts:
            if moe:
                for (t0, n, isctx) in blk:
                    o0 = t0 - b0
                    pc = K.ps[6]
                    p.mm(pc[:, 0:n], sel[:, e, :], cbT[:, o0:o0 + n])
                    p.copy(cbE[:, o0:o0 + n], pc[:, 0:n], e="act")
            for s_ in range(NS):
                w1 = w1_r.next(); w3 = w3_r.next(); w2 = w2_r.next()
                p.dma(w1.v, w1fn(e, s_ * FS, FS), q="pool", max_dma_last_dim=2048 * 4)
                p.dma(w3.v, w3fn(e, s_ * FS, FS), q="pool", max_dma_last_dim=2048 * 4)
                p.dma(w2.v, w2fn(e, s_ * FK, FK), q="pool", max_dma_last_dim=2048 * 4)
                for j in range(FK):
                    for (t0, n, isctx) in blk:
                        o0 = t0 - b0
                        pu = K.ps[pi % 2]
                        pv = K.ps[2 + pi % 2]
                        pi += 1
                        for kc in range(KC):
                            p.mm(pu[:, 0:n], w1[:, kc, j * 128:(j + 1) * 128], hTb[:, kc, o0:o0 + n], start=(kc == 0), stop=(kc == KC - 1))
                        for kc in range(KC):
                            p.mm(pv[:, 0:n], w3[:, kc, j * 128:(j + 1) * 128], hTb[:, kc, o0:o0 + n], start=(kc == 0), stop=(kc == KC - 1))
                        su = su_r.next()
                        p.act(su[:, 0:n], pu[:, 0:n], AF.Silu)
                        p.tt(g[:, j, o0:o0 + n], su[:, 0:n], pv[:, 0:n], OP.mult)
                for o in range(KC):
                    for (t0, n, isctx) in blk:
                        o0 = t0 - b0
                        po = K.ps[4 + pi % 2]
                        pi += 1
                        for kc in range(FK):
                            p.mm(po[:, 0:n], w2[:, kc, o * 128:(o + 1) * 128], g[:, kc, o0:o0 + n], start=(kc == 0), stop=(kc == FK - 1))
                        av = acc[:, o, o0:o0 + n]
                        if not moe:
                            if first:
                                p.copy(av, po[:, 0:n])
                            else:
                                p.tt(av, av, po[:, 0:n], OP.add)
                        elif first:
                            p.tt(av, po[:, 0:n], cbE[:, o0:o0 + n], OP.mult)
                        else:
                            tm = tm_r.next()
                            p.tt(tm[:, 0:n], po[:, 0:n], cbE[:, o0:o0 + n], OP.mult)
                            p.tt(av, av, tm[:, 0:n], OP.add)
                first = False
        for (t0, n, isctx) in blk:
            o0 = t0 - b0
            for o in range(KC):
                xt = xt_r.next()
                xv = V(K.xT.sub((t0, o)), K.xT.v.ap[o * 128:(o + 1) * 128, t0:t0 + n])
                p.dma(xt[:, 0:n], xv)
                p.stt(xt[:, 0:n], acc[:, o, o0:o0 + n], K.modT[:, 5 * 8 + o, isctx:isctx + 1], xt[:, 0:n], OP.mult, OP.add)
                p.dma(xv, xt[:, 0:n])
        ph2.close()
    ph.close()


def phase_store_output(K):
    p = K.p
    ph = Phase(p)
    xs_r = ph.rot("so_xs", [128, KC, 512], F32, 2)
    xo_r = ph.rot("so_xo", [128, 4, D], F32, 2)
    xTv = K.xT.v.re("(c q) t -> q c t", q=128)
    pi = 0
    for (t0, n, isctx) in sub_blocks(K.NT):
        if isctx and not K.debug_full_out:
            continue
        off = 0 if (K.debug_full_out or _NOCTX[0]) else NCTX
        xs = xs_r.next()
        p.dma(xs[:, :, 0:n], V(K.xT.sub(t0), xTv.ap[:, :, t0:t0 + n]))
        xo = xo_r.next()
        for j in range(n // 128):
            for half in range(2):
                ps = K.ps[pi % 4]
                pi += 1
                for c4 in range(4):
                    c = half * 4 + c4
                    p.tr(ps[:, c4 * 128:(c4 + 1) * 128], xs[:, c, j * 128:(j + 1) * 128], K.ident_f.v)
                p.copy(xo[:, j, half * 512:(half + 1) * 512], ps.v, e=("act" if half else "dve"))
        p.dma(K.xout[t0 - off:t0 - off + n, :].re("(j q) f -> q j f", q=128), xo[:, 0:n // 128, :])
    ph.close()


def phase_select_half(K):
    p = K.p
    ph = Phase(p)
    H = K.TL // 2
    xH = p.dram("xH", [D, H], F32)
    sv = ph.sb("sel_sv", [128, 2], F32)
    p.dma(sv.v, K.selv.v)
    a_r = ph.rot("sel_a", [128, KC, 512], F32, 2)
    b_r = ph.rot("sel_b", [128, KC, 512], F32, 2)
    xTv = K.xT.v.re("(c q) t -> q c t", q=128)
    xHv = xH.v.re("(c q) t -> q c t", q=128)
    for t0 in range(0, H, 512):
        a = a_r.next(); b = b_r.next()
        p.dma(a.v, xTv[:, :, NCTX + t0:NCTX + t0 + 512])
        p.dma(b.v, xTv[:, :, NCTX + H + t0:NCTX + H + t0 + 512])
        p.ts(a.v, a.v, sv[:, 0:1], None, op0=OP.mult)
        p.stt(a.v, b.v, sv[:, 1:2], a.v, OP.mult, OP.add)
        p.dma(V(xH.sub(t0), xHv.ap[:, :, t0:t0 + 512]), a.v)
    ph.close()
    K.xT = xH
    K.NT = H
    _NOCTX[0] = True


SSD_INNER = 2048
SSD_H = 32
SSD_G = 8


def ssd_layer(K, li, j):
    p = K.p
    NT = K.NT
    NTI = NT // 128
    w_in = K.W["ssd_w_in"]
    szT = p.dram("ssd_szT", [2048, NT], BF16)
    xbcT = p.dram("ssd_xbcT", [4096, NT], BF16)
    Xtm = p.dram("ssd_Xtm", [NT, 2048], BF16)
    Btm = p.dram("ssd_Btm", [NT, 1024], BF16)
    BT = p.dram("ssd_BT", [1024, NT], BF16)
    CT = p.dram("ssd_CT", [1024, NT], BF16)
    dxsT = p.dram("ssd_dxsT", [2048, NT], BF16)
    YfT = p.dram("ssd_YfT", [2048, NT], F32)
    ynT = p.dram("ssd_ynT", [2048, NT], BF16)

    lph = Phase(p)
    dt_all = lph.sb("dt_all", [128, NTI, 64], F32)
    lndt_all = lph.sb("lndt_all", [128, NTI, 64], F32)
    dtA_all = lph.sb("dtA_all", [128, NTI, 64], F32)
    dtb_bc = lph.sb("dtb_bc", [128, 64], F32)
    A_bc = lph.sb("A_bc", [128, 64], F32)
    p.dma(dtb_bc.v, K.W["ssd_dt_bias"][j].re("d h -> (d h)").unsq(0).bc([128, 64]))
    p.dma(A_bc.v, K.W["ssd_a_log"][j].re("d h -> (d h)").unsq(0).bc([128, 64]))
    p.act(A_bc.v, A_bc.v, AF.Exp)
    p.ts(A_bc.v, A_bc.v, -1.0, None, op0=OP.mult)
    convw = load_vec_fm(K, lph, K.W["ssd_conv_w"][j].re("k (n q) -> (k n) q", q=128), 5 * 32, "ssd_cw")
    convb = load_vec_fm(K, lph, K.W["ssd_conv_b"][j].re("(n q) -> n q", q=128), 32, "ssd_cb")
    ngT = load_vec_fm(K, lph, K.W["ssd_norm_g"][j].re("(n q) -> n q", q=128), 16, "ssd_ng")
    dT = lph.sb("ssd_dT", [128, 16], F32)
    dv = K.W["ssd_d"][j].re("(c two) -> two c", two=2)
    p.dma(dT[0:64, :], dv[0:1, :].bc([64, 16]), allow_slow_non_contiguous=True)
    p.dma(dT[64:128, :], dv[1:2, :].bc([64, 16]), allow_slow_non_contiguous=True)

    def setupA(st):
        st["sz"] = st["ph"].rot("sz", [128, 16, 512], BF16, 1)
        st["Wdt"] = st["ph"].sb("Wdt", [128, KC, 64], BF16)
        load_w_bf16(K, st["Wdt"], lambda kc: w_in[j, kc * 128:(kc + 1) * 128, 6144:6208], KC, 64)
        st["t64"] = st["ph"].rot("t64", [128, 64], F32, 2)

    def sinkA(st, jj, ps, t0, n, isctx):
        if jj == 0:
            st["cur"] = st["sz"].next()
        p.act(st["cur"][:, jj, 0:n], ps[:, 0:n], AF.Silu)
        if jj == 15:
            p.dma(V(szT.sub(t0), szT.v.re("(c q) t -> q c t", q=128).ap[:, :, t0:t0 + n]), st["cur"][:, :, 0:n])

    def extraA(st, hT, hf, t0, n, isctx):
        for jt in range(n // 128):
            ti = t0 // 128 + jt
            ps = K.ps[5]
            for kc in range(KC):
                p.mm(ps[:, 0:64], hT[:, kc, jt * 128:(jt + 1) * 128], st["Wdt"][:, kc, :], start=(kc == 0), stop=(kc == KC - 1))
            t = st["t64"].next()
            p.tt(t.v, ps[:, 0:64], dtb_bc.v, OP.add)
            p.act(t.v, t.v, AF.Exp)
            p.act(dt_all[:, ti, :], t.v, AF.Ln, bias=1.0)
            p.act(lndt_all[:, ti, :], dt_all[:, ti, :], AF.Ln)
            p.tt(dtA_all[:, ti, :], dt_all[:, ti, :], A_bc.v, OP.mult)

    phase_proj(K, 0, lambda kc: w_in[j, kc * 128:(kc + 1) * 128, 0:2048], 2048, sinkA, extraA, setup=setupA)

    def setupB(st):
        st["xb"] = st["ph"].rot("xb", [128, 16, 512], BF16, 2)

    def sinkB(st, jj, ps, t0, n, isctx):
        if jj % 16 == 0:
            st["cur"] = st["xb"].next()
        p.copy(st["cur"][:, jj % 16, 0:n], ps[:, 0:n], e=("act" if jj % 2 else "dve"))
        if jj % 16 == 15:
            half = jj // 16
            p.dma(V(xbcT.sub((t0, half)), xbcT.v.re("(c q) t -> q c t", q=128).ap[:, half * 16:(half + 1) * 16, t0:t0 + n]),
                  st["cur"][:, :, 0:n])

    phase_proj(K, 0, lambda kc: w_in[j, kc * 128:(kc + 1) * 128, 2048:6144], 4096, sinkB, setup=setupB)
    if getattr(K, "dbg_stop", 0) == 1:
        lph.close()
        return

    ph = Phase(p)
    CB = 1024
    xin_r = ph.rot("cv_in", [128, 4, CB + 4], BF16, 2)
    acc_r = ph.rot("cv_acc", [128, CB], F32, 2)
    out_r = ph.rot("cv_out", [128, 4, CB], BF16, 2)
    dx_r = ph.rot("cv_dx", [128, 4, CB], BF16, 2)
    tm_r = ph.rot("cv_tm", [128, CB // 128, 512], BF16, 2)
    segs = [(0, NCTX), (NCTX, NT - NCTX)]
    pi = 0
    for s0, slen in segs:
        for b0 in range(0, slen, CB):
            n = min(CB, slen - b0)
            t0 = s0 + b0
            lo = 2 if b0 > 0 else 0
            hi = 2 if b0 + n < slen else 0
            for cg in range(8):
                xin = xin_r.next()
                if lo == 0:
                    p.memset(xin[:, :, 0:2], 0.0, e="pool")
                if hi == 0:
                    p.memset(xin[:, :, n + 2:n + 4], 0.0, e="pool")
                p.dma(xin[:, :, 2 - lo:n + 2 + hi],
                      xbcT.v.re("(c q) t -> q c t", q=128)[:, cg * 4:(cg + 1) * 4, t0 - lo:t0 + n + hi])
                out = out_r.next()
                for c4 in range(4):
                    cc = cg * 4 + c4
                    acc = acc_r.next()
                    p.ts(acc[:, 0:n], xin[:, c4, 0:n], convw[:, cc:cc + 1], convb[:, cc:cc + 1], op0=OP.mult, op1=OP.add)
                    for k in range(1, 5):
                        p.stt(acc[:, 0:n], xin[:, c4, k:k + n], convw[:, k * 32 + cc:k * 32 + cc + 1], acc[:, 0:n], OP.mult, OP.add)
                    p.act(out[:, c4, 0:n], acc[:, 0:n], AF.Silu)
                if cg < 4:
                    dx = dx_r.next()
                    for c4 in range(4):
                        cc = cg * 4 + c4
                        p.act(dx[:, c4, 0:n], out[:, c4, 0:n], AF.Identity, scale=dT[:, cc:cc + 1])
                    p.dma(V(dxsT.sub((t0, cg)), dxsT.v.re("(c q) t -> q c t", q=128).ap[:, cg * 4:(cg + 1) * 4, t0:t0 + n]), dx[:, :, 0:n])
                if cg < 6:
                    tm = tm_r.next()
                    for jt in range(n // 128):
                        ps = K.ps[pi % 4]
                        pi += 1
                        psb = ps.v.bitcast(BF16)
                        for c4 in range(4):
                            p.tr(psb[:, c4 * 128:(c4 + 1) * 128], out[:, c4, jt * 128:(jt + 1) * 128], K.ident_b.v)
                        p.copy(tm[:, jt, :], psb[:, 0:512], e=("act" if jt % 2 else "dve"))
                    if cg < 4:
                        dst = V(Xtm.sub((t0, cg)), Xtm.v.re("(jt q) c -> q jt c", q=128).ap[:, t0 // 128:(t0 + n) // 128, cg * 512:(cg + 1) * 512])
                    else:
                        dst = V(Btm.sub((t0, cg)), Btm.v.re("(jt q) c -> q jt c", q=128).ap[:, t0 // 128:(t0 + n) // 128, (cg - 4) * 512:(cg - 3) * 512])
                    p.dma(dst, tm[:, 0:n // 128, :])
                if 4 <= cg < 6:
                    p.dma(V(BT.sub((t0, cg)), BT.v.re("(c q) t -> q c t", q=128).ap[:, (cg - 4) * 4:(cg - 3) * 4, t0:t0 + n]), out[:, :, 0:n])
                if cg >= 6:
                    p.dma(V(CT.sub((t0, cg)), CT.v.re("(c q) t -> q c t", q=128).ap[:, (cg - 6) * 4:(cg - 5) * 4, t0:t0 + n]), out[:, :, 0:n])
    ph.close()
    if getattr(K, "dbg_stop", 0) == 2:
        lph.close()
        return

    ssd_scan(K, dict(Xtm=Xtm, Btm=Btm, BT=BT, CT=CT, dxsT=dxsT, szT=szT, YfT=YfT, ynT=ynT,
                     dtA_all=dtA_all, lndt_all=lndt_all))
    if getattr(K, "dbg_stop", 0) == 3:
        lph.close()
        return
    w_out = K.W["ssd_w_out"]
    phase_outproj(K, ynT, 16, lambda kc: w_out[j, kc * 128:(kc + 1) * 128, :], 2, row_scale=ngT)
    lph.close()


def ssd_scan(K, T):
    p = K.p
    NT = K.NT
    NTI = NT // 128
    nctx_t = NCTX // 128
    ph = Phase(p)
    S32 = ph.sb("S32", [128, 2048], F32)
    Sbf = ph.sb("Sbf", [128, 2048], BF16)
    X_r = ph.rot("sc_X", [128, 2048], BF16, 2)
    X2_r = ph.rot("sc_X2", [128, 2048], BF16, 2)
    Bt_r = ph.rot("sc_Bt", [128, 1024], BF16, 2)
    BT_r = ph.rot("sc_BT", [128, 8, 128], BF16, 2)
    CT_r = ph.rot("sc_CT", [128, 8, 128], BF16, 2)
    sm_r = ph.rot("sc_sm", [128, 4, 32], F32, 2)
    E_r = ph.rot("sc_E", [128, 4, 128], F32, 3)
    ea_r = ph.rot("sc_ea", [128, 4, 128], F32, 3)
    WT_r = ph.rot("sc_WT", [128, 4, 128], BF16, 3)
    Cp_r = ph.rot("sc_Cp", [128, 4, 128], BF16, 3)
    Y_r = ph.rot("sc_Y", [128, 16, 128], F32, 2)
    Yf_r = ph.rot("sc_Yf", [128, 16, 128], F32, 2)
    dx_r = ph.rot("sc_dx", [128, 16, 128], BF16, 2)
    sz_r = ph.rot("sc_sz", [128, 16, 128], BF16, 2)
    sq_r = ph.rot("sc_sq", [128, 16, 128], BF16, 1)
    rs_r = ph.rot("sc_rs", [128, 8, 128], F32, 1)
    yn_r = ph.rot("sc_yn", [128, 16, 128], BF16, 2)
    tmp_r = ph.rot("sc_tmp", [128, 256], F32, 2)
    fm = lambda t: t.v.re("(c q) t -> q c t", q=128)

    def run(d, chunks, fresh):
        mask = K.mask_le if d == 0 else K.mask_ge
        if fresh:
            p.memset(S32.v, 0.0)
            p.memset(Sbf.v, 0.0, e="pool")
        for ci in chunks:
            c0 = ci * 128
            X = X_r.next(); Bt = Bt_r.next(); BTc = BT_r.next(); CTc = CT_r.next()
            p.dma(X.v, T["Xtm"][c0:c0 + 128, :])
            p.dma(Bt.v, T["Btm"][c0:c0 + 128, :])
            p.dma(BTc.v, fm(T["BT"])[:, :, c0:c0 + 128])
            p.dma(CTc.v, fm(T["CT"])[:, :, c0:c0 + 128])
            dtA = T["dtA_all"][:, ci, d * 32:(d + 1) * 32]
            lndt = T["lndt_all"][:, ci, d * 32:(d + 1) * 32]
            pm = K.ps[7]
            p.mm(pm[:, 0:32], mask.v, dtA)
            p.mm(pm[:, 32:64], K.ones_f.v, dtA)
            sm = sm_r.next()
            nb = sm[:, 1, :]; wend = sm[:, 2, :]; et = sm[:, 3, :]
            p.tt(nb, lndt, pm[:, 0:32], OP.subtract)
            p.tt(wend, pm[:, 32:64], nb, OP.add)
            p.act(wend, wend, AF.Exp)
            p.act(et, pm[:, 32:64], AF.Exp)
            X2 = X2_r.next()
            p.tt(X2.v.re("q (h e) -> q h e", e=64), X.v.re("q (h e) -> q h e", e=64), wend.unsq(2).bc([128, 32, 64]), OP.mult)
            Ysb = Y_r.next()

            def stageA(g):
                pa = K.ps[g % 2]
                pav = pa.v.re("q (r l) -> q r l", r=4)
                for r in range(4):
                    h = 4 * g + r
                    p.mm(pav[:, r, :], dtA[:, h:h + 1].bc([128, 128]), mask.v)
                pg = K.ps[2 + g % 2]
                p.mm(pg[:, 0:128], BTc[:, g, :], CTc[:, g, :])

            BUF = {}

            def stageB(g):
                pa = K.ps[g % 2]
                pav = pa.v.re("q (r l) -> q r l", r=4)
                pg = K.ps[2 + g % 2]
                E = E_r.next(); ea = ea_r.next()
                for r in range(4):
                    h = 4 * g + r
                    p.act(E[:, r, :], pav[:, r, :], AF.Exp, bias=nb[:, h:h + 1])
                p.act(ea.v, pav, AF.Exp)
                if d == 0:
                    p.aselect(E.v, E.v, [[0, 4], [1, 128]], OP.is_ge, 0.0, base=0, cm=-1)
                else:
                    p.aselect(E.v, E.v, [[0, 4], [-1, 128]], OP.is_ge, 0.0, base=0, cm=1)
                WT = WT_r.next(); Cp = Cp_r.next()
                p.tt(WT.v, E.v, pg[:, 0:128].unsq(1).bc([128, 4, 128]), OP.mult)
                p.tt(Cp.v, ea.v, CTc[:, g, :].unsq(1).bc([128, 4, 128]), OP.mult)
                BUF[g] = (WT, Cp)

            def stageCD(g):
                WT, Cp = BUF.pop(g)
                py = K.ps[4 + g % 2]
                pyv = py[:, 0:256].re("q (c l) -> q c l", c=2)
                for r in range(4):
                    h = 4 * g + r
                    o = pyv[(r % 2) * 64:(r % 2) * 64 + 64, r // 2, :]
                    p.mm(o, X[:, h * 64:(h + 1) * 64], WT[:, r, :], start=True, stop=False)
                    p.mm(o, V(Sbf.sub(g), Sbf.h[:, h * 64:(h + 1) * 64]), Cp[:, r, :], start=False, stop=True)
                pst = K.ps[6 + g % 2] if g % 2 == 0 else K.ps[6]
                pst = K.ps[6]
                p.mm(pst[:, 0:256], Bt[:, g * 128:(g + 1) * 128], X2[:, g * 256:(g + 1) * 256])
                p.copy(Ysb[:, 2 * g:2 * g + 2, :], pyv, e="act")
                sg = V(S32.sub(g), S32.h[:, g * 256:(g + 1) * 256])
                p.tt(sg.re("q (r e) -> q r e", e=64), sg.re("q (r e) -> q r e", e=64),
                     et[:, 4 * g:4 * g + 4].unsq(2).bc([128, 4, 64]), OP.mult)
                p.tt(sg, sg, pst[:, 0:256], OP.add)
                p.copy(V(Sbf.sub(g), Sbf.h[:, g * 256:(g + 1) * 256]), sg, e="pool")

            stageA(0)
            stageB(0)
            stageA(1)
            for g in range(8):
                if g + 1 < 8:
                    stageB(g + 1)
                if g + 2 < 8:
                    stageA(g + 2)
                stageCD(g)
            if d == 0:
                p.dma(V(T["YfT"].sub(ci), fm(T["YfT"]).ap[:, :, c0:c0 + 128]), Ysb.v)
            else:
                Yf = Yf_r.next(); dx = dx_r.next(); sz = sz_r.next()
                p.dma(Yf.v, V(T["YfT"].sub(ci), fm(T["YfT"]).ap[:, :, c0:c0 + 128]))
                p.dma(dx.v, fm(T["dxsT"])[:, :, c0:c0 + 128])
                p.dma(sz.v, fm(T["szT"])[:, :, c0:c0 + 128])
                p.tt(Ysb.v, Ysb.v, Yf.v, OP.add)
                p.tt(Ysb.v, Ysb.v, dx.v, OP.add, e="pool")
                p.tt(Ysb.v, Ysb.v, sz.v, OP.mult)
                sq = sq_r.next()
                p.act(sq.v, Ysb.v, AF.Square)
                pr0 = K.ps[0]; pr1 = K.ps[1]
                for g in range(8):
                    pr = (pr0 if g < 4 else pr1)[:, (g % 4) * 128:(g % 4 + 1) * 128]
                    p.mm(pr, K.ones_b.v, sq[:, 2 * g, :], start=True, stop=False)
                    p.mm(pr, K.ones_b.v, sq[:, 2 * g + 1, :], start=False, stop=True)
                rs = rs_r.next()
                p.act(rs[:, 0:4, :], pr0.v.re("q (g l) -> q g l", g=4), AF.Ln, scale=1.0 / 256, bias=K.eps_t[:, 0:1])
                p.act(rs[:, 4:8, :], pr1.v.re("q (g l) -> q g l", g=4), AF.Ln, scale=1.0 / 256, bias=K.eps_t[:, 0:1])
                p.act(rs.v, rs.v, AF.Exp, scale=-0.5)
                yn = yn_r.next()
                p.tt(yn.v.re("q (g c) l -> q g c l", c=2), Ysb.v.re("q (g c) l -> q g c l", c=2),
                     rs.v.unsq(2).bc([128, 8, 2, 128]), OP.mult)
                p.dma(V(T["ynT"].sub(ci), fm(T["ynT"]).ap[:, :, c0:c0 + 128]), yn.v)

    ctx_chunks = list(range(nctx_t))
    lat_chunks = list(range(nctx_t, NTI))
    run(0, ctx_chunks + lat_chunks, True)
    p.barrier()
    run(1, ctx_chunks[::-1] + lat_chunks[::-1], True)
    ph.close()


def hgrn_layer(K, li, j):
    p = K.p
    NT = K.NT
    w_in = K.W["hgrn_w_in"]
    qT = p.dram("hg_qT", [1024, NT], BF16)
    fT = p.dram("hg_fT", [2048, NT], F32)
    sgT = p.dram("hg_sgT", [1024, NT], BF16)
    Vtm = p.dram("hg_Vtm", [NT, 1024], BF16)
    OfT = p.dram("hg_OfT", [1024, NT], F32)
    ynT = p.dram("hg_ynT", [1024, NT], BF16)
    fm = lambda t: t.v.re("(c q) t -> q c t", q=128)

    lph = Phase(p)
    ngT = load_vec_fm(K, lph, K.W["hgrn_norm_g"][j].re("(n q) -> n q", q=128), 8, "hg_ng")
    lbr = load_vec_fm(K, lph, K.W["hgrn_lb"].v.re("d l (n q) -> (d l n) q", q=128), 64, "hg_lbr")
    lb = lph.sb("hg_lb", [128, 2, 8], F32)
    oml = lph.sb("hg_oml", [128, 2, 8], F32)
    ssum = lph.sb("hg_ssum", [128, 2, 8], F32)
    p.act(lbr.v, lbr.v, AF.Exp)
    l4 = lbr.v.re("q (d l n) -> q d l n", d=2, l=4)
    p.tt(ssum.v, l4[:, :, 0, :], l4[:, :, 1, :], OP.add)
    p.tt(ssum.v, ssum.v, l4[:, :, 2, :], OP.add)
    p.tt(ssum.v, ssum.v, l4[:, :, 3, :], OP.add)
    p.recip(ssum.v, ssum.v)
    p.memset(lb.v, 0.0)
    for l in range(1, li + 1):
        p.tt(lb.v, lb.v, l4[:, :, l, :], OP.add)
    p.tt(lb.v, lb.v, ssum.v, OP.mult)
    p.ts(oml.v, lb.v, -1.0, 1.0, op0=OP.mult, op1=OP.add)

    def setupA(st):
        st["qs"] = st["ph"].rot("qs", [128, 8, 512], BF16, 1)
        st["fs"] = st["ph"].rot("fs", [128, 8, 512], F32, 2)

    def sinkA(st, jj, ps, t0, n, isctx):
        if jj < 8:
            if jj == 0:
                st["cq"] = st["qs"].next()
            p.act(st["cq"][:, jj, 0:n], ps[:, 0:n], AF.Silu)
            if jj == 7:
                p.ts(st["cq"][:, :, 0:n], st["cq"][:, :, 0:n], 128.0 ** -0.5, None, op0=OP.mult, e="pool")
                p.dma(V(qT.sub(t0), fm(qT).ap[:, :, t0:t0 + n]), st["cq"][:, :, 0:n])
        else:
            k = (jj - 8) % 8
            if k == 0:
                st["cf"] = st["fs"].next()
            p.copy(st["cf"][:, k, 0:n], ps[:, 0:n], e=("act" if jj % 2 else "dve"))
            if k == 7:
                half = (jj - 8) // 8
                p.dma(V(fT.sub((t0, half)), fm(fT).ap[:, half * 8:(half + 1) * 8, t0:t0 + n]), st["cf"][:, :, 0:n])

    phase_proj(K, 0, lambda kc: w_in[j, kc * 128:(kc + 1) * 128, 0:3072], 3072, sinkA, setup=setupA)

    def setupB(st):
        st["gs"] = st["ph"].rot("gs", [128, 8, 512], BF16, 1)
        st["vs"] = st["ph"].rot("vs", [128, 4, 1024], BF16, 2)

    def sinkB(st, jj, ps, t0, n, isctx):
        if jj < 8:
            return
        k = jj - 8
        if k == 0:
            st["cg"] = st["gs"].next()
        p.act(st["cg"][:, k, 0:n], ps[:, 0:n], AF.Silu)
        if k == 7:
            p.dma(V(sgT.sub(t0), fm(sgT).ap[:, :, t0:t0 + n]), st["cg"][:, :, 0:n])

    def extraB(st, hT, hf, t0, n, isctx):
        vs = st["vs"].next()
        W = st["W"]
        for jt in range(n // 128):
            for half in range(2):
                ps = K.ps[4 + half]
                for kc in range(KC):
                    p.mm(ps.v, hT[:, kc, jt * 128:(jt + 1) * 128], W[:, kc, half * 512:(half + 1) * 512],
                         start=(kc == 0), stop=(kc == KC - 1))
                p.copy(vs[:, jt, half * 512:(half + 1) * 512], ps.v, e=("act" if half else "dve"))
        p.dma(V(Vtm.sub(t0), Vtm.v.re("(jt q) c -> q jt c", q=128).ap[:, t0 // 128:(t0 + n) // 128, :]), vs[:, 0:n // 128, :])

    class SkipSink:
        pass

    def sinkB_wrap(st, jj, ps, t0, n, isctx):
        sinkB(st, jj, ps, t0, n, isctx)

    phase_proj(K, 0, lambda kc: w_in[j, kc * 128:(kc + 1) * 128, 3072:5120], 2048, sinkB_wrap, extraB, setup=setupB,
               )

    ph = Phase(p)
    S = ph.sb("hg_S", [128, 8, 128], F32)
    qf_r = ph.rot("hg_q", [128, 512], BF16, 2)
    ff_r = ph.rot("hg_f", [128, 512], F32, 2)
    t1_r = ph.rot("hg_t1", [128, 512], F32, 2)
    t2_r = ph.rot("hg_t2", [128, 512], F32, 2)
    t3_r = ph.rot("hg_t3", [128, 512], F32, 2)
    kk_r = ph.rot("hg_kk", [128, 512], F32, 2)
    qt_a = ph.sb("hg_qt", [128, 8, 512], BF16)
    kt_a = ph.sb("hg_kt", [128, 8, 512], BF16)
    kh_r = ph.rot("hg_kh", [128, 512], BF16, 2)
    khtm = ph.sb("hg_khtm", [128, 4, 8, 128], BF16)
    sc_a = ph.sb("hg_sc", [128, 8, 3, 8], F32)
    sm_r = ph.rot("hg_sm", [128, 4, 8], F32, 2)
    V_r = ph.rot("hg_V", [128, 4, 1024], BF16, 2)
    at_r = ph.rot("hg_at", [128, 8, 64], BF16, 2)
    Sp_r = ph.rot("hg_Sp", [128, 8, 128], BF16, 2)
    O_r = ph.rot("hg_O", [128, 8, 128], F32, 2)
    Of_r = ph.rot("hg_Of", [128, 8, 128], F32, 2)
    sg_r = ph.rot("hg_sg", [128, 8, 128], BF16, 2)
    sq_r = ph.rot("hg_sq", [128, 8, 128], BF16, 1)
    rs_r = ph.rot("hg_rs", [128, 8, 128], F32, 1)
    yn_r = ph.rot("hg_yn", [128, 8, 128], BF16, 2)

    blocks = sub_blocks(NT)

    def run(d):
        sig = 1.0 if d == 0 else -1.0
        p.memset(S.v, 0.0)
        order = blocks if d == 0 else [blocks[0]] + blocks[1:][::-1]
        pi = 0
        for (t0, n, isctx) in order:
            nch = n // 64
            ntl = n // 128
            Vt = V_r.next()
            p.dma(Vt[:, 0:ntl, :], Vtm.v.re("(jt q) c -> q jt c", q=128)[:, t0 // 128:(t0 + n) // 128, :])
            for h in range(8):
                qf = qf_r.next(); ff = ff_r.next()
                p.dma(qf[:, 0:n], qT[h * 128:(h + 1) * 128, t0:t0 + n])
                p.dma(ff[:, 0:n], fT[(d * 8 + h) * 128:(d * 8 + h + 1) * 128, t0:t0 + n])
                t1 = t1_r.next(); t2 = t2_r.next(); t3 = t3_r.next(); kk = kk_r.next()
                p.act(t1[:, 0:n], ff[:, 0:n], AF.Sigmoid)
                p.ts(t1[:, 0:n], t1[:, 0:n], oml[:, d, h:h + 1], lb[:, d, h:h + 1], op0=OP.mult, op1=OP.add)
                p.act(t2[:, 0:n], t1[:, 0:n], AF.Ln)
                p.ts(kk[:, 0:n], t1[:, 0:n], -1.0, 1.0, op0=OP.mult, op1=OP.add, e="pool")
                p.scan(t3[:, 0:n], K.ones_f[:, 0:1].bc([128, n]), t2[:, 0:n], 0.0, OP.mult, OP.add)
                sm = sm_r.next()
                ref, lo, hi, tm = sm[:, 0, 0:nch], sm[:, 1, 0:nch], sm[:, 2, 0:nch], sm[:, 3, 0:nch]
                P3 = t3[:, 0:n].re("q (c l) -> q c l", l=64)
                L3 = t2[:, 0:n].re("q (c l) -> q c l", l=64)
                p.tt(lo, P3[:, :, 0], L3[:, :, 0], OP.subtract)
                p.copy(hi, P3[:, :, 63])
                if d == 0:
                    Z = t3
                    p.copy(ref, P3[:, :, 32])
                else:
                    Z = t1
                    p.tt(Z[:, 0:n], t3[:, 0:n], t2[:, 0:n], OP.subtract)
                    p.copy(ref, Z[:, 0:n].re("q (c l) -> q c l", l=64)[:, :, 32])
                p.tt(Z[:, 0:n].re("q (c l) -> q c l", l=64), Z[:, 0:n].re("q (c l) -> q c l", l=64),
                     ref.unsq(2).bc([128, nch, 64]), OP.subtract)
                p.act(t2[:, 0:n], Z[:, 0:n], AF.Exp, scale=sig)
                p.tt(qt_a[:, h, 0:n], qf[:, 0:n], t2[:, 0:n], OP.mult)
                p.act(t2[:, 0:n], Z[:, 0:n], AF.Exp, scale=-sig)
                p.tt(kt_a[:, h, 0:n], kk[:, 0:n], t2[:, 0:n], OP.mult)
                sc = sc_a[:, h]
                a_, b_ = (0, 1) if d == 0 else (1, 0)
                p.tt(tm, ref, lo, OP.subtract)
                p.act(sc[:, a_, 0:nch], tm, AF.Exp)
                p.tt(tm, hi, ref, OP.subtract)
                p.act(sc[:, b_, 0:nch], tm, AF.Exp)
                p.tt(tm, hi, lo, OP.subtract)
                p.act(sc[:, 2, 0:nch], tm, AF.Exp)
                kh = kh_r.next()
                p.tt(kh[:, 0:n].re("q (c l) -> q c l", l=64), kt_a[:, h, 0:n].re("q (c l) -> q c l", l=64),
                     sc[:, 1, 0:nch].unsq(2).bc([128, nch, 64]), OP.mult)
                for jt in range(ntl):
                    ps = K.ps[7]
                    psb = ps.v.bitcast(BF16)
                    p.tr(psb[:, 0:128], kh[:, jt * 128:(jt + 1) * 128], K.ident_b.v)
                    p.copy(khtm[:, jt, h, :], psb[:, 0:128], e=("act" if jt % 2 else "dve"))
            tiles = list(range(ntl)) if d == 0 else list(range(ntl))[::-1]
            for jt in tiles:
                c0 = t0 + jt * 128
                Osb = O_r.next()
                for half in ((0, 1) if d == 0 else (1, 0)):
                    ci = jt * 2 + half
                    pb = half * 64
                    cs = slice(ci * 64, ci * 64 + 64)
                    pat = K.ps[pi % 2]
                    po = K.ps[2 + pi % 2]
                    pi += 1
                    at = at_r.next(); Sp = Sp_r.next()
                    mk = (K.mask_le_b if d == 0 else K.mask_ge_b)[pb:pb + 64, pb:pb + 64]
                    for h in range(8):
                        p.mm(pat[pb:pb + 64, h * 64:(h + 1) * 64], kt_a[:, h, cs], qt_a[:, h, cs])
                    p.tt(at[pb:pb + 64, :, :], pat[pb:pb + 64, :].re("q (h l) -> q h l", l=64),
                         mk.unsq(1).bc([64, 8, 64]), OP.mult)
                    p.tt(Sp.v, S.v, sc_a[:, :, 0, ci:ci + 1].bc([128, 8, 128]), OP.mult, e="pool")
                    for h in range(8):
                        o = po[:, h * 64:(h + 1) * 64]
                        p.mm(o, Vt[pb:pb + 64, jt, h * 128:(h + 1) * 128], at[pb:pb + 64, h, :], start=True, stop=False)
                        p.mm(o, Sp[:, h, :], qt_a[:, h, cs], start=False, stop=True)
                    p.copy(Osb[:, :, half * 64:(half + 1) * 64], po.v.re("q (h l) -> q h l", l=64), e="act")
                    pS = [K.ps[4], K.ps[5]]
                    for h in range(8):
                        p.mm(pS[h // 4][:, (h % 4) * 128:(h % 4 + 1) * 128], khtm[pb:pb + 64, jt, h, :],
                             Vt[pb:pb + 64, jt, h * 128:(h + 1) * 128])
                    p.tt(S.v, S.v, sc_a[:, :, 2, ci:ci + 1].bc([128, 8, 128]), OP.mult)
                    p.tt(S[:, 0:4, :], S[:, 0:4, :], pS[0].v.re("q (h v) -> q h v", v=128), OP.add)
                    p.tt(S[:, 4:8, :], S[:, 4:8, :], pS[1].v.re("q (h v) -> q h v", v=128), OP.add)
                if d == 0:
                    p.dma(V(OfT.sub(c0), fm(OfT).ap[:, :, c0:c0 + 128]), Osb.v)
                else:
                    if isctx and not K.want_ctx:
                        continue
                    Of = Of_r.next(); sg = sg_r.next()
                    p.dma(Of.v, V(OfT.sub(c0), fm(OfT).ap[:, :, c0:c0 + 128]))
                    p.dma(sg.v, fm(sgT)[:, :, c0:c0 + 128])
                    p.tt(Osb.v, Osb.v, Of.v, OP.add, e="pool")
                    sq = sq_r.next()
                    p.act(sq.v, Osb.v, AF.Square)
                    pr = [K.ps[6], K.ps[7]]
                    for hh in range(2):
                        p.mm(pr[hh].v, K.ones_b.v, sq[:, hh * 4:(hh + 1) * 4, :])
                    rs = rs_r.next()
                    for hh in range(2):
                        p.act(rs[:, hh * 4:(hh + 1) * 4, :], pr[hh].v.re("q (h l) -> q h l", l=128), AF.Ln,
                              scale=1.0 / 128, bias=K.eps_t[:, 0:1])
                    p.act(rs.v, rs.v, AF.Exp, scale=-0.5)
                    p.tt(Osb.v, Osb.v, rs.v, OP.mult)
                    yn = yn_r.next()
                    p.tt(yn.v, Osb.v, sg.v, OP.mult)
                    p.dma(V(ynT.sub(c0), fm(ynT).ap[:, :, c0:c0 + 128]), yn.v)

    run(0)
    p.barrier()
    run(1)
    ph.close()
    w_out = K.W["hgrn_w_out"]
    phase_outproj(K, ynT, 8, lambda kc: w_out[j, kc * 128:(kc + 1) * 128, :], 2, row_scale=ngT)
    lph.close()


import math


def attn_layer(K, li, j):
    p = K.p
    NT = K.NT
    TL = K.TL
    w_qkv = K.W["attn_w_qkv"]
    qkT = p.dram("at_qkT", [1280, NT], BF16)
    Vtm = p.dram("at_Vtm", [NT, 256], BF16)
    oT = p.dram("at_oT", [1024, NT], BF16)

    lph = Phase(p)
    gq = lph.sb("at_gq", [128, 2], F32)
    for half in range(2):
        p.dma(gq[half * 64:(half + 1) * 64, 0:1], K.W["attn_q_g"][j].unsq(1), allow_slow_non_contiguous=True)
        p.dma(gq[half * 64:(half + 1) * 64, 1:2], K.W["attn_k_g"][j].unsq(1), allow_slow_non_contiguous=True)

    def setupA(st):
        ph = st["ph"]
        bd = ph.sb("at_bd", [128, 128], BF16)
        p.memset(bd.v, 0.0)
        p.memset(bd[0:64, 0:64], 1.0)
        p.memset(bd[64:128, 64:128], 1.0)
        st["bd"] = bd
        mfree = ph.sb("at_mfree", [128, 128], F32)
        p.iota(mfree.v.re("q (a b c) -> q a b c", a=4, b=2), [[0, 4], [1, 2], [0, 16]], base=0, cm=0)
        up = ph.sb("at_up", [128, 128], F32)
        dn = ph.sb("at_dn", [128, 128], F32)
        p.aselect(up.v, K.ones_f.v, [[1, 128]], OP.is_equal, 0.0, base=-16, cm=-1)
        p.aselect(dn.v, K.ones_f.v, [[-1, 128]], OP.is_equal, 0.0, base=-16, cm=1)
        Rm = ph.sb("at_Rm", [128, 128], F32)
        p.tt(up.v, up.v, mfree.v, OP.mult)
        p.ts(mfree.v, mfree.v, -1.0, 1.0, op0=OP.mult, op1=OP.add)
        p.tt(dn.v, dn.v, mfree.v, OP.mult)
        p.tt(Rm.v, up.v, dn.v, OP.subtract)
        st["Rm"] = Rm
        pid = ph.sb("at_pid", [128, 1], I32)
        p.iota(pid.v, [[0, 1]], base=0, cm=1)
        p.emit("dve", [pid.v], [pid.v], lambda: p.nc.vector.tensor_single_scalar(pid.v.ap, pid.v.ap, 15, op=OP.bitwise_and))
        inv = ph.sb("at_inv", [128, 1], F32)
        p.copy(inv.v, pid.v)
        p.act(inv.v, inv.v, AF.Exp, scale=-math.log(10000.0) / 16.0)
        cosT = ph.sb("at_cos", [128, TL], F32)
        sinT = ph.sb("at_sin", [128, TL], F32)
        GW = 64
        rows = TL // GW
        for q0 in range(0, 128, 32):
            pat = [[1, rows], [0, GW]] if (q0 % 64) < 32 else [[0, rows], [1, GW]]
            p.iota(sinT[q0:q0 + 32, :].re("q (r c) -> q r c", c=GW), pat, base=0, cm=0)
        p.ts(sinT.v, sinT.v, inv[:, 0:1], None, op0=OP.mult)
        kk = ph.sb("at_kk", [128, TL], I32)
        p.ts(cosT.v, sinT.v, 1.0 / (2 * math.pi), 0.5, op0=OP.mult, op1=OP.add)
        p.copy(kk.v, cosT.v)
        p.copy(cosT.v, kk.v)
        p.stt(sinT.v, cosT.v, -2.0 * math.pi, sinT.v, OP.mult, OP.add)
        wt = ph.sb("at_wt", [128, TL], F32)

        def wrap(dst, src, shift):
            if shift != 0.0:
                p.ts(dst.v, src.v, shift, None, op0=OP.add)
            elif dst is not src:
                p.copy(dst.v, src.v)
            p.ts(wt.v, dst.v, math.pi, -2.0 * math.pi, op0=OP.is_gt, op1=OP.mult)
            p.tt(dst.v, dst.v, wt.v, OP.add)
            p.ts(wt.v, dst.v, -math.pi, 2.0 * math.pi, op0=OP.is_lt, op1=OP.mult)
            p.tt(dst.v, dst.v, wt.v, OP.add)

        wrap(sinT, sinT, 0.0)
        wrap(cosT, sinT, math.pi / 2)
        p.ts(sinT.v, sinT.v, math.pi, -math.pi, op0=OP.min, op1=OP.max)
        p.ts(cosT.v, cosT.v, math.pi, -math.pi, op0=OP.min, op1=OP.max)
        p.act(sinT.v, sinT.v, AF.Sin)
        p.act(cosT.v, cosT.v, AF.Sin)
        st["cos"], st["sin"] = cosT, sinT
        st["sq"] = ph.rot("at_sq", [128, 512], BF16, 2)
        st["rs"] = ph.rot("at_rs", [128, 512], F32, 2)
        st["qn"] = ph.rot("at_qn", [128, 512], F32, 2)
        st["t1"] = ph.rot("at_t1", [128, 512], F32, 2)
        st["t2"] = ph.rot("at_t2", [128, 512], F32, 2)
        st["ob"] = ph.rot("at_ob", [128, 512], BF16, 3)
        st["vs"] = ph.rot("at_vs", [128, 4, 256], BF16, 2)

    def sinkA(st, jj, ps, t0, n, isctx):
        if jj >= 10:
            return
        isk = 1 if jj >= 8 else 0
        sq = st["sq"].next()
        p.act(sq[:, 0:n], ps[:, 0:n], AF.Square)
        pss = K.ps[4 + jj % 2]
        p.mm(pss[:, 0:n], st["bd"].v, sq[:, 0:n])
        rs = st["rs"].next()
        p.act(rs[:, 0:n], pss[:, 0:n], AF.Sqrt, scale=1.0 / 64, bias=K.eps_t[:, 0:1])
        p.recip(rs[:, 0:n], rs[:, 0:n])
        qn = st["qn"].next()
        p.stt(qn[:, 0:n], ps[:, 0:n], gq[:, isk:isk + 1], rs[:, 0:n], OP.mult, OP.mult)
        ob = st["ob"].next()
        if isctx:
            p.copy(ob[:, 0:n], qn[:, 0:n], e="act")
        else:
            l0 = t0 - NCTX
            pr = K.ps[6 + jj % 2]
            p.mm(pr[:, 0:n], st["Rm"].v, qn[:, 0:n])
            t1 = st["t1"].next(); t2 = st["t2"].next()
            p.tt(t1[:, 0:n], qn[:, 0:n], st["cos"][:, l0:l0 + n], OP.mult, e="pool")
            p.tt(t2[:, 0:n], pr[:, 0:n], st["sin"][:, l0:l0 + n], OP.mult)
            p.tt(ob[:, 0:n], t1[:, 0:n], t2[:, 0:n], OP.add)
        p.dma(V(qkT.sub((t0, jj)), qkT.v.ap[jj * 128:(jj + 1) * 128, t0:t0 + n]), ob[:, 0:n])

    def extraA(st, hT, hf, t0, n, isctx):
        vs = st["vs"].next()
        W = st["W"]
        for jt in range(n // 128):
            ps = K.ps[5]
            for kc in range(KC):
                p.mm(ps[:, 0:256], hT[:, kc, jt * 128:(jt + 1) * 128], W[:, kc, 1280:1536], start=(kc == 0), stop=(kc == KC - 1))
            p.copy(vs[:, jt, :], ps[:, 0:256], e="act")
        p.dma(V(Vtm.sub(t0), Vtm.v.re("(jt q) c -> q jt c", q=128).ap[:, t0 // 128:(t0 + n) // 128, :]), vs[:, 0:n // 128, :])

    phase_proj(K, 0, lambda kc: w_qkv[j, kc * 128:(kc + 1) * 128, :], 1536, sinkA, extraA, setup=setupA)

    ph = Phase(p)
    NKT = NT // 128
    KT_r = ph.rot("at_KT", [64, NT], BF16, 2)
    Vg_r = ph.rot("at_Vg", [128, NKT, 128], BF16, 2)
    Q_r = ph.rot("at_Q", [64, 4, 128], BF16, 4)
    P_r = ph.rot("at_P", [128, 512], BF16, 4)
    rl_r = ph.rot("at_rl", [128, 512], F32, 2)
    rs_r = ph.rot("at_rs2", [64, 512], F32, 2)
    o_r = ph.rot("at_o", [64, 512], BF16, 2)
    shiftM = ph.sb("at_shift", [128, 64], F32)
    p.aselect(shiftM.v, K.ones_f[:, 0:64], [[-1, 64]], OP.is_equal, 0.0, base=-64, cm=1)
    for t in rl_r.tiles:
        p.memset(t.v, 0.0)
    nctx_t = NCTX // 128
    for g in range(4):
        KTg = KT_r.next(); Vg = Vg_r.next()
        p.dma(KTg.v, qkT[1024 + g * 64:1024 + (g + 1) * 64, :])
        p.memset(Vg[:, :, 64:128], 1.0, e="pool")
        p.dma(Vg[:, :, 0:64], Vtm.v.re("(kt q) (g d) -> q kt g d", q=128, g=4)[:, :, g, :])
        steps = []
        for jq in range(NKT):
            isctx = jq < nctx_t
            if isctx and not K.want_ctx:
                continue
            kts = list(range(nctx_t)) if isctx else list(range(NKT))
            for ki, kt in enumerate(kts):
                steps.append((jq, ki, kt, len(kts)))
        Qs = {}

        def getQ(jq):
            if jq not in Qs:
                Q = Q_r.next()
                p.dma(Q.v, qkT.v.re("(h d) t -> d h t", d=64)[:, g * 4:(g + 1) * 4, jq * 128:(jq + 1) * 128])
                Qs[jq] = Q
            return Qs[jq]

        def emitS(si):
            jq, ki, kt, nk = steps[si]
            Q = getQ(jq)
            p.mm(K.ps[si % 4].v, KTg[:, kt * 128:(kt + 1) * 128], Q.v.re("d r q -> d (r q)"))

        emitS(0)
        for si, (jq, ki, kt, nk) in enumerate(steps):
            if si + 1 < len(steps):
                emitS(si + 1)
            po = K.ps[4 + jq % 2]
            Pt = P_r.next()
            p.act(Pt.v, K.ps[si % 4].v, AF.Exp, scale=0.125)
            p.mm(po.v, Vg[:, kt, :], Pt.v, start=(ki == 0), stop=(ki == nk - 1))
            if ki == nk - 1:
                q0 = jq * 128
                rl = rl_r.next()
                p.recip(rl[64:128, :], po[64:128, :])
                pr = K.ps[6 + jq % 2]
                p.mm(pr[0:64, :], shiftM.v, rl.v)
                rs = rs_r.next()
                p.copy(rs.v, pr[0:64, :], e="act")
                o = o_r.next()
                p.tt(o.v, po[0:64, :], rs.v, OP.mult)
                p.dma(V(oT.sub((g, jq)), oT.v.re("(h d) t -> d h t", d=64).ap[:, g * 4:(g + 1) * 4, q0:q0 + 128]),
                      o.v.re("d (r q) -> d r q", r=4))
                Qs.pop(jq, None)
    ph.close()
    w_o = K.W["attn_w_o"]
    phase_outproj(K, oT, 8, lambda kc: w_o[j, kc * 128:(kc + 1) * 128, :], 2)
    lph.close()


def mlstm_layer(K, li, j):
    p = K.p
    NT = K.NT
    NTI = NT // 128
    nctx_t = NCTX // 128
    w_up = K.W["mlstm_w_up"]
    xmT = p.dram("ml_xmT", [2048, NT], BF16)
    szT = p.dram("ml_szT", [2048, NT], BF16)
    xcT = p.dram("ml_xcT", [2048, NT], BF16)
    sxcT = p.dram("ml_sxcT", [2048, NT], BF16)
    qT = p.dram("ml_qT", [2048, NT], BF16)
    kT = p.dram("ml_kT", [2048, NT], BF16)
    ktm = p.dram("ml_ktm", [NT, 2048], BF16)
    vtm = p.dram("ml_vtm", [NT, 2048], BF16)
    HfT = p.dram("ml_HfT", [2048, NT], F32)
    ynT = p.dram("ml_ynT", [2048, NT], BF16)
    fm = lambda t: t.v.re("(c q) t -> q c t", q=128)
    tmv = lambda t: t.v.re("(jt q) c -> q jt c", q=128)

    lph = Phase(p)
    convw = load_vec_fm(K, lph, K.W["mlstm_conv_w"][j].re("k (n q) -> (k n) q", q=128), 5 * 16, "ml_cw")
    convb = load_vec_fm(K, lph, K.W["mlstm_conv_b"][j].re("(n q) -> n q", q=128), 16, "ml_cb")
    ngT = load_vec_fm(K, lph, K.W["mlstm_norm_g"][j].re("(n q) -> n q", q=128), 16, "ml_ng")
    skT = load_vec_fm(K, lph, K.W["mlstm_skip"][j].re("(n q) -> n q", q=128), 16, "ml_sk")
    li_all = lph.sb("ml_li", [128, NTI, 8], F32)
    lf_all = lph.sb("ml_lf", [128, NTI, 8], F32)
    bg = lph.sb("ml_bg", [16, 1], F32)
    p.dma(bg.v, K.W["mlstm_b_gate"][j].unsq(1), allow_slow_non_contiguous=True)

    def setup1(st):
        st["xs"] = st["ph"].rot("ml_xs", [128, 16, 512], BF16, 2)

    def sink1(st, jj, ps, t0, n, isctx):
        k = jj % 16
        if k == 0:
            st["cur"] = st["xs"].next()
        if jj < 16:
            p.copy(st["cur"][:, k, 0:n], ps[:, 0:n], e=("act" if jj % 2 else "dve"))
        else:
            p.act(st["cur"][:, k, 0:n], ps[:, 0:n], AF.Silu)
        if k == 15:
            dst = xmT if jj < 16 else szT
            p.dma(V(dst.sub(t0), fm(dst).ap[:, :, t0:t0 + n]), st["cur"][:, :, 0:n])

    phase_proj(K, 0, lambda kc: w_up[j, kc * 128:(kc + 1) * 128, :], 4096, sink1, setup=setup1)
    if getattr(K, "dbg_stop", 0) == 1:
        lph.close()
        return

    ph = Phase(p)
    CB = 1024
    xin_r = ph.rot("cv_in", [128, 4, CB + 4], BF16, 2)
    acc_r = ph.rot("cv_acc", [128, CB], F32, 2)
    out_r = ph.rot("cv_out", [128, 4, CB], BF16, 2)
    sx_r = ph.rot("cv_sx", [128, 4, CB], BF16, 2)
    for s0, slen in [(0, NCTX), (NCTX, NT - NCTX)]:
        for b0 in range(0, slen, CB):
            n = min(CB, slen - b0)
            t0 = s0 + b0
            lo = 2 if b0 > 0 else 0
            hi = 2 if b0 + n < slen else 0
            for cg in range(4):
                xin = xin_r.next()
                if lo == 0:
                    p.memset(xin[:, :, 0:2], 0.0, e="pool")
                if hi == 0:
                    p.memset(xin[:, :, n + 2:n + 4], 0.0, e="pool")
                p.dma(xin[:, :, 2 - lo:n + 2 + hi], fm(xmT)[:, cg * 4:(cg + 1) * 4, t0 - lo:t0 + n + hi])
                out = out_r.next(); sx = sx_r.next()
                for c4 in range(4):
                    cc = cg * 4 + c4
                    acc = acc_r.next()
                    p.ts(acc[:, 0:n], xin[:, c4, 0:n], convw[:, cc:cc + 1], convb[:, cc:cc + 1], op0=OP.mult, op1=OP.add)
                    for k in range(1, 5):
                        p.stt(acc[:, 0:n], xin[:, c4, k:k + n], convw[:, k * 16 + cc:k * 16 + cc + 1], acc[:, 0:n], OP.mult, OP.add)
                    p.act(out[:, c4, 0:n], acc[:, 0:n], AF.Silu)
                    p.act(sx[:, c4, 0:n], out[:, c4, 0:n], AF.Identity, scale=skT[:, cc:cc + 1])
                p.dma(V(xcT.sub((t0, cg)), fm(xcT).ap[:, cg * 4:(cg + 1) * 4, t0:t0 + n]), out[:, :, 0:n])
                p.dma(V(sxcT.sub((t0, cg)), fm(sxcT).ap[:, cg * 4:(cg + 1) * 4, t0:t0 + n]), sx[:, :, 0:n])
    ph.close()
    if getattr(K, "dbg_stop", 0) == 2:
        lph.close()
        return

    ph = Phase(p)
    Wq = ph.sb("ml_Wq", [128, 16, 512], BF16)
    Wk = ph.sb("ml_Wk", [128, 16, 512], BF16)
    Wv = ph.sb("ml_Wv", [128, 16, 512], BF16)
    for Wt, nm in ((Wq, "mlstm_w_q"), (Wk, "mlstm_w_k"), (Wv, "mlstm_w_v")):
        src = K.W[nm][j].re("h (dc q) e -> q (h dc) e", q=128)
        for i4 in range(4):
            p.dma(Wt[:, i4 * 4:(i4 + 1) * 4, :], src[:, i4 * 4:(i4 + 1) * 4, :], q="pool", max_dma_last_dim=2048 * 4)
    Wg = ph.sb("ml_Wg", [128, 48, 16], BF16)
    p.dma(Wg.v, K.W["mlstm_w_gate"][j].re("(c q) g -> q c g", q=128), q="pool")
    xc_r = ph.rot("ml_xc", [128, 16, 512], BF16, 1)
    xm_r = ph.rot("ml_xm", [128, 16, 512], BF16, 1)
    qkv_r = ph.rot("ml_qkv", [128, 48, 512], BF16, 1)
    ks_r = ph.rot("ml_ks", [128, 16, 512], BF16, 1)
    tm_r = ph.rot("ml_tm", [128, 4, 2048], BF16, 1)
    gT_r = ph.rot("ml_gT", [16, 512], F32, 2)
    g16_r = ph.rot("ml_g16", [128, 16], F32, 2)
    pi = 0
    import os
    P3L = int(os.environ.get("DBG_P3", "9"))
    for (t0, n, isctx) in sub_blocks(NT):
        if P3L < 9 and t0 > 0:
            break
        if P3L < 1:
            break
        ntl = n // 128
        xc = xc_r.next(); xm = xm_r.next()
        p.dma(xc[:, :, 0:n], fm(xcT)[:, :, t0:t0 + n])
        p.dma(xm[:, :, 0:n], fm(xmT)[:, :, t0:t0 + n])
        qkv = qkv_r.next()
        ks = ks_r.next()
        for which, (Wt, src) in enumerate(((Wq, xc), (Wk, xc), (Wv, xm))):
            for h in range(4):
                for oc in range(4):
                    ps = K.ps[pi % 4]
                    pi += 1
                    for dc in range(4):
                        p.mm(ps[:, 0:n], Wt[:, h * 4 + dc, oc * 128:(oc + 1) * 128], src[:, h * 4 + dc, 0:n],
                             start=(dc == 0), stop=(dc == 3))
                    idx = which * 16 + h * 4 + oc
                    p.copy(qkv[:, idx, 0:n], ps[:, 0:n], e=("act" if pi % 2 else "dve"))
                    if which == 1:
                        p.ts(ks[:, h * 4 + oc, 0:n], ps[:, 0:n], 512.0 ** -0.5, None, op0=OP.mult, e="dve")
        if P3L < 2:
            continue
        pg = K.ps[4]
        for c in range(48):
            p.mm(pg[0:16, 0:n], Wg[:, c, :], qkv[:, c, 0:n], start=(c == 0), stop=(c == 47))
        gT = gT_r.next()
        p.act(gT[:, 0:n], pg[0:16, 0:n], AF.Identity, bias=bg[:, 0:1])
        if P3L < 3:
            continue
        for jt in range(ntl):
            ti = t0 // 128 + jt
            pt = K.ps[5]
            p.tr(pt[:, 0:16], gT[:, jt * 128:(jt + 1) * 128], K.ident_f[0:16, 0:16])
            g16 = g16_r.next()
            p.copy(g16.v, pt[:, 0:16])
            g4 = g16.v.re("q (k h) -> q k h", k=4)
            la = li_all[:, ti, :].re("q (d h) -> q d h", d=2)
            lfv = lf_all[:, ti, :].re("q (d h) -> q d h", d=2)
            p.copy(la[:, 0, :], g4[:, 0, :])
            p.copy(la[:, 1, :], g4[:, 2, :])
            p.act(lfv[:, 0, :], g4[:, 1, :], AF.Exp, scale=-1.0)
            p.act(lfv[:, 1, :], g4[:, 3, :], AF.Exp, scale=-1.0)
            p.act(lf_all[:, ti, :], lf_all[:, ti, :], AF.Ln, bias=1.0)
            p.ts(lf_all[:, ti, :], lf_all[:, ti, :], -1.0, None, op0=OP.mult)
        if P3L < 4:
            continue
        p.dma(V(qT.sub(t0), fm(qT).ap[:, :, t0:t0 + n]), qkv[:, 0:16, 0:n])
        p.dma(V(kT.sub(t0), fm(kT).ap[:, :, t0:t0 + n]), ks[:, :, 0:n])
        if P3L < 5:
            continue
        for which, (src, s0, dst) in enumerate(((ks, 0, ktm), (qkv, 32, vtm))):
            tm = tm_r.next()
            for jt in range(ntl):
                for c4 in range(4):
                    ps = K.ps[6 + pi % 2]
                    pi += 1
                    psb = ps.v.bitcast(BF16)
                    for cc in range(4):
                        p.tr(psb[:, cc * 128:(cc + 1) * 128], src[:, s0 + c4 * 4 + cc, jt * 128:(jt + 1) * 128], K.ident_b.v)
                    p.copy(tm[:, jt, c4 * 512:(c4 + 1) * 512], psb[:, 0:512], e=("act" if pi % 2 else "dve"))
            p.dma(V(dst.sub(t0), tmv(dst).ap[:, t0 // 128:(t0 + n) // 128, :]), tm[:, 0:ntl, :])
    ph.close()
    if getattr(K, "dbg_stop", 0) == 3:
        lph.close()
        return

    ph = Phase(p)
    S32 = ph.sb("ml_S32", [128, 16, 512], F32)
    Sbf = ph.sb("ml_Sbf", [128, 16, 512], BF16)
    n32 = ph.sb("ml_n32", [128, 16], F32)
    nbf = ph.sb("ml_nbf", [128, 16], BF16)
    q_r = ph.rot("ms_q", [128, 16, 128], BF16, 2)
    k_r = ph.rot("ms_k", [128, 16, 128], BF16, 2)
    kt_r = ph.rot("ms_kt", [128, 2048], BF16, 2)
    vt_r = ph.rot("ms_vt", [128, 2048], BF16, 2)
    X2_r = ph.rot("ms_X2", [128, 2048], BF16, 2)
    sm_r = ph.rot("ms_sm", [128, 4, 4], F32, 3)
    wb_r = ph.rot("ms_wb", [128, 4], BF16, 3)
    E_r = ph.rot("ms_E", [128, 4, 128], F32, 2)
    ea_r = ph.rot("ms_ea", [128, 4, 128], F32, 2)
    WT_r = ph.rot("ms_WT", [128, 4, 128], BF16, 2)
    Cp_r = ph.rot("ms_Cp", [128, 16, 128], BF16, 2)
    rd_r = ph.rot("ms_rd", [128, 4, 128], F32, 2)
    H_r = ph.rot("ms_H", [128, 16, 128], F32, 2)
    Hf_r = ph.rot("ms_Hf", [128, 16, 128], F32, 2)
    sx_r = ph.rot("ms_sx", [128, 16, 128], BF16, 2)
    sz_r = ph.rot("ms_sz", [128, 16, 128], BF16, 2)
    sq_r = ph.rot("ms_sq", [128, 16, 128], BF16, 1)
    rs_r = ph.rot("ms_rs", [128, 4, 128], F32, 1)
    yn_r = ph.rot("ms_yn", [128, 16, 128], BF16, 2)

    def run(d, chunks):
        mask = K.mask_le if d == 0 else K.mask_ge
        p.memset(S32.v, 0.0)
        p.memset(Sbf.v, 0.0, e="pool")
        p.memset(n32.v, 0.0)
        p.memset(nbf.v, 0.0, e="pool")

        def stageAB(ci):
            c0 = ci * 128
            qc = q_r.next(); kc_ = k_r.next(); kt = kt_r.next(); vt = vt_r.next()
            p.dma(qc.v, fm(qT)[:, :, c0:c0 + 128])
            p.dma(kc_.v, fm(kT)[:, :, c0:c0 + 128])
            p.dma(kt.v, ktm[c0:c0 + 128, :])
            p.dma(vt.v, vtm[c0:c0 + 128, :])
            lf = lf_all[:, ci, d * 4:(d + 1) * 4]
            lii = li_all[:, ci, d * 4:(d + 1) * 4]
            pm = K.ps[7]
            p.mm(pm[:, 0:4], mask.v, lf)
            p.mm(pm[:, 4:8], K.ones_f.v, lf)
            sm = sm_r.next()
            nb = sm[:, 1, :]; wend = sm[:, 2, :]; et = sm[:, 3, :]
            p.tt(nb, lii, pm[:, 0:4], OP.subtract)
            p.tt(wend, pm[:, 4:8], nb, OP.add)
            p.act(wend, wend, AF.Exp)
            p.act(et, pm[:, 4:8], AF.Exp)
            wb = wb_r.next()
            p.copy(wb.v, wend)
            X2 = X2_r.next()
            for h in range(4):
                p.ts(X2[:, h * 512:(h + 1) * 512], vt[:, h * 512:(h + 1) * 512], wend[:, h:h + 1], None, op0=OP.mult,
                     e=("pool" if h % 2 else "dve"))
            pa = K.ps[0]
            pav = pa.v.re("q (r l) -> q r l", r=4)
            pg = K.ps[1]
            pgv = pg.v.re("q (r l) -> q r l", r=4)
            for h in range(4):
                p.mm(pav[:, h, :], lf[:, h:h + 1].bc([128, 128]), mask.v)
            for h in range(4):
                for dc in range(4):
                    p.mm(pgv[:, h, :], kc_[:, h * 4 + dc, :], qc[:, h * 4 + dc, :], start=(dc == 0), stop=(dc == 3))
            E = E_r.next(); ea = ea_r.next()
            for h in range(4):
                p.act(E[:, h, :], pav[:, h, :], AF.Exp, bias=nb[:, h:h + 1])
            p.act(ea.v, pav, AF.Exp)
            if d == 0:
                p.aselect(E.v, E.v, [[0, 4], [1, 128]], OP.is_ge, 0.0, base=0, cm=-1)
            else:
                p.aselect(E.v, E.v, [[0, 4], [-1, 128]], OP.is_ge, 0.0, base=0, cm=1)
            WT = WT_r.next(); Cp = Cp_r.next()
            p.tt(WT.v, E.v, pgv, OP.mult)
            p.tt(Cp.v.re("q (h c) l -> q h c l", h=4), qc.v.re("q (h c) l -> q h c l", h=4),
                 ea.v.unsq(2).bc([128, 4, 4, 128]), OP.mult)
            return dict(kt=kt, vt=vt, X2=X2, wb=wb, et=et, WT=WT, Cp=Cp)

        def stageC(ci, H_):
            c0 = ci * 128
            isctx = ci < nctx_t
            kt, vt, X2, wb, et, WT, Cp = H_["kt"], H_["vt"], H_["X2"], H_["wb"], H_["et"], H_["WT"], H_["Cp"]
            pd = K.ps[2]
            pdv = pd.v.re("q (r l) -> q r l", r=4)
            for h in range(4):
                p.mm(pdv[:, h, :], K.ones_b.v, WT[:, h, :], start=True, stop=False)
                for dc in range(4):
                    p.mm(pdv[:, h, :], nbf[:, h * 4 + dc:h * 4 + dc + 1].bc([128, 128]), Cp[:, h * 4 + dc, :],
                         start=False, stop=(dc == 3))
            rd = rd_r.next()
            p.ts(rd.v, pdv, -1.0, 1.0, op0=OP.mult, op1=OP.max)
            p.tt(rd.v, rd.v, pdv, OP.max)
            p.recip(rd.v, rd.v)
            Hsb = H_r.next()
            for h in range(4):
                py = K.ps[3 + h % 2]
                pyv = py.v.re("q (c l) -> q c l", c=4)
                for pc in range(4):
                    o = pyv[:, pc, :]
                    p.mm(o, vt[:, h * 512 + pc * 128:h * 512 + (pc + 1) * 128], WT[:, h, :], start=True, stop=False)
                    for dc in range(4):
                        p.mm(o, V(Sbf.sub(h), Sbf.h[:, h * 4 + dc, pc * 128:(pc + 1) * 128]), Cp[:, h * 4 + dc, :],
                             start=False, stop=(dc == 3))
                p.tt(Hsb[:, h * 4:(h + 1) * 4, :], pyv, rd[:, h, :].unsq(1).bc([128, 4, 128]), OP.mult)
                for dc in range(4):
                    pst = K.ps[5 + dc % 2]
                    p.mm(pst.v, kt[:, h * 512 + dc * 128:h * 512 + (dc + 1) * 128], X2[:, h * 512:(h + 1) * 512])
                    sg = V(S32.sub(h), S32.h[:, h * 4 + dc, :])
                    p.stt(sg, sg, et[:, h:h + 1], pst.v, OP.mult, OP.add)
                    p.copy(V(Sbf.sub(h), Sbf.h[:, h * 4 + dc, :]), sg, e=("act" if dc % 2 else "pool"))
            pn = K.ps[7]
            for h in range(4):
                for dc in range(4):
                    p.mm(pn[:, 16 + h * 4 + dc:16 + h * 4 + dc + 1], kt[:, h * 512 + dc * 128:h * 512 + (dc + 1) * 128],
                         wb[:, h:h + 1])
            p.tt(n32.v.re("q (h c) -> q h c", h=4), n32.v.re("q (h c) -> q h c", h=4), et.unsq(2).bc([128, 4, 4]), OP.mult)
            p.tt(n32.v, n32.v, pn[:, 16:32], OP.add)
            p.copy(nbf.v, n32.v)
            if d == 0:
                p.dma(V(HfT.sub(ci), fm(HfT).ap[:, :, c0:c0 + 128]), Hsb.v)
            else:
                if isctx and not K.want_ctx:
                    return
                Hf = Hf_r.next(); sx = sx_r.next(); sz = sz_r.next()
                p.dma(Hf.v, V(HfT.sub(ci), fm(HfT).ap[:, :, c0:c0 + 128]))
                p.dma(sx.v, fm(sxcT)[:, :, c0:c0 + 128])
                p.dma(sz.v, fm(szT)[:, :, c0:c0 + 128])
                p.tt(Hsb.v, Hsb.v, Hf.v, OP.add, e="pool")
                sq = sq_r.next()
                p.act(sq.v, Hsb.v, AF.Square)
                pr = K.ps[2]
                for h in range(4):
                    for pc in range(4):
                        p.mm(pr[:, h * 128:(h + 1) * 128], K.ones_b.v, sq[:, h * 4 + pc, :], start=(pc == 0), stop=(pc == 3))
                rs = rs_r.next()
                p.act(rs.v, pr.v.re("q (h l) -> q h l", h=4), AF.Ln, scale=1.0 / 512, bias=K.eps_t[:, 0:1])
                p.act(rs.v, rs.v, AF.Exp, scale=-0.5)
                p.tt(Hsb.v.re("q (h c) l -> q h c l", h=4), Hsb.v.re("q (h c) l -> q h c l", h=4),
                     rs.v.unsq(2).bc([128, 4, 4, 128]), OP.mult)
                for c in range(16):
                    p.stt(Hsb[:, c, :], Hsb[:, c, :], ngT[:, c:c + 1], sx[:, c, :], OP.mult, OP.add)
                yn = yn_r.next()
                p.tt(yn.v, Hsb.v, sz.v, OP.mult)
                p.dma(V(ynT.sub(ci), fm(ynT).ap[:, :, c0:c0 + 128]), yn.v)

        cur = stageAB(chunks[0])
        for i, ci in enumerate(chunks):
            nxt = stageAB(chunks[i + 1]) if i + 1 < len(chunks) else None
            stageC(ci, cur)
            cur = nxt

    ctx_chunks = list(range(nctx_t))
    lat_chunks = list(range(nctx_t, NTI))
    run(0, ctx_chunks + lat_chunks)
    p.barrier()
    if getattr(K, "dbg_stop", 0) != 4:
        run(1, ctx_chunks[::-1] + lat_chunks[::-1])
    ph.close()
    w_down = K.W["mlstm_w_down"]
    phase_outproj(K, ynT, 16, lambda kc: w_down[j, kc * 128:(kc + 1) * 128, :], 2)
    lph.close()


from concourse.bass_utils import run_bass_kernel_spmd

W_SHAPES = {
    "ada_w": (4, 1024, 6144), "ada_b": (4, 6144), "norm_g": (4, 2, 1024),
    "ssd_w_in": (1, 1024, 6208), "ssd_conv_w": (1, 5, 4096), "ssd_conv_b": (1, 4096), "ssd_dt_bias": (1, 2, 32),
    "ssd_a_log": (1, 2, 32), "ssd_d": (1, 32), "ssd_norm_g": (1, 2048), "ssd_w_out": (1, 2048, 1024),
    "hgrn_w_in": (1, 1024, 5120), "hgrn_lb": (2, 4, 1024), "hgrn_norm_g": (1, 1024), "hgrn_w_out": (1, 1024, 1024),
    "attn_w_qkv": (1, 1024, 1536), "attn_q_g": (1, 64), "attn_k_g": (1, 64), "attn_w_o": (1, 1024, 1024),
    "mlstm_w_up": (1, 1024, 4096), "mlstm_conv_w": (1, 5, 2048), "mlstm_conv_b": (1, 2048),
    "mlstm_w_q": (1, 4, 512, 512), "mlstm_w_k": (1, 4, 512, 512), "mlstm_w_v": (1, 4, 512, 512),
    "mlstm_w_gate": (1, 6144, 16), "mlstm_b_gate": (1, 16), "mlstm_skip": (1, 2048), "mlstm_norm_g": (1, 2048),
    "mlstm_w_down": (1, 2048, 1024),
    "ffn_w1": (2, 1024, 3584), "ffn_w3": (2, 1024, 3584), "ffn_w2": (2, 3584, 1024),
    "moe_router": (2, 1024, 8), "moe_w1": (2, 8, 1024, 3584), "moe_w3": (2, 8, 1024, 3584), "moe_w2": (2, 8, 3584, 1024),
}

LAYER_W = {
    0: ["ssd_w_in", "ssd_conv_w", "ssd_conv_b", "ssd_dt_bias", "ssd_a_log", "ssd_d", "ssd_norm_g", "ssd_w_out",
        "ffn_w1", "ffn_w3", "ffn_w2"],
    1: ["hgrn_w_in", "hgrn_lb", "hgrn_norm_g", "hgrn_w_out", "moe_router", "moe_w1", "moe_w3", "moe_w2"],
    2: ["attn_w_qkv", "attn_q_g", "attn_k_g", "attn_w_o", "ffn_w1", "ffn_w3", "ffn_w2"],
    3: ["mlstm_w_up", "mlstm_conv_w", "mlstm_conv_b", "mlstm_w_q", "mlstm_w_k", "mlstm_w_v", "mlstm_w_gate",
        "mlstm_b_gate", "mlstm_skip", "mlstm_norm_g", "mlstm_w_down", "moe_router", "moe_w1", "moe_w3", "moe_w2"],
}


def needed_weights(layers):
    names = ["ada_w", "ada_b", "norm_g"]
    for li in layers:
        for n in LAYER_W[li]:
            if n not in names:
                names.append(n)
    return names


def build(TL, layers, debug_full_out=False, stop_after=None, dbg_stop=0, split_last=False):
    nc = bass.Bass("TRN2", target_bir_lowering=False)
    p = P(nc)
    import os
    p.scopes = bool(os.environ.get('KSCOPES'))
    K = Ctx()
    K.p = p
    K.NT = NCTX + TL
    K.TL = TL
    K.FFN_BLK = 1280
    K.dbg_stop = dbg_stop
    K.MOE_BLK = 1024
    K.xin = p.dram("xin", [K.NT, D], F32, kind="ExternalInput")
    K.c_row = p.dram("c_row", [2, D], F32, kind="ExternalInput")
    K.W = {}
    for n in needed_weights(layers):
        K.W[n] = p.dram(n, list(W_SHAPES[n]), F32, kind="ExternalInput")
    K.ada_w, K.ada_b, K.norm_g = K.W["ada_w"], K.W["ada_b"], K.W["norm_g"]
    K.debug_full_out = debug_full_out
    _NOCTX[0] = False
    K.split_last = split_last
    if split_last:
        K.selv = p.dram("selv", [128, 2], F32, kind="ExternalInput")
    K.xout = p.dram("xout", [K.NT if debug_full_out else (TL // 2 if split_last else TL), D], F32, kind="ExternalOutput")
    K.xT = p.dram("xT", [D, K.NT], F32)
    setup_consts(K)
    phase_load_input(K)
    for li in layers:
        K.want_ctx = li < 3
        phase_mod(K, li)
        K.modT, K.gm = K.modTs[li], K.gms[li]
        if stop_after == "mod":
            break
        if li == 0:
            ssd_layer(K, li, 0)
        elif li == 1:
            hgrn_layer(K, li, 0)
        elif li == 2:
            attn_layer(K, li, 0)
        else:
            mlstm_layer(K, li, 0)
        if stop_after == "mixer":
            break
        if split_last and li == layers[-1]:
            phase_select_half(K)
        if li % 2 == 0:
            w1, w3, w2 = K.W["ffn_w1"], K.W["ffn_w3"], K.W["ffn_w2"]
            jj = li // 2
            phase_ffn(K, [None],
                      lambda e, c0, n_: w1[jj].re("(c q) f -> q c f", q=128)[:, :, c0:c0 + n_],
                      lambda e, c0, n_: w3[jj].re("(c q) f -> q c f", q=128)[:, :, c0:c0 + n_],
                      lambda e, k0, nk: w2[jj].re("(c q) o -> q c o", q=128)[:, k0:k0 + nk, :])
        else:
            w1, w3, w2 = K.W["moe_w1"], K.W["moe_w3"], K.W["moe_w2"]
            jj = li // 2
            phase_ffn(K, list(range(8)),
                      lambda e, c0, n_: w1[jj, e].re("(c q) f -> q c f", q=128)[:, :, c0:c0 + n_],
                      lambda e, c0, n_: w3[jj, e].re("(c q) f -> q c f", q=128)[:, :, c0:c0 + n_],
                      lambda e, k0, nk: w2[jj, e].re("(c q) o -> q c o", q=128)[:, k0:k0 + nk, :],
                      router=K.W["moe_router"][jj])
    phase_store_output(K)
    p.finish()
    _NOCTX[0] = False
    return nc, K


def run_cores(nc, layers, xins, crows, weights):
    names = needed_weights(layers)
    in_maps = []
    for xi, cr in zip(xins, crows):
        m = {"xin": xi, "c_row": cr}
        for n in names:
            m[n] = weights[n]
        in_maps.append(m)
    res = run_bass_kernel_spmd(nc, in_maps, core_ids=list(range(len(in_maps))))
    return [r["xout"] for r in res.results]


def kernel(**inputs):
    x = np.asarray(inputs["x"], dtype=np.float32)
    c = np.asarray(inputs["c"], dtype=np.float32)
    ctx = np.asarray(inputs["ctx"], dtype=np.float32)
    c_ctx = np.asarray(inputs["c_ctx"], dtype=np.float32)
    B, T, _ = x.shape
    layers = [0, 1, 2, 3]
    nc, K = build(T, layers, split_last=True)
    names = needed_weights(layers)
    weights = {n: np.ascontiguousarray(np.asarray(inputs[n], dtype=np.float32)) for n in names}
    in_maps = []
    for i in range(2 * B):
        b, half = i % B, i // B
        m = {"xin": np.ascontiguousarray(np.concatenate([ctx[b], x[b]], axis=0)),
             "c_row": np.ascontiguousarray(np.stack([c[b], c_ctx], axis=0)),
             "selv": np.ascontiguousarray(np.tile(np.array([[1.0 - half, float(half)]], np.float32), (128, 1)))}
        for n in names:
            m[n] = weights[n]
        in_maps.append(m)
    res = run_bass_kernel_spmd(nc, in_maps, core_ids=list(range(2 * B)))
    out = np.empty((B, T, D), np.float32)
    H = T // 2
    for i in range(2 * B):
        b, half = i % B, i // B
        out[b, half * H:(half + 1) * H] = res.results[i]["xout"]
    return out
```
